# Optimizing a Trainium2 kernel written in Bass

```python
import math
import jax, jax.numpy as jnp
from jax import lax
import numpy as np

D_MODEL = 2048
BATCH = 2
SEQ = 4096
DEPTH = 2

MIX_WIDTH = D_MODEL // 2
N_BRANCH = 4
EPS = 1e-6

LRU_WIDTH = MIX_WIDTH
LRU_BLOCKS = 8
LRU_BLOCK = LRU_WIDTH // LRU_BLOCKS
CONV_WIDTH = 4
LRU_C = 8.0

RET_HEADS = 8
RET_QK_DIM = MIX_WIDTH // (2 * RET_HEADS)
RET_V_DIM = MIX_WIDTH // RET_HEADS
RET_CHUNK = 128
ROPE_THETA = 10000.0

SGU_WIDTH = MIX_WIDTH
SGU_GROUPS = 8
SGU_CHUNK = 128

RWKV_HEAD_DIM = 64
RWKV_HEADS = MIX_WIDTH // RWKV_HEAD_DIM
DECAY_LORA = 64
ICLR_LORA = 64
GATE_LORA = 128
RWKV_FEAT = 3 * MIX_WIDTH + 2 * DECAY_LORA + 2 * ICLR_LORA + GATE_LORA
DECAY_SCALE = math.exp(-0.5)

N_GROUPS = 4
EXPERTS_PER_GROUP = 8
N_EXPERTS = N_GROUPS * EXPERTS_PER_GROUP
TOP_K = 2
EXPERT_HIDDEN = D_MODEL // 4
MOE_BLOCK = 256

IN_SPLITS = (LRU_WIDTH, LRU_WIDTH,
             RET_HEADS * RET_QK_DIM, RET_HEADS * RET_QK_DIM, RET_HEADS * RET_V_DIM, RET_HEADS * RET_V_DIM,
             SGU_WIDTH, SGU_WIDTH,
             RWKV_FEAT,
             N_BRANCH * D_MODEL)
IN_WIDTH = sum(IN_SPLITS)

kernel_name = "hybrid_bidir_lru_ret_sgu_rwkv7_hmoe"

F32 = jnp.float32


def rms_norm(x, g):
    xf = x.astype(F32)
    y = xf * lax.rsqrt(jnp.mean(xf * xf, axis=-1, keepdims=True) + EPS)
    return (y * g.astype(F32)).astype(x.dtype)


def head_norm(x):
    xf = x.astype(F32)
    mu = jnp.mean(xf, axis=-1, keepdims=True)
    var = jnp.mean(jnp.square(xf - mu), axis=-1, keepdims=True)
    return ((xf - mu) * lax.rsqrt(var + EPS)).astype(x.dtype)


def rope(x, cos, sin):
    x1, x2 = jnp.split(x, 2, axis=-1)
    return jnp.concatenate([x1 * cos - x2 * sin, x2 * cos + x1 * sin], axis=-1)


def linear_scan_combine(left, right):
    a_l, b_l = left
    a_r, b_r = right
    return a_l * a_r, a_r * b_l + b_r


def rglru_branch(x_in, gate_in, conv_w, conv_b, w_r, b_r, w_i, b_i, lam):
    B, S, W = x_in.shape
    xc = lax.conv_general_dilated(
        x_in, conv_w[:, None, :], window_strides=(1,),
        padding=[(CONV_WIDTH // 2, CONV_WIDTH - 1 - CONV_WIDTH // 2)],
        dimension_numbers=("NWC", "WIO", "NWC"), feature_group_count=W) + conv_b
    xf = xc.astype(F32)
    xb = xf.reshape(B, S, LRU_BLOCKS, LRU_BLOCK)
    r = jax.nn.sigmoid(jnp.einsum("bsne,znef->zbsnf", xb, w_r.astype(F32)).reshape(2, B, S, W) + b_r[:, None, None, :])
    i = jax.nn.sigmoid(jnp.einsum("bsne,znef->zbsnf", xb, w_i.astype(F32)).reshape(2, B, S, W) + b_i[:, None, None, :])
    log_a = -LRU_C * r * jax.nn.softplus(-lam.astype(F32))[:, None, None, :]
    a = jnp.exp(log_a)
    u = jnp.sqrt(-jnp.expm1(2.0 * log_a)) * i * xf[None]
    _, h_fwd = lax.associative_scan(linear_scan_combine, (a[0], u[0]), axis=1)
    _, h_bwd = lax.associative_scan(linear_scan_combine, (a[1], u[1]), axis=1, reverse=True)
    return (h_fwd + h_bwd).astype(x_in.dtype) * jax.nn.gelu(gate_in)


def retention_branch(q, k, v, g, cos, sin):
    B, S, _ = q.shape
    H, dk, dv, C = RET_HEADS, RET_QK_DIM, RET_V_DIM, RET_CHUNK
    N = S // C
    q = rope(q.reshape(B, S, H, dk), cos, sin) * (dk ** -0.5)
    k = rope(k.reshape(B, S, H, dk), cos, sin)
    qc = q.reshape(B, N, C, H, dk).astype(F32)
    kc = k.reshape(B, N, C, H, dk).astype(F32)
    vc = v.reshape(B, N, C, H, dv).astype(F32)
    log_gamma = jnp.log1p(-jnp.exp2(-5.0 - jnp.arange(H, dtype=F32)))
    pos = jnp.arange(C, dtype=F32)

    def dec(p):
        return jnp.exp(p[:, None] * log_gamma[None, :])[..., None]

    intra = jnp.exp(jnp.abs(pos[:, None] - pos[None, :])[None] * log_gamma[:, None, None])
    scores = jnp.einsum("bnihd,bnjhd->bnhij", qc, kc) * intra
    o = jnp.einsum("bnhij,bnjhe->bnihe", scores, vc)
    kv_fwd = jnp.einsum("bnjhd,bnjhe->nbhde", kc * dec(C - 1 - pos), vc)
    kv_bwd = jnp.einsum("bnjhd,bnjhe->nbhde", kc * dec(pos), vc)
    chunk_decay = jnp.exp(C * log_gamma)[None, :, None, None]

    def carry_step(state, kv):
        return chunk_decay * state + kv, state

    zero = jnp.zeros((B, H, dk, dv), F32)
    _, st_fwd = lax.scan(carry_step, zero, kv_fwd)
    _, st_bwd = lax.scan(carry_step, zero, kv_bwd, reverse=True)
    o = (o + jnp.einsum("bnihd,nbhde->bnihe", qc * dec(pos + 1.0), st_fwd)
           + jnp.einsum("bnihd,nbhde->bnihe", qc * dec(C - pos), st_bwd))
    o = head_norm(o.reshape(B, S, H, dv)).reshape(B, S, H * dv)
    return jax.nn.silu(g) * o.astype(g.dtype)


def spatial_gating_branch(u, v, norm_g, w_s, b_s):
    B, S, W = u.shape
    N = S // SGU_CHUNK
    u = jax.nn.gelu(u)
    v = head_norm(jax.nn.gelu(v)) * norm_g
    vc = v.reshape(B, N, SGU_CHUNK, SGU_GROUPS, W // SGU_GROUPS)
    mixed = jnp.einsum("gij,bnjgd->bnigd", w_s, vc) + b_s.T[:, :, None]
    return u * mixed.reshape(B, S, W)


def rwkv7_branch(feat, mu_prev, mu_next, w0, w_up, a0, a_up, g_up, k_k, k_a, r_k):
    B, S, _ = feat.shape
    H, hd, W = RWKV_HEADS, RWKV_HEAD_DIM, MIX_WIDTH
    zero = jnp.zeros_like(feat[:, :1])
    prev = jnp.concatenate([zero, feat[:, :-1]], axis=1)
    nxt = jnp.concatenate([feat[:, 1:], zero], axis=1)
    feat = feat + mu_prev * (prev - feat) + mu_next * (nxt - feat)
    r, k, v, wd, ad, gd = jnp.split(
        feat, [W, 2 * W, 3 * W, 3 * W + 2 * DECAY_LORA, 3 * W + 2 * DECAY_LORA + 2 * ICLR_LORA], axis=-1)
    w_raw = jnp.einsum("bszr,zrw->zbsw", jnp.tanh(wd.reshape(B, S, 2, DECAY_LORA)), w_up) + w0[:, None, None, :]
    decay = jnp.exp(-DECAY_SCALE * jax.nn.sigmoid(w_raw.astype(F32)))
    a = jax.nn.sigmoid((jnp.einsum("bszr,zrw->zbsw", ad.reshape(B, S, 2, ICLR_LORA), a_up)
                        + a0[:, None, None, :]).astype(F32))
    g = jax.nn.sigmoid(gd) @ g_up
    kk = (k * k_k).reshape(B, S, H, hd).astype(F32)
    kk = kk * lax.rsqrt(jnp.sum(kk * kk, axis=-1, keepdims=True) + EPS)
    k_t = k.astype(F32)[None] * (1.0 + (a - 1.0) * k_a.astype(F32))
    rf = r.astype(F32).reshape(B, S, H, hd)
    vf = v.astype(F32).reshape(B, S, H, hd)

    def shared(t):
        return jnp.stack([t, jnp.flip(t, 1)])

    def own(t):
        return jnp.stack([t[0], jnp.flip(t[1], 1)]).reshape(2, B, S, H, hd)

    seq_in = (shared(rf), own(decay), shared(kk), own(a), own(k_t), shared(vf))
    seq_in = tuple(jnp.moveaxis(t, 2, 0) for t in seq_in)

    def step(state, inp):
        r_t, w_t, kk_t, a_t, k_tt, v_t = inp
        removal = jnp.einsum("zbhvk,zbhk->zbhv", state, kk_t)
        state = (state * w_t[..., None, :] - removal[..., None] * (a_t * kk_t)[..., None, :]
                 + v_t[..., None] * k_tt[..., None, :])
        return state, jnp.einsum("zbhvk,zbhk->zbhv", state, r_t)

    _, y = lax.scan(step, jnp.zeros((2, B, H, hd, hd), F32), seq_in)
    y = jnp.moveaxis(y, 0, 2)
    y = jnp.stack([y[0], jnp.flip(y[1], 1)])
    bonus = jnp.sum(rf[None] * k_t.reshape(2, B, S, H, hd) * r_k, axis=-1, keepdims=True) * vf[None]
    o = jnp.sum(head_norm(y) + bonus, axis=0).reshape(B, S, W)
    return o.astype(feat.dtype) * g


def hierarchical_moe(h, w_grp, b_grp, w_exp, b_exp, w_gu, w_down):
    B, S, D = h.shape
    T = B * S
    hf = h.reshape(T, D)
    grp_prob = jax.nn.softmax((hf @ w_grp + b_grp).astype(F32), axis=-1)
    grp_p, grp_i = lax.top_k(grp_prob, 1)
    exp_logits = (hf @ w_exp + b_exp).astype(F32).reshape(T, N_GROUPS, EXPERTS_PER_GROUP)
    in_group = exp_logits[jnp.arange(T), grp_i[:, 0]]
    top_p, top_i = lax.top_k(jax.nn.softmax(in_group, axis=-1), TOP_K)
    weights = grp_p * top_p / jnp.sum(top_p, axis=-1, keepdims=True)
    flat_e = (grp_i * EXPERTS_PER_GROUP + top_i).reshape(T * TOP_K)

    A = T * TOP_K
    order = jnp.argsort(flat_e)
    sorted_e = flat_e[order]
    counts = jnp.zeros((N_EXPERTS,), jnp.int32).at[flat_e].add(1)
    padded = (counts + MOE_BLOCK - 1) // MOE_BLOCK * MOE_BLOCK
    pad_end = jnp.cumsum(padded)
    pad_start = pad_end - padded
    start = jnp.cumsum(counts) - counts
    dest_sorted = pad_start[sorted_e] + jnp.arange(A, dtype=jnp.int32) - start[sorted_e]
    n_blocks = -(-A // MOE_BLOCK) + N_EXPERTS
    cap = n_blocks * MOE_BLOCK
    slot_token = jnp.full((cap,), T, jnp.int32).at[dest_sorted].set((order // TOP_K).astype(jnp.int32))
    block_expert = jnp.minimum(
        jnp.searchsorted(pad_end, jnp.arange(n_blocks, dtype=jnp.int32) * MOE_BLOCK, side="right"), N_EXPERTS - 1)
    x_pad = jnp.concatenate([hf, jnp.zeros((1, D), hf.dtype)], axis=0)
    xs = x_pad[slot_token].reshape(n_blocks, MOE_BLOCK, D)

    def expert_block(args):
        xb, e = args
        gate, up = jnp.split(xb @ w_gu[e], 2, axis=-1)
        return (jax.nn.silu(gate) * up) @ w_down[e]

    ys = lax.map(expert_block, (xs, block_expert)).reshape(cap, D)
    dest = jnp.zeros((A,), jnp.int32).at[order].set(dest_sorted)
    y_assign = ys[dest].reshape(T, TOP_K, D)
    return jnp.einsum("tk,tkd->td", weights.astype(hf.dtype), y_assign).reshape(B, S, D)


def setup_inputs(seed: int = 0) -> dict:
    key = jax.random.key(seed)
    ks = iter(jax.random.split(key, 48))
    L, D, W = DEPTH, D_MODEL, MIX_WIDTH

    def nrm(shape, scale):
        return scale * jax.random.normal(next(ks), shape, F32)

    def unif(shape, lo, hi):
        return jax.random.uniform(next(ks), shape, F32, lo, hi)

    lru_p = unif((L, 2, W), 0.9, 0.999) ** (1.0 / LRU_C)
    return {
        "x": nrm((BATCH, SEQ, D), 1.0),
        "c": nrm((BATCH, D), 1.0),
        "positions": jnp.broadcast_to(jnp.arange(SEQ, dtype=jnp.int32), (BATCH, SEQ)),
        "norm1_g": 1.0 + nrm((L, D), 0.02),
        "norm2_g": 1.0 + nrm((L, D), 0.02),
        "ada_w": nrm((L, D, 6 * D), 0.5 * D ** -0.5),
        "ada_b": nrm((L, 6 * D), 0.02),
        "w_in": nrm((L, D, IN_WIDTH), D ** -0.5),
        "lru_conv_w": nrm((L, CONV_WIDTH, W), CONV_WIDTH ** -0.5),
        "lru_conv_b": nrm((L, W), 0.02),
        "lru_w_r": nrm((L, 2, LRU_BLOCKS, LRU_BLOCK, LRU_BLOCK), LRU_BLOCK ** -0.5),
        "lru_b_r": nrm((L, 2, W), 0.1),
        "lru_w_i": nrm((L, 2, LRU_BLOCKS, LRU_BLOCK, LRU_BLOCK), LRU_BLOCK ** -0.5),
        "lru_b_i": nrm((L, 2, W), 0.1),
        "lru_lambda": jnp.log(lru_p) - jnp.log1p(-lru_p),
        "sgu_norm_g": 1.0 + nrm((L, W), 0.02),
        "sgu_w": nrm((L, SGU_GROUPS, SGU_CHUNK, SGU_CHUNK), 0.5 * SGU_CHUNK ** -0.5),
        "sgu_b": 1.0 + nrm((L, SGU_GROUPS, SGU_CHUNK), 0.1),
        "rwkv_mu_prev": unif((L, RWKV_FEAT), 0.0, 0.5),
        "rwkv_mu_next": unif((L, RWKV_FEAT), 0.0, 0.5),
        "rwkv_w0": -1.0 + nrm((L, 2, W), 0.5),
        "rwkv_w_up": nrm((L, 2, DECAY_LORA, W), 0.3 * DECAY_LORA ** -0.5),
        "rwkv_a0": nrm((L, 2, W), 0.5),
        "rwkv_a_up": nrm((L, 2, ICLR_LORA, W), 0.3 * ICLR_LORA ** -0.5),
        "rwkv_g_up": nrm((L, GATE_LORA, W), GATE_LORA ** -0.5),
        "rwkv_k_k": 0.85 + nrm((L, W), 0.05),
        "rwkv_k_a": 1.0 + nrm((L, W), 0.05),
        "rwkv_r_k": nrm((L, RWKV_HEADS, RWKV_HEAD_DIM), 0.1),
        "w_branch": nrm((L, N_BRANCH, W, D), W ** -0.5),
        "w_out": nrm((L, D, D), D ** -0.5),
        "router_grp_w": nrm((L, D, N_GROUPS), D ** -0.5),
        "router_grp_b": nrm((L, N_GROUPS), 0.01),
        "router_exp_w": nrm((L, D, N_EXPERTS), D ** -0.5),
        "router_exp_b": nrm((L, N_EXPERTS), 0.01),
        "expert_w_gu": nrm((L, N_EXPERTS, D, 2 * EXPERT_HIDDEN), D ** -0.5),
        "expert_w_down": nrm((L, N_EXPERTS, EXPERT_HIDDEN, D), EXPERT_HIDDEN ** -0.5),
        "final_norm_g": 1.0 + nrm((D,), 0.02),
    }


def reference(x, c, positions, norm1_g, norm2_g, ada_w, ada_b, w_in,
              lru_conv_w, lru_conv_b, lru_w_r, lru_b_r, lru_w_i, lru_b_i, lru_lambda,
              sgu_norm_g, sgu_w, sgu_b,
              rwkv_mu_prev, rwkv_mu_next, rwkv_w0, rwkv_w_up, rwkv_a0, rwkv_a_up, rwkv_g_up,
              rwkv_k_k, rwkv_k_a, rwkv_r_k,
              w_branch, w_out,
              router_grp_w, router_grp_b, router_exp_w, router_exp_b, expert_w_gu, expert_w_down,
              final_norm_g):
    B, S, D = x.shape
    inv_freq = ROPE_THETA ** (-jnp.arange(0, RET_QK_DIM, 2, dtype=F32) / RET_QK_DIM)
    ang = positions.astype(F32)[..., None] * inv_freq
    cos = jnp.cos(ang)[:, :, None, :].astype(x.dtype)
    sin = jnp.sin(ang)[:, :, None, :].astype(x.dtype)
    cond = jax.nn.silu(c)
    split_points = np.cumsum(IN_SPLITS)[:-1].tolist()
    for l in range(DEPTH):
        mod = cond @ ada_w[l] + ada_b[l]
        sh1, sc1, g1, sh2, sc2, g2 = [m[:, None, :] for m in jnp.split(mod, 6, axis=-1)]

        h = rms_norm(x, norm1_g[l]) * (1.0 + sc1) + sh1
        (lru_x, lru_g, ret_q, ret_k, ret_v, ret_g, sgu_u, sgu_v, rwkv_feat,
         merge_logits) = jnp.split(h @ w_in[l], split_points, axis=-1)
        y_a = rglru_branch(lru_x, lru_g, lru_conv_w[l], lru_conv_b[l], lru_w_r[l], lru_b_r[l],
                           lru_w_i[l], lru_b_i[l], lru_lambda[l])
        y_b = retention_branch(ret_q, ret_k, ret_v, ret_g, cos, sin)
        y_c = spatial_gating_branch(sgu_u, sgu_v, sgu_norm_g[l], sgu_w[l], sgu_b[l])
        y_d = rwkv7_branch(rwkv_feat, rwkv_mu_prev[l], rwkv_mu_next[l], rwkv_w0[l], rwkv_w_up[l],
                           rwkv_a0[l], rwkv_a_up[l], rwkv_g_up[l], rwkv_k_k[l], rwkv_k_a[l], rwkv_r_k[l])
        ys = jnp.stack([y_a, y_b, y_c, y_d], axis=2)
        branches = jnp.einsum("bsnw,nwd->bsnd", ys, w_branch[l])
        gates = jax.nn.sigmoid(merge_logits.reshape(B, S, N_BRANCH, D))
        merged = jnp.einsum("bsnd,bsnd->bsd", gates, branches)
        x = x + g1 * (merged @ w_out[l])

        h = rms_norm(x, norm2_g[l]) * (1.0 + sc2) + sh2
        x = x + g2 * hierarchical_moe(h, router_grp_w[l], router_grp_b[l], router_exp_w[l],
                                      router_exp_b[l], expert_w_gu[l], expert_w_down[l])
    return rms_norm(x, final_norm_g)
```

```python
import contextlib
import numpy as np
import concourse.bass as bass
import concourse.mybir as mybir
from concourse.bass_utils import run_bass_kernel_spmd

F32 = mybir.dt.float32
BF16 = mybir.dt.bfloat16
I32 = mybir.dt.int32
AF = mybir.ActivationFunctionType
ALU = mybir.AluOpType
AX = mybir.AxisListType

N_LAUNCH = [0]


class Prog:
    ENG = ('pe', 'act', 'dve', 'pool', 'sp')
    SAME = {'pe': False, 'act': True, 'dve': True, 'pool': True, 'sp': True}
    K = 6

    def __init__(self):
        self.nc = bass.Bass("TRN2", target_bir_lowering=False)
        self.es = contextlib.ExitStack()
        self.ops = {e: [] for e in self.ENG}
        self.res = {}
        self.seen = {e: {} for e in self.ENG}
        self.ndma = {e: 0 for e in self.ENG}
        self.n_t = 0

    def dram(self, name, shape, dt, kind):
        return self.nc.dram_tensor(name, list(shape), dt, kind=kind).ap()

    def sb(self, shape, dt, name=None):
        self.n_t += 1
        name = name or f"t{self.n_t}"
        return self.es.enter_context(self.nc.sbuf_tensor(name, list(shape), dt))

    def ps(self, shape, dt=F32, name=None):
        self.n_t += 1
        name = name or f"p{self.n_t}"
        return self.es.enter_context(self.nc.psum_tensor(name, list(shape), dt))

    def op(self, eng, fn, r=(), w=(), dma=False):
        deps = {}

        def add(tok):
            if tok is None:
                return
            k, v = tok
            if deps.get(k, -1) < v:
                deps[k] = v
        for key in r:
            st = self.res.get(key)
            if st:
                add(st[0])
        for key in w:
            st = self.res.get(key)
            if st:
                add(st[0])
                for t in st[1]:
                    add(t)
        if dma:
            i = self.ndma[eng]
            self.ndma[eng] += 1
            slot, n = i % self.K, i // self.K
            if n > 0:
                add((('d', eng, slot), n))
            tok = (('d', eng, slot), n + 1)
        else:
            tok = (eng, len(self.ops[eng]))
        waits = []
        for k, v in deps.items():
            if k == eng and not self.SAME[eng]:
                continue
            if self.seen[eng].get(k, -1) >= v:
                continue
            self.seen[eng][k] = v
            waits.append((k, v))
        self.ops[eng].append(dict(fn=fn, waits=waits, tok=tok, dma=dma))
        for key in r:
            self.res.setdefault(key, [None, []])[1].append(tok)
        for key in w:
            self.res[key] = [tok, []]

    def dma(self, q, out, in_, r=(), w=(), **kw):
        self.op(q, lambda e: e.dma_start(out=out, in_=in_, **kw), r=r, w=w, dma=True)

    def build(self):
        nc = self.nc
        es = self.es
        csem = {e: es.enter_context(nc.semaphore(f"s_{e}")) for e in self.ENG}
        dsem = {}
        for e in self.ENG:
            for s in range(min(self.K, self.ndma[e])):
                dsem[('d', e, s)] = es.enter_context(nc.semaphore(f"d_{e}{s}"))
        needs = set()
        for e in self.ENG:
            for o in self.ops[e]:
                for k, v in o['waits']:
                    if not isinstance(k, tuple):
                        needs.add((k, v))
        val = {}
        for e in self.ENG:
            c = 0
            for idx, o in enumerate(self.ops[e]):
                if (e, idx) in needs:
                    c += 1
                    val[(e, idx)] = c
        finals = []
        for e in self.ENG:
            for s in range(min(self.K, self.ndma[e])):
                cnt = (self.ndma[e] - s + self.K - 1) // self.K
                finals.append((('d', e, s), cnt))

        def emit(eobj, eng):
            for idx, o in enumerate(self.ops[eng]):
                for k, v in o['waits']:
                    if isinstance(k, tuple):
                        eobj.wait_ge(dsem[k], 16 * v)
                    else:
                        eobj.wait_ge(csem[k], val[(k, v)])
                ins = o['fn'](eobj)
                if o['dma']:
                    ins.then_inc(dsem[o['tok'][0]], 16)
                elif (eng, idx) in needs:
                    ins.then_inc(csem[eng], 1)
            if eng == 'sp':
                for k, v in finals:
                    eobj.wait_ge(dsem[k], 16 * v)

        with nc.Block() as block:
            @block.tensor
            def _(e):
                emit(e, 'pe')

            @block.scalar
            def _(e):
                emit(e, 'act')

            @block.vector
            def _(e):
                emit(e, 'dve')

            @block.gpsimd
            def _(e):
                emit(e, 'pool')

            @block.sync
            def _(e):
                emit(e, 'sp')
        es.close()
        return nc


def launch(nc, in_maps):
    N_LAUNCH[0] += 1
    res = run_bass_kernel_spmd(nc, in_maps, core_ids=list(range(len(in_maps))))
    return res.results


D = 2048
NCORE = 8


def build_ada():
    P = Prog()
    cT = P.dram("cT", [128, 16, 2], F32, "ExternalInput")
    w = P.dram("w", [2048, 3072], F32, "ExternalInput")
    b = P.dram("b", [1, 3072], F32, "ExternalInput")
    out = P.dram("out", [2, 3072], F32, "ExternalOutput")
    ct = P.sb([128, 16, 2], F32)
    sc = P.sb([128, 16, 2], F32)
    bt = P.sb([2, 3072], F32)
    ot = P.sb([2, 3072], F32)
    wts = [P.sb([128, 3072], F32) for _ in range(3)]
    pss = [P.ps([128, 512]) for _ in range(6)]
    P.dma('sp', ct[:], cT, w=['ct'])
    P.dma('sp', bt[0:1, :], b, w=['bt0'])
    P.dma('sp', bt[1:2, :], b, w=['bt1'])
    P.op('act', lambda e: e.activation(out=sc[:], in_=ct[:], func=AF.Silu), r=['ct'], w=['sc'])
    for kc in range(16):
        wt = wts[kc % 3]
        q = 'sp' if kc % 2 == 0 else 'pool'
        P.dma(q, wt[:], w[kc * 128:(kc + 1) * 128, :], w=[('wt', kc % 3)])
        for n in range(6):
            P.op('pe', lambda e, kc=kc, n=n, wt=wt: e.matmul(
                pss[n][0:2, :], lhsT=sc[:, kc, :], rhs=wt[:, n * 512:(n + 1) * 512],
                start=(kc == 0), stop=(kc == 15)),
                r=['sc', ('wt', kc % 3)], w=[('ps', n)])
    for n in range(6):
        P.op('dve', lambda e, n=n: e.tensor_tensor(
            out=ot[:, n * 512:(n + 1) * 512], in0=pss[n][0:2, :], in1=bt[:, n * 512:(n + 1) * 512], op=ALU.add),
            r=[('ps', n), 'bt0', 'bt1'], w=[('ot', n)])
    P.dma('sp', out, ot[:], r=[('ot', n) for n in range(6)])
    return P.build()


def run_ada(c, ada_w, ada_b):
    nc = cached("ada", build_ada)
    cT = np.ascontiguousarray(c.T.reshape(16, 128, 2).transpose(1, 0, 2))
    wall = ada_w.reshape(2, 2048, 4, 3072)
    ball = ada_b.reshape(2, 4, 3072)
    in_maps = []
    for i in range(NCORE):
        l, j = i // 4, i % 4
        in_maps.append({"cT": cT, "w": np.ascontiguousarray(wall[l, :, j, :]),
                        "b": np.ascontiguousarray(ball[l, j][None, :])})
    res = launch(nc, in_maps)
    mod = np.zeros((2, 2, 12288), np.float32)
    for i in range(NCORE):
        l, j = i // 4, i % 4
        mod[l, :, j * 3072:(j + 1) * 3072] = res[i]["out"]
    return mod


def fm(a):
    r, c = a.shape
    return np.ascontiguousarray(a.reshape(r // 128, 128, c).transpose(1, 0, 2))


def vec_fm(v):
    return np.ascontiguousarray(v.reshape(-1, 128).T)


def emit_norm(P, xt, nk, ntok, gam, sc, sh, ones, ps_banks, rstd, tmp, outs, tag):
    Dn = nk * 128
    gm = P.sb([128, nk], F32)
    if sc is not None:
        P.op('dve', lambda e: e.scalar_tensor_tensor(out=gm[:], in0=sc[:], scalar=1.0, in1=gam[:],
                                                     op0=ALU.add, op1=ALU.mult),
             r=[(tag, 'sc'), (tag, 'gam')], w=[(tag, 'gm')])
    else:
        P.op('dve', lambda e: e.tensor_copy(out=gm[:], in_=gam[:]), r=[(tag, 'gam')], w=[(tag, 'gm')])
    nt = ntok // 512
    for t in range(nt):
        ts = slice(t * 512, (t + 1) * 512)
        bank = ps_banks[t % len(ps_banks)]
        bk = ('psb', id(bank))
        for k in range(nk):
            j = k % 2
            P.op('act', lambda e, k=k, j=j, ts=ts: e.activation(out=tmp[:, j, :], in_=xt[:, k, ts], func=AF.Square),
                 r=[(tag, 'x', k)], w=[(tag, 'tmp', j)])
            P.op('pe', lambda e, k=k, j=j, bank=bank: e.matmul(bank[:, :], lhsT=ones[:], rhs=tmp[:, j, :],
                                                             start=(k == 0), stop=(k == nk - 1)),
                 r=[(tag, 'tmp', j), 'ones'], w=[bk])
        P.op('act', lambda e, ts=ts, bank=bank: e.activation(out=rstd[:, ts], in_=bank[:, :], func=AF.Sqrt,
                                                            scale=1.0 / Dn, bias=epsb[0][:]),
             r=[bk, 'epsb'], w=[(tag, 'rstd', t)])
        P.op('dve', lambda e, ts=ts: e.reciprocal(out=rstd[:, ts], in_=rstd[:, ts]),
             r=[(tag, 'rstd', t)], w=[(tag, 'rstd', t)])
        for k in range(nk):
            j = k % 2
            P.op('dve', lambda e, k=k, j=j, ts=ts: e.tensor_tensor(out=tmp[:, j, :], in0=xt[:, k, ts], in1=rstd[:, ts],
                                                                 op=ALU.mult),
                 r=[(tag, 'x', k), (tag, 'rstd', t)], w=[(tag, 'tmp', j)])
            for i, ot in enumerate(outs):
                if sh is not None:
                    P.op('act', lambda e, k=k, j=j, ts=ts, ot=ot: e.activation(
                        out=ot[:, k, ts], in_=tmp[:, j, :], func=AF.Identity, scale=gm[:, k:k + 1], bias=sh[:, k:k + 1]),
                        r=[(tag, 'tmp', j), (tag, 'gm'), (tag, 'sh')], w=[(tag, 'h', i, t)])
                else:
                    P.op('act', lambda e, k=k, j=j, ts=ts, ot=ot: e.activation(
                        out=ot[:, k, ts], in_=tmp[:, j, :], func=AF.Copy, scale=gm[:, k:k + 1]),
                        r=[(tag, 'tmp', j), (tag, 'gm')], w=[(tag, 'h', i, t)])


epsb = [None]


def mk_consts(P):
    ones = P.sb([128, 128], F32, "ones")
    P.op('pool', lambda e: e.memset(ones[:], 1.0), w=['ones'])
    eb = P.sb([128, 1], F32, "epsb")
    P.op('pool', lambda e: e.memset(eb[:], 1e-6), w=['epsb'])
    epsb[0] = eb
    return ones


def build_H(out_dt=BF16):
    P = Prog()
    xT = P.dram("xT", [128, 16, 1024], F32, "ExternalInput")
    prm = P.dram("prm", [128, 3, 16], F32, "ExternalInput")
    hT = P.dram("hT", [128, 16, 1024], out_dt, "ExternalOutput")
    ones = mk_consts(P)
    xt = P.sb([128, 16, 1024], F32)
    ht = P.sb([128, 16, 1024], out_dt)
    pt = P.sb([128, 3, 16], F32)
    rstd = P.sb([128, 1024], F32)
    tmp = P.sb([128, 2, 512], F32)
    banks = [P.ps([128, 512]) for _ in range(2)]
    P.dma('sp', pt[:], prm, w=[('n', 'gam'), ('n', 'sc'), ('n', 'sh')])
    for k in range(16):
        P.dma('sp' if k % 2 == 0 else 'pool', xt[:, k, :], xT[:, k, :], w=[('n', 'x', k)])
    emit_norm(P, xt, 16, 1024, pt[:, 0, :], pt[:, 1, :], pt[:, 2, :], ones, banks, rstd, tmp, [ht], 'n')
    P.dma('sp', hT, ht[:], r=[('n', 'h', 0, 0), ('n', 'h', 0, 1)])
    return P.build()


def run_H(xT_full, gam, sc, sh, out_dt=BF16):
    nc = cached(('H', str(out_dt)), lambda: build_H(out_dt))
    in_maps = []
    for i in range(NCORE):
        b = i // 4
        prm = np.stack([vec_fm(gam), vec_fm(sc[b]), vec_fm(sh[b])], axis=1)
        in_maps.append({"xT": fm(xT_full[:, i * 1024:(i + 1) * 1024]), "prm": np.ascontiguousarray(prm)})
    res = launch(nc, in_maps)
    hT = np.concatenate([r["hT"].transpose(1, 0, 2).reshape(2048, 1024) for r in res], axis=1)
    return hT


def build_P(nk, ncol, ntok):
    P = Prog()
    hT = P.dram("hT", [128, nk, ntok], BF16, "ExternalInput")
    w = P.dram("w", [128, nk, ncol], F32, "ExternalInput")
    out = P.dram("out", [ncol, ntok], F32, "ExternalOutput")
    wbf = P.sb([128, nk, ncol], BF16)
    wst = [P.sb([128, ncol], F32) for _ in range(2)]
    hts = [P.sb([128, nk, 512], BF16) for _ in range(2)]
    obs = [P.sb([128, 512], F32) for _ in range(4)]
    banks = [P.ps([128, 512]) for _ in range(6)]
    for k in range(nk):
        j = k % 2
        P.dma('sp' if j == 0 else 'pool', wst[j][:], w[:, k, :], w=[('wst', j)])
        eng = 'dve' if j == 0 else 'pool'
        P.op(eng, lambda e, k=k, j=j: e.tensor_copy(out=wbf[:, k, :], in_=wst[j][:]), r=[('wst', j)], w=[('wbf', k)])
    nct = (ncol + 127) // 128
    it = 0
    for t in range(ntok // 512):
        hb = t % 2
        P.dma('sp', hts[hb][:], hT[:, :, t * 512:(t + 1) * 512], w=[('ht', hb)])
        for ct in range(nct):
            c0 = ct * 128
            cw = min(128, ncol - c0)
            bank = banks[it % 6]
            ob = obs[it % 4]
            for k in range(nk):
                P.op('pe', lambda e, k=k, c0=c0, cw=cw, bank=bank, hb=hb: e.matmul(
                    bank[0:cw, :], lhsT=wbf[:, k, c0:c0 + cw], rhs=hts[hb][:, k, :], start=(k == 0), stop=(k == nk - 1)),
                    r=[('wbf', k), ('ht', hb)], w=[('bank', it % 6)])
            if it % 2 == 0:
                P.op('act', lambda e, cw=cw, bank=bank, ob=ob: e.activation(out=ob[0:cw, :], in_=bank[0:cw, :], func=AF.Copy),
                     r=[('bank', it % 6)], w=[('ob', it % 4)])
            else:
                P.op('dve', lambda e, cw=cw, bank=bank, ob=ob: e.tensor_copy(out=ob[0:cw, :], in_=bank[0:cw, :]),
                     r=[('bank', it % 6)], w=[('ob', it % 4)])
            P.dma('pool' if it % 2 == 0 else 'act', out[c0:c0 + cw, t * 512:(t + 1) * 512], ob[0:cw, :], r=[('ob', it % 4)])
            it += 1
    return P.build()


_cache = {}


def cached(key, fn):
    if key not in _cache:
        _cache[key] = fn()
    return _cache[key]


def run_P(hT_bf, W):
    K_, NC_ = W.shape
    ntok = hT_bf.shape[1]
    ncol = NC_ // NCORE
    assert ncol * NCORE == NC_
    nk = K_ // 128
    nc = cached(('P', nk, ncol, ntok), lambda: build_P(nk, ncol, ntok))
    hfm = fm(hT_bf)
    in_maps = [{"hT": hfm, "w": fm(W[:, i * ncol:(i + 1) * ncol])} for i in range(NCORE)]
    res = launch(nc, in_maps)
    return np.concatenate([r["out"] for r in res], axis=0)


S_ = 4096


def build_LRU():
    P = Prog()
    xin = P.dram("x", [128, 2 * S_], F32, "ExternalInput")
    gin = P.dram("g", [128, 2 * S_], F32, "ExternalInput")
    prm = P.dram("prm", [128, 11], F32, "ExternalInput")
    wri = P.dram("wri", [128, 4, 128], F32, "ExternalInput")
    out = P.dram("out", [128, 2 * S_], F32, "ExternalOutput")
    pt = P.sb([128, 11], F32)
    wt = P.sb([128, 4, 128], F32)
    nsp = P.sb([128, 4], F32)
    X, XC, R, I, M, HF, HB, G = [P.sb([128, S_], F32) for _ in range(8)]
    banks = [P.ps([128, 512]) for _ in range(4)]
    P.dma('sp', pt[:], prm, w=['pt'])
    P.dma('sp', wt[:], wri, w=['wt'])
    P.op('act', lambda e: e.activation(out=nsp[:, 0:2], in_=pt[:, 9:11], func=AF.Exp, scale=-1.0), r=['pt'], w=['nsp'])
    P.op('act', lambda e: e.activation(out=nsp[:, 0:2], in_=nsp[:, 0:2], func=AF.Ln, bias=1.0), r=['nsp'], w=['nsp'])
    P.op('dve', lambda e: e.tensor_scalar(out=nsp[:, 2:4], in0=nsp[:, 0:2], scalar1=-16.0, scalar2=None, op0=ALU.mult),
         r=['nsp'], w=['nsp'])
    P.op('dve', lambda e: e.tensor_scalar(out=nsp[:, 0:2], in0=nsp[:, 0:2], scalar1=-8.0, scalar2=None, op0=ALU.mult),
         r=['nsp'], w=['nsp'])
    bi = 0
    for b in range(2):
        bs = slice(b * S_, (b + 1) * S_)
        P.dma('sp', X[:], xin[:, bs], w=['X'])
        P.dma('pool', G[:], gin[:, bs], w=['G'])
        P.op('dve', lambda e: e.tensor_scalar(out=XC[:], in0=X[:], scalar1=pt[:, 2:3], scalar2=pt[:, 4:5],
                                              op0=ALU.mult, op1=ALU.add), r=['X', 'pt'], w=['XC'])
        for j in (0, 1, 3):
            t0 = max(0, 2 - j)
            t1 = min(S_, S_ + 2 - j)
            P.op('dve', lambda e, j=j, t0=t0, t1=t1: e.scalar_tensor_tensor(
                out=XC[:, t0:t1], in0=X[:, t0 + j - 2:t1 + j - 2], scalar=pt[:, j:j + 1], in1=XC[:, t0:t1],
                op0=ALU.mult, op1=ALU.add), r=['X', 'pt', 'XC'], w=['XC'])
        P.op('act', lambda e: e.activation(out=G[:], in_=G[:], func=AF.Gelu_apprx_tanh), r=['G'], w=['G'])
        for z in range(2):
            for t in range(S_ // 512):
                ts = slice(t * 512, (t + 1) * 512)
                for (widx, dst, bcol, nm) in ((z, R, 5 + z, 'R'), (2 + z, I, 7 + z, 'I')):
                    bank = banks[bi % 4]
                    P.op('pe', lambda e, widx=widx, ts=ts, bank=bank: e.matmul(
                        bank[:, :], lhsT=wt[:, widx, :], rhs=XC[:, ts], start=True, stop=True),
                        r=['wt', 'XC'], w=[('bank', bi % 4)])
                    P.op('act', lambda e, dst=dst, ts=ts, bank=bank, bcol=bcol: e.activation(
                        out=dst[:, ts], in_=bank[:, :], func=AF.Sigmoid, bias=pt[:, bcol:bcol + 1]),
                        r=[('bank', bi % 4), 'pt'], w=[nm])
                    bi += 1
            P.op('act', lambda e, z=z: e.activation(out=M[:], in_=R[:], func=AF.Exp, scale=nsp[:, 2 + z:3 + z]),
                 r=['R', 'nsp'], w=['M'])
            P.op('act', lambda e, z=z: e.activation(out=R[:], in_=R[:], func=AF.Exp, scale=nsp[:, z:z + 1]),
                 r=['R', 'nsp'], w=['R'])
            P.op('act', lambda e: e.activation(out=M[:], in_=M[:], func=AF.Sqrt, scale=-1.0, bias=1.0), r=['M'], w=['M'])
            P.op('dve', lambda e: e.tensor_tensor(out=M[:], in0=M[:], in1=I[:], op=ALU.mult), r=['M', 'I'], w=['M'])
            P.op('dve', lambda e: e.tensor_tensor(out=M[:], in0=M[:], in1=XC[:], op=ALU.mult), r=['M', 'XC'], w=['M'])
            if z == 0:
                P.op('dve', lambda e: e.tensor_tensor_scan(out=HF[:], data0=R[:], data1=M[:], initial=0.0,
                                                           op0=ALU.mult, op1=ALU.add), r=['R', 'M'], w=['HF'])
            else:
                P.op('dve', lambda e: e.tensor_tensor_scan(out=HB[:, ::-1], data0=R[:, ::-1], data1=M[:, ::-1], initial=0.0,
                                                           op0=ALU.mult, op1=ALU.add), r=['R', 'M'], w=['HB'])
        P.op('dve', lambda e: e.tensor_tensor(out=HF[:], in0=HF[:], in1=HB[:], op=ALU.add), r=['HF', 'HB'], w=['HF'])
        P.op('dve', lambda e: e.tensor_tensor(out=HF[:], in0=HF[:], in1=G[:], op=ALU.mult), r=['HF', 'G'], w=['HF'])
        P.dma('sp', out[:, bs], HF[:], r=['HF'])
    return P.build()


def run_LRU(pT, inp, l):
    nc = cached('LRU', build_LRU)
    in_maps = []
    for i in range(NCORE):
        cs = slice(i * 128, (i + 1) * 128)
        prm = np.concatenate([inp["lru_conv_w"][l][:, cs].T, inp["lru_conv_b"][l][cs][:, None],
                              inp["lru_b_r"][l][:, cs].T, inp["lru_b_i"][l][:, cs].T, inp["lru_lambda"][l][:, cs].T], axis=1)
        wri = np.stack([inp["lru_w_r"][l][0, i], inp["lru_w_r"][l][1, i], inp["lru_w_i"][l][0, i], inp["lru_w_i"][l][1, i]], axis=1)
        in_maps.append({"x": np.ascontiguousarray(pT[i * 128:(i + 1) * 128]),
                        "g": np.ascontiguousarray(pT[1024 + i * 128:1024 + (i + 1) * 128]),
                        "prm": np.ascontiguousarray(prm.astype(np.float32)), "wri": np.ascontiguousarray(wri)})
    res = launch(nc, in_maps)
    return np.concatenate([r["out"] for r in res], axis=0)


def build_SGU():
    P = Prog()
    uT = P.dram("uT", [128, 8, 1024], F32, "ExternalInput")
    vtm = P.dram("vtm", [128, 8, 1024], F32, "ExternalInput")
    ng = P.dram("ng", [1, 1024], F32, "ExternalInput")
    wsT = P.dram("wsT", [128, 8, 128], F32, "ExternalInput")
    bs = P.dram("bs", [1, 1024], F32, "ExternalInput")
    out = P.dram("out", [128, 8, 1024], F32, "ExternalOutput")
    U = P.sb([128, 8, 1024], F32)
    V = P.sb([128, 8, 1024], F32)
    NG = P.sb([128, 1024], F32)
    BS = P.sb([128, 8, 128], F32)
    WS = P.sb([128, 8, 128], F32)
    SQ = P.sb([128, 1024], F32)
    st = P.sb([128, 8, 8], F32)
    O = P.sb([128, 8, 1024], F32)
    banks = [P.ps([128, 512]) for _ in range(4)]
    mk_consts(P)
    P.dma('sp', U[:], uT, w=['U'])
    P.dma('pool', V[:], vtm, w=[('V', n) for n in range(8)])
    P.dma('sp', NG[:], ng.partition_broadcast(128), w=['NG'])
    P.dma('sp', BS[:].rearrange("p g i -> p (g i)"), bs.partition_broadcast(128), w=['BS'])
    P.dma('sp', WS[:], wsT, w=['WS'])
    P.op('act', lambda e: e.activation(out=U[:].rearrange("p g t -> p (g t)"), in_=U[:].rearrange("p g t -> p (g t)"),
                                       func=AF.Gelu_apprx_tanh), r=['U'], w=['U'])
    bi = 0
    for n in range(8):
        Vn = V[:, n, :]
        s = st[:, n, :]
        P.op('act', lambda e, Vn=Vn, s=s: e.activation(out=Vn, in_=Vn, func=AF.Gelu_apprx_tanh, accum_out=s[:, 0:1]),
             r=[('V', n)], w=[('V', n), ('st', n)])
        P.op('act', lambda e, Vn=Vn, s=s: e.activation(out=SQ[:], in_=Vn, func=AF.Square, accum_out=s[:, 1:2]),
             r=[('V', n), ('st', n)], w=['SQ', ('st', n)])
        P.op('dve', lambda e, s=s: e.tensor_scalar(out=s[:, 2:3], in0=s[:, 0:1], scalar1=1.0 / 1024, scalar2=None, op0=ALU.mult),
             r=[('st', n)], w=[('st', n)])
        P.op('dve', lambda e, s=s: e.tensor_tensor(out=s[:, 3:4], in0=s[:, 2:3], in1=s[:, 2:3], op=ALU.mult),
             r=[('st', n)], w=[('st', n)])
        P.op('dve', lambda e, s=s: e.scalar_tensor_tensor(out=s[:, 4:5], in0=s[:, 1:2], scalar=1.0 / 1024, in1=s[:, 3:4],
                                                         op0=ALU.mult, op1=ALU.subtract), r=[('st', n)], w=[('st', n)])
        P.op('act', lambda e, s=s: e.activation(out=s[:, 5:6], in_=s[:, 4:5], func=AF.Sqrt, bias=epsb[0][:]),
             r=[('st', n), 'epsb'], w=[('st', n)])
        P.op('dve', lambda e, s=s: e.reciprocal(out=s[:, 5:6], in_=s[:, 5:6]), r=[('st', n)], w=[('st', n)])
        P.op('dve', lambda e, Vn=Vn, s=s: e.tensor_scalar(out=Vn, in0=Vn, scalar1=s[:, 2:3], scalar2=s[:, 5:6],
                                                        op0=ALU.subtract, op1=ALU.mult), r=[('V', n), ('st', n)], w=[('V', n)])
        P.op('dve', lambda e, Vn=Vn: e.tensor_tensor(out=Vn, in0=Vn, in1=NG[:], op=ALU.mult), r=[('V', n), 'NG'], w=[('V', n)])
        for gh in range(2):
            bank = banks[bi % 4]
            for gg in range(4):
                g = gh * 4 + gg
                P.op('pe', lambda e, g=g, gg=gg, n=n, bank=bank: e.matmul(
                    bank[:, gg * 128:(gg + 1) * 128], lhsT=V[:, n, g * 128:(g + 1) * 128], rhs=WS[:, g, :],
                    start=True, stop=True), r=[('V', n), 'WS'], w=[('bank', bi % 4, gg)])
            Ov = O[:, gh * 4:(gh + 1) * 4, n * 128:(n + 1) * 128]
            P.op('dve', lambda e, bank=bank, gh=gh, Ov=Ov: e.tensor_tensor(
                out=Ov, in0=bank[:, :].rearrange("p (g i) -> p g i", g=4), in1=BS[:, gh * 4:(gh + 1) * 4, :], op=ALU.add),
                r=[('bank', bi % 4, gg) for gg in range(4)] + ['BS'], w=[('O', n, gh)])
            P.op('pool', lambda e, gh=gh, n=n, Ov=Ov: e.tensor_tensor(
                out=Ov, in0=Ov, in1=U[:, gh * 4:(gh + 1) * 4, n * 128:(n + 1) * 128], op=ALU.mult),
                r=[('O', n, gh), 'U'], w=[('O', n, gh)])
            bi += 1
    P.dma('sp', out, O[:], r=[('O', n, gh) for n in range(8) for gh in range(2)])
    return P.build()


def run_SGU(pT, inp, l):
    nc = cached('SGU', lambda: (mk_dummy(), build_SGU())[1])
    in_maps = []
    wsT = np.ascontiguousarray(inp["sgu_w"][l].transpose(2, 0, 1))
    for i in range(NCORE):
        ts = slice(i * 1024, (i + 1) * 1024)
        uT = pT[5120:6144, ts].reshape(8, 128, 1024).transpose(1, 0, 2)
        vtm = pT[6144:7168, ts].T.reshape(8, 128, 1024).transpose(1, 0, 2)
        in_maps.append({"uT": np.ascontiguousarray(uT), "vtm": np.ascontiguousarray(vtm),
                        "ng": np.ascontiguousarray(inp["sgu_norm_g"][l][None, :]), "wsT": wsT,
                        "bs": np.ascontiguousarray(inp["sgu_b"][l].reshape(1, 1024))})
    res = launch(nc, in_maps)
    return np.concatenate([r["out"].transpose(1, 0, 2).reshape(1024, 1024) for r in res], axis=1)


def mk_dummy():
    pass


TWO_PI = 6.283185


def build_RET():
    P = Prog()
    qk = P.dram("qk", [32, 4, 2 * S_], F32, "ExternalInput")
    vtm = P.dram("vtm", [128, 64, 128], F32, "ExternalInput")
    gtm = P.dram("gtm", [128, 64, 128], F32, "ExternalInput")
    pos = P.dram("pos", [1, 2 * S_], I32, "ExternalInput")
    c_intra = P.dram("c_intra", [128, 128], F32, "ExternalInput")
    c_decq = P.dram("c_decq", [32, 2, 2, 128], F32, "ExternalInput")
    c_vdec = P.dram("c_vdec", [128, 2], F32, "ExternalInput")
    c_misc = P.dram("c_misc", [32, 4], F32, "ExternalInput")
    idd = P.dram("idd", [128, 128], F32, "ExternalInput")
    out = P.dram("out", [128, 64, 128], F32, "ExternalOutput")
    mk_consts(P)
    QK = P.sb([32, 4, S_], F32)
    V = P.sb([128, 32, 128], F32)
    G = P.sb([128, 32, 128], F32)
    ST = P.sb([32, 32, 2, 256], F32)
    INTRA = P.sb([128, 128], F32)
    DECQ = P.sb([32, 2, 2, 128], F32)
    VDEC = P.sb([128, 2], F32)
    MISC = P.sb([32, 4], F32)
    IDN = P.sb([128, 128], F32)
    SEG = 1024
    PI = P.sb([32, SEG], I32)
    T0, T1, T2, SN, CS, TA, TB = [P.sb([32, SEG], F32) for _ in range(7)]
    KTM = [P.sb([128, 64], F32) for _ in range(2)]
    VFB = [P.sb([128, 256], F32) for _ in range(2)]
    SCT = [P.sb([128, 128], F32) for _ in range(2)]
    QFB = [P.sb([32, 2, 2, 128], F32) for _ in range(2)]
    OS = [P.sb([128, 128], F32) for _ in range(2)]
    SQ = P.sb([128, 128], F32)
    stt = P.sb([128, 64, 8], F32)
    bk = [P.ps([128, 512]) for _ in range(6)]
    for (t_, d_, k_) in ((INTRA, c_intra, 'INTRA'), (DECQ, c_decq, 'DECQ'), (VDEC, c_vdec, 'VDEC'), (MISC, c_misc, 'MISC'),
                        (IDN, idd, 'IDN')):
        P.dma('sp', t_[:], d_, w=[k_])
    for b in range(2):
        P.dma('sp', QK[:], qk[:, :, b * S_:(b + 1) * S_], w=[('QK', s) for s in range(4)])
        P.dma('pool', V[:], vtm[:, b * 32:(b + 1) * 32, :], w=['V'])
        P.dma('pool', G[:], gtm[:, b * 32:(b + 1) * 32, :], w=[('G', n) for n in range(32)])
        P.op('act', lambda e: e.activation(out=G[:].rearrange("p n e -> p (n e)"), in_=G[:].rearrange("p n e -> p (n e)"),
                                           func=AF.Silu), r=[('G', n) for n in range(32)], w=[('G', n) for n in range(32)])
        for s in range(S_ // SEG):
            ss = slice(s * SEG, (s + 1) * SEG)
            P.dma('sp', PI[:], pos[:, b * S_ + s * SEG: b * S_ + (s + 1) * SEG].partition_broadcast(32), w=['PI'])
            P.op('dve', lambda e: e.tensor_copy(out=T0[:], in_=PI[:]), r=['PI'], w=['T0'])
            P.op('dve', lambda e: e.tensor_scalar(out=T0[:], in0=T0[:], scalar1=MISC[:, 0:1], scalar2=MISC[:, 1:2],
                                                  op0=ALU.mult, op1=ALU.mult), r=['T0', 'MISC'], w=['T0'])
            for (dst, shift) in ((SN, 0.0), (CS, 0.25)):
                nm = 'SN' if dst is SN else 'CS'
                P.op('dve', lambda e, shift=shift: e.tensor_scalar(out=T1[:], in0=T0[:], scalar1=shift, scalar2=None, op0=ALU.add),
                     r=['T0'], w=['T1'])
                P.op('dve', lambda e: e.tensor_copy(out=PI[:], in_=T1[:]), r=['T1'], w=['PI'])
                P.op('dve', lambda e: e.tensor_copy(out=T2[:], in_=PI[:]), r=['PI'], w=['T2'])
                P.op('dve', lambda e: e.tensor_tensor(out=T1[:], in0=T1[:], in1=T2[:], op=ALU.subtract), r=['T1', 'T2'], w=['T1'])
                P.op('dve', lambda e: e.tensor_scalar(out=T2[:], in0=T1[:], scalar1=0.5, scalar2=None, op0=ALU.is_gt),
                     r=['T1'], w=['T2'])
                P.op('dve', lambda e: e.tensor_tensor(out=T1[:], in0=T1[:], in1=T2[:], op=ALU.subtract), r=['T1', 'T2'], w=['T1'])
                P.op('dve', lambda e: e.tensor_scalar(out=T2[:], in0=T1[:], scalar1=-0.5, scalar2=None, op0=ALU.is_lt),
                     r=['T1'], w=['T2'])
                P.op('dve', lambda e: e.tensor_tensor(out=T1[:], in0=T1[:], in1=T2[:], op=ALU.add), r=['T1', 'T2'], w=['T1'])
                P.op('act', lambda e, dst=dst: e.activation(out=dst[:], in_=T1[:], func=AF.Sin, scale=TWO_PI), r=['T1'], w=[nm])
            for base in (0, 2):
                x1 = QK[:, base, ss]
                x2 = QK[:, base + 1, ss]
                k1, k2 = ('QK', base), ('QK', base + 1)
                eng = 'dve' if base == 0 else 'pool'
                ta, tb = (TA, TB) if base == 0 else (T0, T2)
                kta, ktb = ('TA', 'TB') if base == 0 else ('T0', 'T2')
                P.op(eng, lambda e, x1=x1, ta=ta: e.tensor_tensor(out=ta[:], in0=x1, in1=CS[:], op=ALU.mult), r=[k1, 'CS'], w=[kta])
                P.op(eng, lambda e, x2=x2, tb=tb: e.tensor_tensor(out=tb[:], in0=x2, in1=SN[:], op=ALU.mult), r=[k2, 'SN'], w=[ktb])
                P.op(eng, lambda e, ta=ta, tb=tb: e.tensor_tensor(out=ta[:], in0=ta[:], in1=tb[:], op=ALU.subtract), r=[kta, ktb], w=[kta])
                P.op(eng, lambda e, x1=x1, tb=tb: e.tensor_tensor(out=tb[:], in0=x1, in1=SN[:], op=ALU.mult), r=[k1, 'SN'], w=[ktb])
                P.op(eng, lambda e, x2=x2: e.tensor_tensor(out=x2, in0=x2, in1=CS[:], op=ALU.mult), r=[k2, 'CS'], w=[k2])
                P.op(eng, lambda e, x2=x2, tb=tb: e.tensor_tensor(out=x2, in0=x2, in1=tb[:], op=ALU.add), r=[k2, ktb], w=[k2])
                P.op(eng, lambda e, x1=x1, ta=ta: e.tensor_copy(out=x1, in_=ta[:]), r=[kta], w=[k1])
        P.op('pool', lambda e: e.memset(ST[:, 0, :, 0:128], 0.0), w=[('ST', 0)])
        P.op('pool', lambda e: e.memset(ST[:, 31, :, 128:256], 0.0), w=[('STB', 31)])
        it = 0
        for n in range(32):
            cs = slice(n * 128, (n + 1) * 128)
            bank = bk[it % 2]
            bkk = ('bk', it % 2)
            ktm = KTM[it % 2]
            vfb = VFB[it % 2]
            P.op('pe', lambda e, bank=bank, cs=cs: e.transpose(out=bank[:, 0:32], in_=QK[:, 2, cs], identity=IDN[0:32, 0:32]),
                 r=[('QK', 2), 'IDN'], w=[bkk])
            P.op('pe', lambda e, bank=bank, cs=cs: e.transpose(out=bank[:, 32:64], in_=QK[:, 3, cs], identity=IDN[0:32, 0:32]),
                 r=[('QK', 3), 'IDN'], w=[bkk])
            P.op('act', lambda e, bank=bank, ktm=ktm: e.activation(out=ktm[:], in_=bank[:, 0:64], func=AF.Copy),
                 r=[bkk], w=[('ktm', it % 2)])
            P.op('dve', lambda e, n=n, vfb=vfb: e.tensor_scalar(out=vfb[:, 0:128], in0=V[:, n, :], scalar1=VDEC[:, 0:1], scalar2=None,
                                                              op0=ALU.mult), r=['V', 'VDEC'], w=[('vfb', it % 2)])
            P.op('dve', lambda e, n=n, vfb=vfb: e.tensor_scalar(out=vfb[:, 128:256], in0=V[:, n, :], scalar1=VDEC[:, 1:2], scalar2=None,
                                                              op0=ALU.mult), r=['V', 'VDEC', ('vfb', it % 2)], w=[('vfb', it % 2)])
            b2 = bk[2 + it % 2]
            b2k = ('bk', 2 + it % 2)
            for h in range(2):
                P.op('pe', lambda e, h=h, b2=b2, ktm=ktm, vfb=vfb: e.matmul(
                    b2[0:32, h * 256:(h + 1) * 256], lhsT=ktm[:, h * 32:(h + 1) * 32], rhs=vfb[:], start=True, stop=True),
                    r=[('ktm', it % 2), ('vfb', it % 2)], w=[b2k])
            if n < 31:
                P.op('act', lambda e, n=n, b2=b2: e.activation(
                    out=ST[:, n + 1, :, 0:128], in_=b2[0:32, :].rearrange("p (h x) -> p h x", h=2)[:, :, 0:128], func=AF.Copy),
                    r=[b2k], w=[('ST', n + 1)])
            if n > 0:
                P.op('act', lambda e, n=n, b2=b2: e.activation(
                    out=ST[:, n - 1, :, 128:256], in_=b2[0:32, :].rearrange("p (h x) -> p h x", h=2)[:, :, 128:256], func=AF.Copy),
                    r=[b2k], w=[('STB', n - 1)])
            it += 1
        for n in range(1, 32):
            P.op('dve', lambda e, n=n: e.scalar_tensor_tensor(
                out=ST[:, n, :, 0:128], in0=ST[:, n - 1, :, 0:128], scalar=MISC[:, 2:3], in1=ST[:, n, :, 0:128],
                op0=ALU.mult, op1=ALU.add), r=[('ST', n - 1), ('ST', n), 'MISC'], w=[('ST', n)])
        for n in range(30, -1, -1):
            P.op('dve', lambda e, n=n: e.scalar_tensor_tensor(
                out=ST[:, n, :, 128:256], in0=ST[:, n + 1, :, 128:256], scalar=MISC[:, 2:3], in1=ST[:, n, :, 128:256],
                op0=ALU.mult, op1=ALU.add), r=[('STB', n + 1), ('STB', n), 'MISC'], w=[('STB', n)])
        for n in range(32):
            cs = slice(n * 128, (n + 1) * 128)
            j = n % 2
            bs_, bsk = bk[j], ('bk', j)
            P.op('pe', lambda e, bs_=bs_, cs=cs: e.matmul(bs_[:, 0:128], lhsT=QK[:, 2, cs], rhs=QK[:, 0, cs], start=True, stop=False),
                 r=[('QK', 0), ('QK', 2)], w=[bsk])
            P.op('pe', lambda e, bs_=bs_, cs=cs: e.matmul(bs_[:, 0:128], lhsT=QK[:, 3, cs], rhs=QK[:, 1, cs], start=False, stop=True),
                 r=[('QK', 1), ('QK', 3)], w=[bsk])
            P.op('dve', lambda e, bs_=bs_, j=j: e.tensor_tensor(out=SCT[j][:], in0=bs_[:, 0:128], in1=INTRA[:], op=ALU.mult),
                 r=[bsk, 'INTRA'], w=[('sct', j)])
            for dr in range(2):
                P.op('pool', lambda e, dr=dr, j=j, cs=cs: e.tensor_tensor(out=QFB[j][:, dr, :, :], in0=QK[:, 0:2, cs],
                                                                        in1=DECQ[:, dr, :, :], op=ALU.mult),
                     r=[('QK', 0), ('QK', 1), 'DECQ'], w=[('qfb', j, dr)])
            bo, bok = bk[4 + j], ('bk', 4 + j)
            P.op('pe', lambda e, bo=bo, j=j, n=n: e.matmul(bo[:, 0:128], lhsT=SCT[j][:], rhs=V[:, n, :], start=True, stop=False),
                 r=[('sct', j), 'V'], w=[bok])
            for dr in range(2):
                for h in range(2):
                    last = (dr == 1 and h == 1)
                    P.op('pe', lambda e, bo=bo, j=j, n=n, dr=dr, h=h, last=last: e.matmul(
                        bo[:, 0:128], lhsT=QFB[j][:, dr, h, :], rhs=ST[:, n, h, dr * 128:(dr + 1) * 128], start=False, stop=last),
                        r=[('qfb', j, dr), ('ST', n), ('STB', n)], w=[bok])
            s = stt[:, b * 32 + n, :]
            sk = ('stt', b * 32 + n)
            P.op('act', lambda e, bo=bo, j=j, s=s: e.activation(out=OS[j][:], in_=bo[:, 0:128], func=AF.Copy, accum_out=s[:, 0:1]),
                 r=[bok], w=[('os', j), sk])
            P.op('act', lambda e, j=j, s=s: e.activation(out=SQ[:], in_=OS[j][:], func=AF.Square, accum_out=s[:, 1:2]),
                 r=[('os', j), sk], w=['SQ', sk])
            P.op('dve', lambda e, s=s: e.tensor_scalar(out=s[:, 2:3], in0=s[:, 0:1], scalar1=1.0 / 128, scalar2=None, op0=ALU.mult),
                 r=[sk], w=[sk])
            P.op('dve', lambda e, s=s: e.tensor_tensor(out=s[:, 3:4], in0=s[:, 2:3], in1=s[:, 2:3], op=ALU.mult), r=[sk], w=[sk])
            P.op('dve', lambda e, s=s: e.scalar_tensor_tensor(out=s[:, 4:5], in0=s[:, 1:2], scalar=1.0 / 128, in1=s[:, 3:4],
                                                             op0=ALU.mult, op1=ALU.subtract), r=[sk], w=[sk])
            P.op('act', lambda e, s=s: e.activation(out=s[:, 5:6], in_=s[:, 4:5], func=AF.Sqrt, bias=epsb[0][:]),
                 r=[sk, 'epsb'], w=[sk])
            P.op('dve', lambda e, s=s: e.reciprocal(out=s[:, 5:6], in_=s[:, 5:6]), r=[sk], w=[sk])
            P.op('dve', lambda e, j=j, s=s: e.tensor_scalar(out=OS[j][:], in0=OS[j][:], scalar1=s[:, 2:3], scalar2=s[:, 5:6],
                                                          op0=ALU.subtract, op1=ALU.mult), r=[('os', j), sk], w=[('os', j)])
            P.op('dve', lambda e, j=j, n=n: e.tensor_tensor(out=G[:, n, :], in0=G[:, n, :], in1=OS[j][:], op=ALU.mult),
                 r=[('os', j), ('G', n)], w=[('G', n)])
        P.dma('sp', out[:, b * 32:(b + 1) * 32, :], G[:], r=[('G', n) for n in range(32)])
    return P.build()


def ret_consts(h):
    C = 128
    lg = np.log1p(-np.exp2(-5.0 - h))
    i = np.arange(C)
    intra = 0.125 * np.exp(np.abs(i[:, None] - i[None, :]) * lg)
    decq = np.zeros((32, 2, 2, C), np.float64)
    decq[:, 0, :, :] = 0.125 * np.exp((i + 1.0) * lg)
    decq[:, 1, :, :] = 0.125 * np.exp((C - i) * lg)
    vdec = np.stack([np.exp((C - 1 - i) * lg), np.exp(i * lg)], axis=1)
    inv_freq = (10000.0 ** (-np.arange(0, 64, 2, dtype=np.float32) / 64)).astype(np.float32)
    misc = np.zeros((32, 4), np.float32)
    misc[:, 0] = inv_freq
    misc[:, 1] = 1.0 / (2 * np.pi)
    misc[:, 2] = np.exp(C * lg)
    return (intra.astype(np.float32), decq.astype(np.float32), vdec.astype(np.float32), misc)


def tm_chunks(aT):
    return np.ascontiguousarray(aT.T.reshape(64, 128, aT.shape[0]).transpose(1, 0, 2))


def run_RET(pT, positions):
    nc = cached('RET', build_RET)
    in_maps = []
    idd = np.eye(128, dtype=np.float32)
    posr = np.ascontiguousarray(positions.reshape(1, -1).astype(np.int32))
    for i in range(NCORE):
        q = pT[2048 + i * 64:2048 + (i + 1) * 64]
        k = pT[2560 + i * 64:2560 + (i + 1) * 64]
        qk = np.stack([q[0:32], q[32:64], k[0:32], k[32:64]], axis=1)
        intra, decq, vdec, misc = ret_consts(i)
        in_maps.append({"qk": np.ascontiguousarray(qk), "vtm": tm_chunks(pT[3072 + i * 128:3072 + (i + 1) * 128]),
                        "gtm": tm_chunks(pT[4096 + i * 128:4096 + (i + 1) * 128]), "pos": posr,
                        "c_intra": intra, "c_decq": decq, "c_vdec": vdec, "c_misc": misc, "idd": idd})
    res = launch(nc, in_maps)
    return np.concatenate([r["out"].transpose(2, 1, 0).reshape(128, 8192) for r in res], axis=0)


def build_M():
    P = Prog()
    ysT = P.dram("ysT", [128, 4, 8, 1024], F32, "ExternalInput")
    lgT = P.dram("lgT", [128, 16, 4, 1024], F32, "ExternalInput")
    xT = P.dram("xT", [128, 16, 1024], F32, "ExternalInput")
    g1 = P.dram("g1", [128, 16], F32, "ExternalInput")
    wb = P.dram("wb", [128, 4, 8, 2048], F32, "ExternalInput")
    wo = P.dram("wo", [128, 16, 2048], F32, "ExternalInput")
    out = P.dram("out", [128, 16, 1024], F32, "ExternalOutput")
    X = P.sb([128, 16, 512], F32)
    YS = P.sb([128, 4, 8, 512], BF16)
    YST = [P.sb([128, 8, 512], F32) for _ in range(2)]
    MG = P.sb([128, 16, 512], BF16)
    G1 = P.sb([128, 16], F32)
    WBS = [P.sb([128, 4, 8, 128], F32) for _ in range(2)]
    WBB = [P.sb([128, 4, 8, 128], BF16) for _ in range(2)]
    LG = [P.sb([128, 4, 512], F32) for _ in range(2)]
    ACC = P.sb([128, 512], F32)
    TMP = P.sb([128, 512], F32)
    WOS = [P.sb([128, 16, 128], F32) for _ in range(2)]
    WOB = [P.sb([128, 16, 128], BF16) for _ in range(2)]
    banks = [P.ps([128, 512]) for _ in range(4)]
    P.dma('sp', G1[:], g1, w=['G1'])
    bi = 0
    for hf in range(2):
        hs = slice(hf * 512, (hf + 1) * 512)
        P.dma('sp', X[:], xT[:, :, hs], w=['X'])
        for n in range(4):
            P.dma('pool', YST[n % 2][:], ysT[:, n, :, hs], w=[('yst', n % 2)])
            P.op('dve' if n % 2 == 0 else 'pool', lambda e, n=n: e.tensor_copy(out=YS[:, n, :, :], in_=YST[n % 2][:]),
                 r=[('yst', n % 2)], w=[('YS', n)])
        for dt in range(16):
            j = dt % 2
            P.dma('sp', WBS[j][:], wb[:, :, :, dt * 128:(dt + 1) * 128], w=[('wbs', j)])
            P.op('pool', lambda e, j=j: e.tensor_copy(out=WBB[j][:].rearrange("p n c d -> p (n c d)"),
                                                      in_=WBS[j][:].rearrange("p n c d -> p (n c d)")),
                 r=[('wbs', j)], w=[('wbb', j)])
            P.dma('act', LG[j][:], lgT[:, dt, :, hs], w=[('lg', j)])
            P.op('act', lambda e, j=j: e.activation(out=LG[j][:].rearrange("p n t -> p (n t)"),
                                                    in_=LG[j][:].rearrange("p n t -> p (n t)"), func=AF.Sigmoid),
                 r=[('lg', j)], w=[('lg', j)])
            for n in range(4):
                bank = banks[bi % 4]
                bkk = ('bank', bi % 4)
                for c in range(8):
                    P.op('pe', lambda e, n=n, c=c, j=j, bank=bank: e.matmul(
                        bank[:, :], lhsT=WBB[j][:, n, c, :], rhs=YS[:, n, c, :], start=(c == 0), stop=(c == 7)),
                        r=[('wbb', j), ('YS', n)], w=[bkk])
                if n == 0:
                    P.op('dve', lambda e, j=j, bank=bank: e.tensor_tensor(out=ACC[:], in0=bank[:, :], in1=LG[j][:, 0, :], op=ALU.mult),
                         r=[bkk, ('lg', j)], w=['ACC'])
                else:
                    P.op('dve', lambda e, j=j, n=n, bank=bank: e.tensor_tensor(out=TMP[:], in0=bank[:, :], in1=LG[j][:, n, :],
                                                                             op=ALU.mult), r=[bkk, ('lg', j)], w=['TMP'])
                    if n < 3:
                        P.op('dve', lambda e: e.tensor_tensor(out=ACC[:], in0=ACC[:], in1=TMP[:], op=ALU.add),
                             r=['ACC', 'TMP'], w=['ACC'])
                    else:
                        P.op('dve', lambda e, dt=dt: e.tensor_tensor(out=MG[:, dt, :], in0=ACC[:], in1=TMP[:], op=ALU.add),
                             r=['ACC', 'TMP'], w=[('MG', dt)])
                bi += 1
        for dp in range(16):
            j = dp % 2
            P.dma('sp', WOS[j][:], wo[:, :, dp * 128:(dp + 1) * 128], w=[('wos', j)])
            P.op('pool', lambda e, j=j: e.tensor_copy(out=WOB[j][:].rearrange("p c d -> p (c d)"),
                                                      in_=WOS[j][:].rearrange("p c d -> p (c d)")),
                 r=[('wos', j)], w=[('wob', j)])
            bank = banks[bi % 4]
            bkk = ('bank', bi % 4)
            for c in range(16):
                P.op('pe', lambda e, c=c, j=j, bank=bank: e.matmul(bank[:, :], lhsT=WOB[j][:, c, :], rhs=MG[:, c, :],
                                                                 start=(c == 0), stop=(c == 15)),
                     r=[('wob', j), ('MG', c)], w=[bkk])
            P.op('dve', lambda e, dp=dp, bank=bank: e.scalar_tensor_tensor(
                out=X[:, dp, :], in0=bank[:, :], scalar=G1[:, dp:dp + 1], in1=X[:, dp, :], op0=ALU.mult, op1=ALU.add),
                r=[bkk, 'G1', 'X'], w=[('XO', dp)])
            bi += 1
        P.dma('sp', out[:, :, hs], X[:], r=[('XO', dp) for dp in range(16)] + ['X'], w=['X'])
    return P.build()


def run_M(ysT4, pT, xT_full, g1, w_branch, w_out):
    nc = cached('M', build_M)
    wb = np.ascontiguousarray(w_branch.reshape(4, 8, 128, 2048).transpose(2, 0, 1, 3))
    wo = fm(w_out)
    in_maps = []
    for i in range(NCORE):
        ts = slice(i * 1024, (i + 1) * 1024)
        ys = ysT4[:, :, ts].reshape(4, 8, 128, 1024).transpose(2, 0, 1, 3)
        lg = pT[10624:18816, ts].reshape(4, 16, 128, 1024).transpose(2, 1, 0, 3)
        in_maps.append({"ysT": np.ascontiguousarray(ys), "lgT": np.ascontiguousarray(lg), "xT": fm(xT_full[:, ts]),
                        "g1": vec_fm(g1[i // 4]), "wb": wb, "wo": wo})
    res = launch(nc, in_maps)
    return np.concatenate([r["out"].transpose(1, 0, 2).reshape(2048, 1024) for r in res], axis=1)


def build_E():
    P = Prog()
    xT = P.dram("xT", [128, 16, 1024], F32, "ExternalInput")
    prm = P.dram("prm", [128, 4, 16], F32, "ExternalInput")
    wr = P.dram("wr", [128, 16, 36], F32, "ExternalInput")
    rb = P.dram("rb", [1, 36], F32, "ExternalInput")
    idd = P.dram("idd", [128, 128], F32, "ExternalInput")
    wgu = P.dram("wgu", [32, 128, 16, 1024], F32, "ExternalInput")
    wdn = P.dram("wdn", [32, 128, 4, 2048], F32, "ExternalInput")
    wscr = P.dram("wscr", [32, 1024], F32, "ExternalOutput")
    out = P.dram("out", [128, 16, 1024], F32, "ExternalOutput")
    ones = mk_consts(P)
    XA = P.sb([128, 16, 1024], F32)
    H2B = P.sb([128, 16, 1024], BF16)
    H2F = P.sb([128, 16 * 512], F32)
    H2Fv = H2F[:].rearrange("p (k t) -> p k t", k=16)
    RSTD = P.sb([128, 1024], F32)
    TMP = P.sb([128, 2, 512], F32)
    PT = P.sb([128, 4, 16], F32)
    WR = P.sb([128, 16, 36], F32)
    RB = P.sb([128, 36], F32)
    IDN = P.sb([128, 128], F32)
    L = P.sb([128, 8, 36], F32)
    sm = P.sb([128, 8, 16], F32)
    OH = P.sb([128, 4], F32)
    GE = P.sb([128, 4], F32)
    IG = P.sb([128, 8], F32)
    IG2 = P.sb([128, 8], F32)
    M1 = P.sb([128, 8], F32)
    M2 = P.sb([128, 8], F32)
    WM = P.sb([128, 8], F32)
    WF = P.sb([128, 8, 32], F32)
    WGT = P.sb([32, 1024], F32)
    WB = [P.sb([128, 1024], F32) for _ in range(2)]
    GUB = [[P.sb([128, 16, 128], BF16) for _ in range(2)] for _ in range(2)]
    TMPA = [P.sb([128, 512], F32) for _ in range(2)]
    ACTB = P.sb([128, 4, 1024], BF16)
    WDB = [P.sb([128, 4, 512], BF16) for _ in range(2)]
    GS = [H2F[:, 0:2048].rearrange("p (k j) -> p k j", k=16), H2F[:, 2048:4096].rearrange("p (k j) -> p k j", k=16)]
    WDS = H2F[:, 4096:6144].rearrange("p (c d) -> p c d", c=4)
    XR = H2F[:, 6144:7168]
    banks = [P.ps([128, 512]) for _ in range(8)]
    P.dma('sp', PT[:], prm, w=[('n', 'gam'), ('n', 'sc'), ('n', 'sh'), 'G2'])
    P.dma('sp', WR[:], wr, w=['WR'])
    P.dma('sp', RB[:], rb.partition_broadcast(128), w=['RB'])
    P.dma('sp', IDN[:], idd, w=['IDN'])
    for k in range(16):
        P.dma('sp' if k % 2 == 0 else 'pool', XA[:, k, :], xT[:, k, :], w=[('n', 'x', k)])
    h2f_keys = []

    def after_tile(t):
        for tt in range(4):
            bank = banks[2 + (tt % 2)]
            bkk = ('bk', 2 + (tt % 2))
            for k in range(16):
                P.op('pe', lambda e, k=k, tt=tt, bank=bank: e.matmul(
                    bank[:, 0:36], lhsT=H2Fv[:, k, tt * 128:(tt + 1) * 128], rhs=WR[:, k, :], start=(k == 0), stop=(k == 15)),
                    r=[('n', 'h', 1, t), 'WR'], w=[bkk])
            P.op('dve', lambda e, tt=tt, bank=bank, t=t: e.tensor_tensor(out=L[:, t * 4 + tt, :], in0=bank[:, 0:36], in1=RB[:],
                                                                       op=ALU.add), r=[bkk, 'RB'], w=[('L', t * 4 + tt)])
    emit_norm_cb(P, XA, 16, 1024, PT[:, 0, :], PT[:, 1, :], PT[:, 2, :], ones, banks[0:2], RSTD, TMP,
                 [(H2B, False), (H2Fv, True)], 'n', after_tile)
    for tt in range(8):
        s = sm[:, tt, :]
        sk = ('sm', tt)
        lg = L[:, tt, 0:4]
        Lk = ('L', tt)
        P.op('dve', lambda e, lg=lg, s=s: e.tensor_reduce(out=s[:, 0:1], in_=lg, axis=AX.X, op=ALU.max), r=[Lk], w=[sk])
        P.op('dve', lambda e, lg=lg, s=s: e.tensor_scalar(out=OH[:], in0=lg, scalar1=s[:, 0:1], scalar2=None, op0=ALU.is_ge),
             r=[Lk, sk], w=['OH'])
        P.op('dve', lambda e, s=s: e.tensor_scalar(out=s[:, 1:2], in0=s[:, 0:1], scalar1=-1.0, scalar2=None, op0=ALU.mult),
             r=[sk], w=[sk])
        P.op('act', lambda e, lg=lg, s=s: e.activation(out=GE[:], in_=lg, func=AF.Exp, bias=s[:, 1:2], accum_out=s[:, 2:3]),
             r=[Lk, sk], w=['GE', sk])
        P.op('dve', lambda e, s=s: e.reciprocal(out=s[:, 3:4], in_=s[:, 2:3]), r=[sk], w=[sk])
        for g in range(4):
            le = L[:, tt, 4 + g * 8:4 + (g + 1) * 8]
            if g == 0:
                P.op('dve', lambda e, le=le: e.tensor_scalar(out=IG[:], in0=le, scalar1=OH[:, 0:1], scalar2=None, op0=ALU.mult),
                     r=[Lk, 'OH'], w=['IG'])
            else:
                P.op('dve', lambda e, le=le, g=g: e.scalar_tensor_tensor(out=IG[:], in0=le, scalar=OH[:, g:g + 1], in1=IG[:],
                                                                       op0=ALU.mult, op1=ALU.add), r=[Lk, 'OH', 'IG'], w=['IG'])
        P.op('dve', lambda e, s=s: e.tensor_reduce(out=s[:, 4:5], in_=IG[:], axis=AX.X, op=ALU.max), r=['IG', sk], w=[sk])
        P.op('dve', lambda e, s=s: e.tensor_scalar(out=M1[:], in0=IG[:], scalar1=s[:, 4:5], scalar2=None, op0=ALU.is_ge),
             r=['IG', sk], w=['M1'])
        P.op('dve', lambda e: e.scalar_tensor_tensor(out=IG2[:], in0=M1[:], scalar=-1e30, in1=IG[:], op0=ALU.mult, op1=ALU.add),
             r=['M1', 'IG'], w=['IG2'])
        P.op('dve', lambda e, s=s: e.tensor_reduce(out=s[:, 5:6], in_=IG2[:], axis=AX.X, op=ALU.max), r=['IG2', sk], w=[sk])
        P.op('dve', lambda e, s=s: e.tensor_scalar(out=M2[:], in0=IG2[:], scalar1=s[:, 5:6], scalar2=None, op0=ALU.is_ge),
             r=['IG2', sk], w=['M2'])
        P.op('dve', lambda e, s=s: e.tensor_tensor(out=s[:, 6:7], in0=s[:, 5:6], in1=s[:, 4:5], op=ALU.subtract), r=[sk], w=[sk])
        P.op('act', lambda e, s=s: e.activation(out=s[:, 7:8], in_=s[:, 6:7], func=AF.Exp), r=[sk], w=[sk])
        P.op('dve', lambda e, s=s: e.tensor_scalar(out=s[:, 8:9], in0=s[:, 7:8], scalar1=1.0, scalar2=None, op0=ALU.add),
             r=[sk], w=[sk])
        P.op('dve', lambda e, s=s: e.reciprocal(out=s[:, 8:9], in_=s[:, 8:9]), r=[sk], w=[sk])
        P.op('dve', lambda e, s=s: e.tensor_tensor(out=s[:, 9:10], in0=s[:, 8:9], in1=s[:, 3:4], op=ALU.mult), r=[sk], w=[sk])
        P.op('dve', lambda e, s=s: e.tensor_tensor(out=s[:, 10:11], in0=s[:, 9:10], in1=s[:, 7:8], op=ALU.mult), r=[sk], w=[sk])
        P.op('dve', lambda e, s=s: e.tensor_scalar(out=WM[:], in0=M1[:], scalar1=s[:, 9:10], scalar2=None, op0=ALU.mult),
             r=['M1', sk], w=['WM'])
        P.op('dve', lambda e, s=s: e.scalar_tensor_tensor(out=WM[:], in0=M2[:], scalar=s[:, 10:11], in1=WM[:],
                                                         op0=ALU.mult, op1=ALU.add), r=['M2', sk, 'WM'], w=['WM'])
        for g in range(4):
            P.op('dve', lambda e, g=g, tt=tt: e.tensor_scalar(out=WF[:, tt, g * 8:(g + 1) * 8], in0=WM[:], scalar1=OH[:, g:g + 1],
                                                            scalar2=None, op0=ALU.mult), r=['WM', 'OH'], w=[('WF', tt)])
        bank = banks[4 + tt // 4]
        P.op('pe', lambda e, tt=tt, bank=bank: e.transpose(out=bank[0:32, (tt % 4) * 128:(tt % 4 + 1) * 128], in_=WF[:, tt, :],
                                                         identity=IDN[:]), r=[('WF', tt), 'IDN'], w=[('bk', 4 + tt // 4)])
    for hh in range(2):
        P.op('act', lambda e, hh=hh: e.activation(out=WGT[:, hh * 512:(hh + 1) * 512], in_=banks[4 + hh][0:32, :], func=AF.Copy),
             r=[('bk', 4 + hh)], w=['WGT'])
    P.dma('sp', wscr, WGT[:], r=['WGT'], w=['wscr'])
    h2f_all = [('n', 'h', 1, t) for t in range(2)]
    bi = 0
    ld = 0
    for ex in range(32):
        wb = WB[ex % 2]
        wbk = ('WB', ex % 2)
        P.dma('pool', wb[:], wscr[ex:ex + 1, :].partition_broadcast(128), r=['wscr'], w=[wbk])
        for jt in range(4):
            gb = GUB[ld % 2]
            for gu in range(2):
                c0 = gu * 512 + jt * 128
                P.dma('sp' if gu == 0 else 'act', GS[gu], wgu[ex, :, :, c0:c0 + 128], w=[('GS', gu)] + (h2f_all if ex == 0 and jt == 0 else []))
                P.op('pool' if gu == 0 else 'dve', lambda e, gu=gu, gb=gb: e.tensor_copy(out=gb[gu][:], in_=GS[gu]),
                     r=[('GS', gu)], w=[('GUB', ld % 2, gu)])
            for t2 in range(2):
                ts = slice(t2 * 512, (t2 + 1) * 512)
                bg, bu = banks[(bi * 2) % 6], banks[(bi * 2 + 1) % 6]
                kg, ku = ('bk', (bi * 2) % 6), ('bk', (bi * 2 + 1) % 6)
                for gu, (bank, bkk) in enumerate(((bg, kg), (bu, ku))):
                    for k in range(16):
                        P.op('pe', lambda e, k=k, gu=gu, gb=gb, bank=bank, ts=ts: e.matmul(
                            bank[:, :], lhsT=gb[gu][:, k, :], rhs=H2B[:, k, ts], start=(k == 0), stop=(k == 15)),
                            r=[('GUB', ld % 2, gu), ('n', 'h', 0, t2)], w=[bkk])
                ta = TMPA[bi % 2]
                tak = ('TMPA', bi % 2)
                P.op('act', lambda e, ta=ta, bg=bg: e.activation(out=ta[:], in_=bg[:, :], func=AF.Silu), r=[kg], w=[tak])
                P.op('dve', lambda e, ta=ta, bu=bu: e.tensor_tensor(out=ta[:], in0=bu[:, :], in1=ta[:], op=ALU.mult), r=[ku, tak], w=[tak])
                P.op('dve', lambda e, ta=ta, wb=wb, jt=jt, ts=ts: e.tensor_tensor(out=ACTB[:, jt, ts], in0=ta[:], in1=wb[:, ts], op=ALU.mult),
                     r=[tak, wbk], w=[('ACTB', jt, t2)])
                bi += 1
            ld += 1
        for dq in range(4):
            wd = WDB[dq % 2]
            wdk = ('WDB', dq % 2)
            P.dma('sp', WDS, wdn[ex, :, :, dq * 512:(dq + 1) * 512], w=['WDS'] + (h2f_all if ex == 0 and dq == 0 else []))
            P.op('pool', lambda e, wd=wd: e.tensor_copy(out=wd[:], in_=WDS), r=['WDS'], w=[wdk])
            for dt in range(4):
                d = dq * 4 + dt
                for t2 in range(2):
                    ts = slice(t2 * 512, (t2 + 1) * 512)
                    bank = banks[6 + (bi % 2)]
                    bkk = ('bk', 6 + (bi % 2))
                    for c in range(4):
                        P.op('pe', lambda e, c=c, wd=wd, dt=dt, bank=bank, ts=ts: e.matmul(
                            bank[:, :], lhsT=wd[:, c, dt * 128:(dt + 1) * 128], rhs=ACTB[:, c, ts], start=(c == 0), stop=(c == 3)),
                            r=[wdk, ('ACTB', c, t2)], w=[bkk])
                    if ex == 0:
                        P.op('act', lambda e, d=d, ts=ts, bank=bank: e.activation(out=XA[:, d, ts], in_=bank[:, :], func=AF.Copy),
                             r=[bkk, ('n', 'x', d)], w=[('ACC', d, t2)])
                    else:
                        P.op('dve', lambda e, d=d, ts=ts, bank=bank: e.tensor_tensor(out=XA[:, d, ts], in0=bank[:, :], in1=XA[:, d, ts],
                                                                                   op=ALU.add), r=[bkk, ('ACC', d, t2)], w=[('ACC', d, t2)])
                    bi += 1
    for d in range(16):
        P.dma('sp', XR, xT[:, d, :], w=['XR'] + (['WDS', ('GS', 0), ('GS', 1)] if d == 0 else []))
        P.op('dve', lambda e, d=d: e.scalar_tensor_tensor(out=XA[:, d, :], in0=XA[:, d, :], scalar=PT[:, 3, d:d + 1], in1=XR,
                                                        op0=ALU.mult, op1=ALU.add),
             r=[('ACC', d, 0), ('ACC', d, 1), 'XR', 'G2'], w=[('ACC', d, 0), ('ACC', d, 1)])
        P.dma('pool', out[:, d, :], XA[:, d, :], r=[('ACC', d, 0), ('ACC', d, 1)])
    return P.build()


def emit_norm_cb(P, xt, nk, ntok, gam, sc, sh, ones, ps_banks, rstd, tmp, outs, tag, cb):
    Dn = nk * 128
    gm = P.sb([128, nk], F32)
    P.op('dve', lambda e: e.scalar_tensor_tensor(out=gm[:], in0=sc, scalar=1.0, in1=gam, op0=ALU.add, op1=ALU.mult),
         r=[(tag, 'sc'), (tag, 'gam')], w=[(tag, 'gm')])
    for t in range(ntok // 512):
        ts = slice(t * 512, (t + 1) * 512)
        bank = ps_banks[t % len(ps_banks)]
        bk = ('psb', id(bank))
        for k in range(nk):
            j = k % 2
            P.op('act', lambda e, k=k, j=j, ts=ts: e.activation(out=tmp[:, j, :], in_=xt[:, k, ts], func=AF.Square),
                 r=[(tag, 'x', k)], w=[(tag, 'tmp', j)])
            P.op('pe', lambda e, k=k, j=j, bank=bank: e.matmul(bank[:, :], lhsT=ones[:], rhs=tmp[:, j, :],
                                                             start=(k == 0), stop=(k == nk - 1)),
                 r=[(tag, 'tmp', j), 'ones'], w=[bk])
        P.op('act', lambda e, ts=ts, bank=bank: e.activation(out=rstd[:, ts], in_=bank[:, :], func=AF.Sqrt,
                                                            scale=1.0 / Dn, bias=epsb[0][:]),
             r=[bk, 'epsb'], w=[(tag, 'rstd', t)])
        P.op('dve', lambda e, ts=ts: e.reciprocal(out=rstd[:, ts], in_=rstd[:, ts]),
             r=[(tag, 'rstd', t)], w=[(tag, 'rstd', t)])
        for k in range(nk):
            j = k % 2
            P.op('dve', lambda e, k=k, j=j, ts=ts: e.tensor_tensor(out=tmp[:, j, :], in0=xt[:, k, ts], in1=rstd[:, ts],
                                                                 op=ALU.mult),
                 r=[(tag, 'x', k), (tag, 'rstd', t)], w=[(tag, 'tmp', j)])
            for i, (ot, local) in enumerate(outs):
                osl = slice(0, 512) if local else ts
                P.op('act', lambda e, k=k, j=j, osl=osl, ot=ot: e.activation(
                    out=ot[:, k, osl], in_=tmp[:, j, :], func=AF.Identity, scale=gm[:, k:k + 1], bias=sh[:, k:k + 1]),
                    r=[(tag, 'tmp', j), (tag, 'gm'), (tag, 'sh')], w=[(tag, 'h', i, t)])
        cb(t)


def run_E(x1T, gam, sc2, sh2, g2, inp, l):
    nc = cached('E', build_E)
    wr = fm(np.concatenate([inp["router_grp_w"][l], inp["router_exp_w"][l]], axis=1))
    rb = np.concatenate([inp["router_grp_b"][l], inp["router_exp_b"][l]])[None, :].astype(np.float32)
    wgu = np.ascontiguousarray(inp["expert_w_gu"][l].reshape(32, 16, 128, 1024).transpose(0, 2, 1, 3))
    wdn = np.ascontiguousarray(inp["expert_w_down"][l].reshape(32, 4, 128, 2048).transpose(0, 2, 1, 3))
    idd = np.eye(128, dtype=np.float32)
    in_maps = []
    for i in range(NCORE):
        b = i // 4
        prm = np.stack([vec_fm(gam), vec_fm(sc2[b]), vec_fm(sh2[b]), vec_fm(g2[b])], axis=1)
        in_maps.append({"xT": fm(x1T[:, i * 1024:(i + 1) * 1024]), "prm": np.ascontiguousarray(prm), "wr": wr,
                        "rb": np.ascontiguousarray(rb), "idd": idd, "wgu": wgu, "wdn": wdn})
    res = launch(nc, in_maps)
    return np.concatenate([r["out"].transpose(1, 0, 2).reshape(2048, 1024) for r in res], axis=1)


DECAY_SCALE_ = float(np.exp(-0.5))


def build_RW1():
    P = Prog()
    feats = P.dram("feats", [6, 128, 2 * S_], F32, "ExternalInput")
    prm = P.dram("prm", [128, 6, 2], F32, "ExternalInput")
    pv = P.dram("pv", [128, 8], F32, "ExternalInput")
    wl = P.dram("wl", [128, 5, 128], F32, "ExternalInput")
    bones = P.dram("bones", [128, 128], F32, "ExternalInput")
    outs = P.dram("outs", [11, 128, 2 * S_], F32, "ExternalOutput")
    mk_consts(P)
    PR = P.sb([128, 6, 2], F32)
    C0 = P.sb([128, 6], F32)
    PV = P.sb([128, 8], F32)
    OMK = P.sb([128, 1], F32)
    WL = P.sb([128, 5, 128], F32)
    BO = P.sb([128, 128], F32)
    STG = P.sb([128, S_], F32)
    F = [P.sb([128, S_], F32) for _ in range(6)]
    T1 = P.sb([128, S_], F32)
    T2 = P.sb([128, S_], F32)
    T3 = P.sb([128, S_], F32)
    banks = [P.ps([128, 512]) for _ in range(4)]
    P.dma('sp', PR[:], prm, w=['PR'])
    P.dma('sp', PV[:], pv, w=['PV'])
    P.dma('sp', WL[:], wl, w=['WL'])
    P.dma('sp', BO[:], bones, w=['BO'])
    P.op('dve', lambda e: e.tensor_tensor(out=C0[:], in0=PR[:, :, 0], in1=PR[:, :, 1], op=ALU.add), r=['PR'], w=['C0'])
    P.op('dve', lambda e: e.tensor_scalar(out=C0[:], in0=C0[:], scalar1=-1.0, scalar2=1.0, op0=ALU.mult, op1=ALU.add),
         r=['C0'], w=['C0'])
    P.op('dve', lambda e: e.tensor_scalar(out=OMK[:], in0=PV[:, 5:6], scalar1=-1.0, scalar2=1.0, op0=ALU.mult, op1=ALU.add),
         r=['PV'], w=['OMK'])
    bi = [0]

    def mm_act(lhsT, src, skey, dst, dkey, func, bias=None, scale=1.0, extra_r=()):
        for t in range(S_ // 512):
            ts = slice(t * 512, (t + 1) * 512)
            bank = banks[bi[0] % 4]
            bkk = ('bk', bi[0] % 4)
            P.op('pe', lambda e, ts=ts, bank=bank: e.matmul(bank[:, :], lhsT=lhsT, rhs=src[:, ts], start=True, stop=True),
                 r=[skey, 'WL', 'BO'], w=[bkk])
            if bias is not None:
                P.op('act', lambda e, ts=ts, bank=bank: e.activation(out=dst[:, ts], in_=bank[:, :], func=func, bias=bias, scale=scale),
                     r=[bkk, 'PV'] + list(extra_r), w=[dkey])
            else:
                P.op('act', lambda e, ts=ts, bank=bank: e.activation(out=dst[:, ts], in_=bank[:, :], func=func, scale=scale),
                     r=[bkk] + list(extra_r), w=[dkey])
            bi[0] += 1

    def store(idx, src, skey, b):
        P.dma('pool', outs[idx, :, b * S_:(b + 1) * S_], src[:], r=[skey])

    for b in range(2):
        bs = slice(b * S_, (b + 1) * S_)
        for a in range(6):
            P.dma('sp', STG[:], feats[a, :, bs], w=['STG'])
            Fa, fk = F[a], ('F', a)
            P.op('dve', lambda e, Fa=Fa, a=a: e.tensor_scalar(out=Fa[:], in0=STG[:], scalar1=C0[:, a:a + 1], scalar2=None, op0=ALU.mult),
                 r=['STG', 'C0'], w=[fk])
            P.op('dve', lambda e, Fa=Fa, a=a: e.scalar_tensor_tensor(out=Fa[:, 1:S_], in0=STG[:, 0:S_ - 1], scalar=PR[:, a, 0:1],
                                                                   in1=Fa[:, 1:S_], op0=ALU.mult, op1=ALU.add),
                 r=['STG', 'PR', fk], w=[fk])
            P.op('dve', lambda e, Fa=Fa, a=a: e.scalar_tensor_tensor(out=Fa[:, 0:S_ - 1], in0=STG[:, 1:S_], scalar=PR[:, a, 1:2],
                                                                   in1=Fa[:, 0:S_ - 1], op0=ALU.mult, op1=ALU.add),
                 r=['STG', 'PR', fk], w=[fk])
        R_, K_, V_, WD, AD, GD = F
        store(0, R_, ('F', 0), b)
        store(2, V_, ('F', 2), b)
        P.op('act', lambda e: e.activation(out=GD[:], in_=GD[:], func=AF.Sigmoid), r=[('F', 5)], w=[('F', 5)])
        mm_act(WL[:, 4, :], GD, ('F', 5), T1, 'T1', AF.Copy)
        store(9, T1, 'T1', b)
        P.op('dve', lambda e: e.tensor_scalar(out=T2[:], in0=K_[:], scalar1=PV[:, 4:5], scalar2=None, op0=ALU.mult),
             r=[('F', 1), 'PV'], w=['T2'])
        P.op('act', lambda e: e.activation(out=T3[:], in_=T2[:], func=AF.Square), r=['T2'], w=['T3'])
        mm_act(BO[:], T3, 'T3', T1, 'T1', AF.Sqrt, bias=epsb[0][:], extra_r=['epsb'])
        P.op('dve', lambda e: e.reciprocal(out=T1[:], in_=T1[:]), r=['T1'], w=['T1'])
        P.op('dve', lambda e: e.tensor_tensor(out=T2[:], in0=T2[:], in1=T1[:], op=ALU.mult), r=['T1', 'T2'], w=['T2'])
        store(1, T2, 'T2', b)
        P.op('act', lambda e: e.activation(out=WD[:], in_=WD[:], func=AF.Tanh), r=[('F', 3)], w=[('F', 3)])
        for z in range(2):
            mm_act(WL[:, 2 + z, :], AD, ('F', 4), T1, 'T1', AF.Sigmoid, bias=PV[:, 2 + z:3 + z])
            P.op('dve', lambda e: e.tensor_tensor(out=T3[:], in0=T1[:], in1=T2[:], op=ALU.mult), r=['T1', 'T2'], w=['T3'])
            store(3 + z, T3, 'T3', b)
            P.op('dve', lambda e: e.tensor_scalar(out=T1[:], in0=T1[:], scalar1=PV[:, 5:6], scalar2=OMK[:, 0:1],
                                                  op0=ALU.mult, op1=ALU.add), r=['T1', 'PV', 'OMK'], w=['T1'])
            P.op('dve', lambda e: e.tensor_tensor(out=T1[:], in0=T1[:], in1=K_[:], op=ALU.mult), r=['T1', ('F', 1)], w=['T1'])
            store(5 + z, T1, 'T1', b)
            P.op('dve', lambda e: e.scalar_tensor_tensor(out=T3[:], in0=R_[:], scalar=PV[:, 6:7], in1=T1[:], op0=ALU.mult, op1=ALU.mult),
                 r=[('F', 0), 'PV', 'T1', 'T3'], w=['T3'])
            mm_act(BO[:], T3, 'T3', T3, 'T3b', AF.Copy)
            if z == 0:
                P.op('dve', lambda e: e.tensor_tensor(out=STG[:], in0=T3[:], in1=V_[:], op=ALU.mult), r=['T3b', ('F', 2), 'STG'], w=['STG'])
            else:
                P.op('dve', lambda e: e.tensor_tensor(out=T3[:], in0=T3[:], in1=V_[:], op=ALU.mult), r=['T3b', ('F', 2)], w=['T3b'])
                P.op('dve', lambda e: e.tensor_tensor(out=STG[:], in0=STG[:], in1=T3[:], op=ALU.add), r=['T3b', 'STG'], w=['STG'])
                store(10, STG, 'STG', b)
            mm_act(WL[:, z, :], WD, ('F', 3), T1, 'T1', AF.Sigmoid, bias=PV[:, z:z + 1])
            P.op('dve', lambda e: e.tensor_scalar(out=T1[:], in0=T1[:], scalar1=-DECAY_SCALE_, scalar2=None, op0=ALU.mult),
                 r=['T1'], w=['T1'])
            store(7 + z, T1, 'T1', b)
    return P.build()


RW_OFF = 7168


def run_RW1(pT, inp, l):
    nc = cached('RW1', build_RW1)
    fT = pT[RW_OFF:RW_OFF + 3456]
    mu_p, mu_n = inp["rwkv_mu_prev"][l], inp["rwkv_mu_next"][l]
    bones = np.kron(np.eye(2, dtype=np.float32), np.ones((64, 64), np.float32))
    in_maps = []
    for i in range(NCORE):
        cs = slice(i * 128, (i + 1) * 128)
        rows = [slice(i * 128, (i + 1) * 128), slice(1024 + i * 128, 1024 + (i + 1) * 128),
                slice(2048 + i * 128, 2048 + (i + 1) * 128), slice(3072, 3200), slice(3200, 3328), slice(3328, 3456)]
        feats = np.stack([fT[r] for r in rows])
        prm = np.stack([np.stack([mu_p[r], mu_n[r]], axis=1) for r in rows], axis=1)
        pv = np.zeros((128, 8), np.float32)
        pv[:, 0] = inp["rwkv_w0"][l][0, cs]; pv[:, 1] = inp["rwkv_w0"][l][1, cs]
        pv[:, 2] = inp["rwkv_a0"][l][0, cs]; pv[:, 3] = inp["rwkv_a0"][l][1, cs]
        pv[:, 4] = inp["rwkv_k_k"][l][cs]; pv[:, 5] = inp["rwkv_k_a"][l][cs]
        pv[:, 6] = inp["rwkv_r_k"][l].reshape(-1)[cs]
        wl = np.zeros((128, 5, 128), np.float32)
        for z in range(2):
            wl[z * 64:(z + 1) * 64, z, :] = inp["rwkv_w_up"][l][z][:, cs]
            wl[z * 64:(z + 1) * 64, 2 + z, :] = inp["rwkv_a_up"][l][z][:, cs]
        wl[:, 4, :] = inp["rwkv_g_up"][l][:, cs]
        in_maps.append({"feats": np.ascontiguousarray(feats), "prm": np.ascontiguousarray(prm.astype(np.float32)),
                        "pv": pv, "wl": wl, "bones": bones})
    res = launch(nc, in_maps)
    return [r["outs"] for r in res]


def build_RW2(nseg=8):
    P = Prog()
    arr = P.dram("arr", [4, 8, 128, 6, 512], F32, "ExternalInput")
    cm = P.dram("cm", [128, 5, 128], F32, "ExternalInput")
    yout = P.dram("yout", [4, 128, 64, 64], F32, "ExternalOutput")
    CM = P.sb([128, 5, 128], F32)
    NL, NU, UU, UI, IDN = [CM[:, i, :] for i in range(5)]
    MSK = P.sb([128, 512], F32)
    IN = [[P.sb([128, 6, 512], F32) for _ in range(2)] for _ in range(4)]
    CUM = [P.sb([128, 512], F32) for _ in range(4)]
    YB = [[P.sb([128, 8, 64], F32) for _ in range(2)] for _ in range(4)]
    S0 = [P.sb([128, 64], F32) for _ in range(4)]
    EX = [[P.sb([128, 64], F32) for _ in range(4)] for _ in range(4)]
    names = ['A', 'B', 'K', 'R', 'BG', 'KG', 'V']
    BD = [{nm: P.sb([128, 128], F32) for nm in names} for _ in range(4)]
    WK = [{nm: P.sb([128, 128], F32) for nm in ['P0', 'PT0', 'P1', 'PT1', 'TT', 'NKA', 'MBR', 'MKR', 'BGT', 'KGT']} for _ in range(4)]
    SM = [{nm: P.sb([128, 64], F32) for nm in ['VST', 'VH', 'W0', 'NRHO']} for _ in range(4)]
    banks = [P.ps([128, 512]) for _ in range(8)]
    slot = [0]

    def ps():
        s = slot[0] % 8
        slot[0] += 1
        return banks[s][:, 0:128], ('ps', s)

    P.dma('sp', CM[:], cm, w=['CM'])
    P.op('pool', lambda e: e.memset(MSK[:], 1.0), w=['MSK'])
    P.op('pool', lambda e: e.memset(MSK[:].rearrange("p (n t) -> p n t", t=64)[:, :, 0:1], 0.0), r=['MSK'], w=['MSK'])
    for p in range(4):
        for nm in names:
            P.op('pool', lambda e, p=p, nm=nm: e.memset(BD[p][nm][:], 0.0), w=[('BD', p, nm)])
        P.op('pool', lambda e, p=p: e.memset(S0[p][:], 0.0), w=[('S0', p)])
    for sg in range(nseg):
        ib = sg % 2
        for p in range(4):
            ik = ('IN', p, ib)
            P.dma('sp' if p % 2 == 0 else 'act', IN[p][ib][:], arr[p, sg], w=[ik])
            P.op('dve', lambda e, p=p, ib=ib: e.tensor_tensor_scan(out=CUM[p][:], data0=MSK[:], data1=IN[p][ib][:, 5, :], initial=0.0,
                                                                   op0=ALU.mult, op1=ALU.add), r=[ik, 'MSK'], w=[('CUM', p)])
            P.op('pool', lambda e, p=p, ib=ib: e.tensor_tensor(out=IN[p][ib][:, 5, :], in0=CUM[p][:], in1=IN[p][ib][:, 5, :],
                                                               op=ALU.subtract), r=[('CUM', p), ik], w=[ik])
        for j in range(8):
            cs = slice(j * 64, (j + 1) * 64)
            for p in range(4):
                ik = ('IN', p, ib)
                I_ = IN[p][ib]
                Ep, En, Ex, Ec = EX[p]
                ek = [('EX', p, i) for i in range(4)]
                ck = ('CUM', p)
                P.op('act', lambda e, p=p, Ep=Ep, cs=cs: e.activation(out=Ep[:], in_=CUM[p][:, cs], func=AF.Exp), r=[ck], w=[ek[0]])
                P.op('act', lambda e, p=p, En=En, cs=cs: e.activation(out=En[:], in_=CUM[p][:, cs], func=AF.Exp, scale=-1.0), r=[ck], w=[ek[1]])
                P.op('act', lambda e, I_=I_, Ex=Ex, cs=cs: e.activation(out=Ex[:], in_=I_[:, 5, cs], func=AF.Exp), r=[ik], w=[ek[2]])
                P.op('act', lambda e, p=p, Ec=Ec, cs=cs, j=j: e.activation(out=Ec[:], in_=CUM[p][:, cs], func=AF.Exp, scale=-1.0,
                                                                         bias=CUM[p][:, j * 64 + 63:j * 64 + 64]), r=[ck], w=[ek[3]])
                specs = [('A', 1, Ex, ek[2]), ('B', 3, En, ek[1]), ('K', 4, En, ek[1]), ('R', 0, Ep, ek[0]),
                         ('BG', 3, Ec, ek[3]), ('KG', 4, Ec, ek[3]), ('V', 2, None, None)]
                for si, (nm, ai, Et, etk) in enumerate(specs):
                    for c in range(2):
                        ps_ = slice(c * 64, (c + 1) * 64)
                        dst = BD[p][nm][ps_, c * 64:(c + 1) * 64]
                        eng = 'pool' if si % 2 == 0 else 'dve'
                        if Et is None:
                            P.op(eng, lambda e, dst=dst, I_=I_, ai=ai, ps_=ps_, cs=cs: e.tensor_copy(out=dst, in_=I_[ps_, ai, cs]),
                                 r=[ik], w=[('BD', p, nm)])
                        else:
                            P.op(eng, lambda e, dst=dst, I_=I_, ai=ai, ps_=ps_, cs=cs, Et=Et: e.tensor_tensor(
                                out=dst, in0=I_[ps_, ai, cs], in1=Et[ps_, :], op=ALU.mult), r=[ik, etk], w=[('BD', p, nm)])
                bd = BD[p]
                bk_ = lambda nm, p=p: ('BD', p, nm)
                wk = WK[p]
                wkk = lambda nm, p=p: ('WK', p, nm)
                smm = SM[p]
                smk = lambda nm, p=p: ('SM', p, nm)

                def mm(lhsT, lk, rhs, rk, n=128):
                    o, ok = ps()
                    oo = o[:, 0:n]
                    P.op('pe', lambda e, oo=oo, lhsT=lhsT, rhs=rhs: e.matmul(oo, lhsT=lhsT, rhs=rhs, start=True, stop=True),
                         r=[lk, rk], w=[ok])
                    return oo, ok

                def mmacc(terms, n=64):
                    o, ok = ps()
                    oo = o[:, 0:n]
                    for ti, (lhsT, lk, rhs, rk) in enumerate(terms):
                        P.op('pe', lambda e, oo=oo, lhsT=lhsT, rhs=rhs, ti=ti: e.matmul(
                            oo, lhsT=lhsT, rhs=rhs, start=(ti == 0), stop=(ti == len(terms) - 1)), r=[lk, rk], w=[ok])
                    return oo, ok

                def evac_mask(dst, dk, src, sk, mask):
                    P.op('dve', lambda e, dst=dst, src=src, mask=mask: e.tensor_tensor(out=dst, in0=src, in1=mask, op=ALU.mult),
                         r=['CM'], w=[dk, sk])

                def evac_copy(dst, dk, src, sk, scale=1.0):
                    P.op('act', lambda e, dst=dst, src=src, scale=scale: e.activation(out=dst, in_=src, func=AF.Copy, scale=scale),
                         r=[], w=[dk, sk])

                o, ok = mm(bd['A'][:], bk_('A'), bd['B'][:], bk_('B'))
                evac_mask(wk['P0'][:], wkk('P0'), o, ok, NL)
                o, ok = mm(bd['B'][:], bk_('B'), bd['A'][:], bk_('A'))
                evac_mask(wk['PT0'][:], wkk('PT0'), o, ok, NU)
                P.op('pool', lambda e, wk=wk: e.tensor_tensor(out=wk['TT'][:], in0=wk['PT0'][:], in1=IDN, op=ALU.add),
                     r=[wkk('PT0'), 'CM'], w=[wkk('TT')])
                o, ok = mm(bd['K'][:], bk_('K'), bd['A'][:], bk_('A'))
                evac_mask(wk['NKA'][:], wkk('NKA'), o, ok, UU)
                o, ok = mm(bd['B'][:], bk_('B'), bd['R'][:], bk_('R'))
                evac_mask(wk['MBR'][:], wkk('MBR'), o, ok, UI)
                o, ok = mm(bd['K'][:], bk_('K'), bd['R'][:], bk_('R'))
                evac_mask(wk['MKR'][:], wkk('MKR'), o, ok, UI)
                o, ok = ps()
                P.op('pe', lambda e, o=o, bd=bd: e.transpose(out=o, in_=bd['V'][:], identity=IDN), r=[bk_('V'), 'CM'], w=[ok])
                evac_copy(smm['VH'][:], smk('VH'), o[:, 0:64], ok)
                P.op('dve', lambda e, o=o, smm=smm: e.tensor_tensor(out=smm['VST'][:], in0=o[:, 64:128], in1=smm['VH'][:], op=ALU.add),
                     r=[smk('VH')], w=[smk('VST'), ok])
                for (src, dstn) in (('BG', 'BGT'), ('KG', 'KGT')):
                    o, ok = ps()
                    P.op('pe', lambda e, o=o, bd=bd, src=src: e.transpose(out=o, in_=bd[src][:], identity=IDN), r=[bk_(src), 'CM'], w=[ok])
                    evac_copy(wk[dstn][:], wkk(dstn), o, ok)
                cur, curT = 'P0', 'PT0'
                for lv in range(1, 6):
                    nxt, nxtT = ('P1', 'PT1') if cur == 'P0' else ('P0', 'PT0')
                    o, ok = mm(wk[curT][:], wkk(curT), wk[cur][:], wkk(cur))
                    evac_copy(wk[nxt][:], wkk(nxt), o, ok)
                    if lv < 5:
                        o2, ok2 = mm(wk[cur][:], wkk(cur), wk[curT][:], wkk(curT))
                        evac_copy(wk[nxtT][:], wkk(nxtT), o2, ok2)
                    o3, ok3 = mm(wk[nxt][:], wkk(nxt), wk['TT'][:], wkk('TT'))
                    P.op('dve', lambda e, wk=wk, o3=o3: e.tensor_tensor(out=wk['TT'][:], in0=o3, in1=wk['TT'][:], op=ALU.add),
                         r=[wkk('TT')], w=[wkk('TT'), ok3])
                    cur, curT = nxt, nxtT
                s0k = ('S0', p)
                o, ok = mmacc([(bd['A'][:], bk_('A'), S0[p][:], s0k), (wk['NKA'][:], wkk('NKA'), smm['VST'][:], smk('VST'))])
                evac_copy(smm['W0'][:], smk('W0'), o, ok)
                o, ok = mm(wk['TT'][:], wkk('TT'), smm['W0'][:], smk('W0'), n=64)
                evac_copy(smm['NRHO'][:], smk('NRHO'), o, ok, scale=-1.0)
                o, ok = mmacc([(bd['R'][:], bk_('R'), S0[p][:], s0k), (wk['MBR'][:], wkk('MBR'), smm['NRHO'][:], smk('NRHO')),
                               (wk['MKR'][:], wkk('MKR'), smm['VST'][:], smk('VST'))])
                evac_copy(YB[p][ib][:, j, :], ('YB', p, ib), o, ok)
                o, ok = mmacc([(wk['BGT'][:], wkk('BGT'), smm['NRHO'][:], smk('NRHO')), (wk['KGT'][:], wkk('KGT'), smm['VST'][:], smk('VST'))])
                P.op('dve', lambda e, p=p, Ep=Ep, o=o: e.scalar_tensor_tensor(out=S0[p][:], in0=S0[p][:], scalar=Ep[:, 63:64], in1=o,
                                                                             op0=ALU.mult, op1=ALU.add), r=[s0k, ek[0]], w=[s0k, ok])
        for p in range(4):
            P.dma('pool', yout[p, :, sg * 8:(sg + 1) * 8, :], YB[p][ib][:], r=[('YB', p, ib)])
    return P.build()


def rw2_consts():
    t = np.arange(64)
    L = (t[None, :] < t[:, None]).astype(np.float32)
    U = L.T.copy()
    UI = U + np.eye(64, dtype=np.float32)
    bd = lambda m: np.kron(np.eye(2, dtype=np.float32), m)
    return np.ascontiguousarray(np.stack([bd(-L), bd(-U), bd(U), bd(UI), np.eye(128, dtype=np.float32)], axis=1))


def run_RW2(rw1, nseg=8):
    nc = cached(('RW2', nseg), lambda: build_RW2(nseg))
    cm = rw2_consts()
    in_maps = []
    for i in range(NCORE):
        o = rw1[i]
        arrs = np.zeros((4, 8, 128, 6, 512), np.float32)
        for z in range(2):
            for b in range(2):
                sel = np.stack([o[0], o[1], o[2], o[3 + z], o[5 + z], o[7 + z]], axis=1)[:, :, b * S_:(b + 1) * S_]
                if z == 1:
                    sel = sel[:, :, ::-1]
                arrs[z * 2 + b] = sel.reshape(128, 6, 8, 512).transpose(2, 0, 1, 3)
        in_maps.append({"arr": arrs, "cm": cm})
    res = launch(nc, in_maps)
    return [r["yout"] for r in res]


def build_RW3():
    P = Prog()
    yfb = P.dram("yfb", [2, 128, 8, 1024], F32, "ExternalInput")
    bvg = P.dram("bvg", [2, 128, 8, 1024], F32, "ExternalInput")
    out = P.dram("out", [128, 8, 1024], F32, "ExternalOutput")
    mk_consts(P)
    Y = [P.sb([128, 1024], F32) for _ in range(2)]
    BV = P.sb([128, 1024], F32)
    G = P.sb([128, 1024], F32)
    SQ = P.sb([128, 1024], F32)
    O = [P.sb([128, 1024], F32) for _ in range(2)]
    st = P.sb([128, 2, 4, 16], F32)
    for tt in range(8):
        ob = O[tt % 2]
        okk = ('O', tt % 2)
        P.dma('sp', BV[:], bvg[0, :, tt, :], w=['BV'])
        P.dma('sp', G[:], bvg[1, :, tt, :], w=['G'])
        for z in range(2):
            yk = ('Y', z)
            P.dma('pool', Y[z][:], yfb[z, :, tt, :], w=[yk])
            y3 = Y[z][:].rearrange("p (h v) -> p h v", h=16)
            s1, s2, mn, rs = [st[:, z, i, :] for i in range(4)]
            sk = ('st', z)
            P.op('dve', lambda e, y3=y3, s1=s1: e.tensor_reduce(out=s1, in_=y3, axis=AX.X, op=ALU.add), r=[yk], w=[sk])
            P.op('act', lambda e, z=z: e.activation(out=SQ[:], in_=Y[z][:], func=AF.Square), r=[yk], w=['SQ'])
            P.op('dve', lambda e, s2=s2: e.tensor_reduce(out=s2, in_=SQ[:].rearrange("p (h v) -> p h v", h=16), axis=AX.X, op=ALU.add),
                 r=['SQ', sk], w=[sk])
            P.op('dve', lambda e, s1=s1, mn=mn: e.tensor_scalar(out=mn, in0=s1, scalar1=1.0 / 64, scalar2=None, op0=ALU.mult), r=[sk], w=[sk])
            P.op('dve', lambda e, s1=s1, mn=mn: e.tensor_tensor(out=s1, in0=mn, in1=mn, op=ALU.mult), r=[sk], w=[sk])
            P.op('dve', lambda e, s1=s1, s2=s2: e.scalar_tensor_tensor(out=s2, in0=s2, scalar=1.0 / 64, in1=s1, op0=ALU.mult, op1=ALU.subtract),
                 r=[sk], w=[sk])
            P.op('act', lambda e, s2=s2, rs=rs: e.activation(out=rs, in_=s2, func=AF.Sqrt, bias=epsb[0][:]), r=[sk, 'epsb'], w=[sk])
            P.op('dve', lambda e, rs=rs: e.reciprocal(out=rs, in_=rs), r=[sk], w=[sk])
            for h in range(16):
                hs = slice(h * 64, (h + 1) * 64)
                eng = 'dve' if h % 2 == 0 else 'pool'
                P.op(eng, lambda e, z=z, hs=hs, h=h, mn=mn, rs=rs: e.tensor_scalar(
                    out=Y[z][:, hs], in0=Y[z][:, hs], scalar1=mn[:, h:h + 1], scalar2=rs[:, h:h + 1], op0=ALU.subtract, op1=ALU.mult),
                    r=[sk, yk], w=[yk])
        P.op('dve', lambda e, ob=ob: e.tensor_tensor(out=ob[:], in0=Y[0][:], in1=Y[1][:], op=ALU.add), r=[('Y', 0), ('Y', 1)], w=[okk])
        P.op('dve', lambda e, ob=ob: e.tensor_tensor(out=ob[:], in0=ob[:], in1=BV[:], op=ALU.add), r=['BV', okk], w=[okk])
        P.op('dve', lambda e, ob=ob: e.tensor_tensor(out=ob[:], in0=ob[:], in1=G[:], op=ALU.mult), r=['G', okk], w=[okk])
        P.dma('sp', out[:, tt, :], ob[:], r=[okk])
    return P.build()


def run_RW3(rw1, rw2):
    nc = cached('RW3', build_RW3)
    yz = np.zeros((2, 2, S_, 16, 64), np.float32)
    for i in range(NCORE):
        for z in range(2):
            for b in range(2):
                y = rw2[i][z * 2 + b].reshape(2, 64, 64, 64).transpose(2, 1, 0, 3).reshape(S_, 2, 64)
                if z == 1:
                    y = y[::-1]
                yz[z, b, :, 2 * i:2 * i + 2, :] = y
    yz = yz.reshape(2, 2 * S_, 1024)
    bv = np.concatenate([o[10] for o in rw1], axis=0).T
    g = np.concatenate([o[9] for o in rw1], axis=0).T
    in_maps = []
    for i in range(NCORE):
        ts = slice(i * 1024, (i + 1) * 1024)
        tmj = lambda a: a[ts].reshape(8, 128, 1024).transpose(1, 0, 2)
        in_maps.append({"yfb": np.ascontiguousarray(np.stack([tmj(yz[0]), tmj(yz[1])])),
                        "bvg": np.ascontiguousarray(np.stack([tmj(bv), tmj(g)]))})
    res = launch(nc, in_maps)
    yd = np.concatenate([r["out"].transpose(1, 0, 2).reshape(1024, 1024) for r in res], axis=0)
    return np.ascontiguousarray(yd.T)


def kernel(**inp):
    inp = {k: np.asarray(v) for k, v in inp.items()}
    x = inp["x"].astype(np.float32)
    B, S, Dm = x.shape
    mod = run_ada(inp["c"], inp["ada_w"], inp["ada_b"])
    xT = np.ascontiguousarray(x.reshape(B * S, Dm).T)
    for l in range(2):
        sh1, sc1, g1, sh2, sc2, g2 = [mod[l][:, j * 2048:(j + 1) * 2048] for j in range(6)]
        hT = run_H(xT, inp["norm1_g"][l], sc1, sh1)
        pT = run_P(hT, inp["w_in"][l])
        del hT
        ya = run_LRU(pT, inp, l)
        yb = run_RET(pT, inp["positions"])
        yc = run_SGU(pT, inp, l)
        rw1 = run_RW1(pT, inp, l)
        rw2 = run_RW2(rw1)
        yd = run_RW3(rw1, rw2)
        del rw1, rw2
        ys = np.stack([ya, yb, yc, yd])
        x1T = run_M(ys, pT, xT, g1, inp["w_branch"][l], inp["w_out"][l])
        del pT, ys
        xT = run_E(x1T, inp["norm2_g"][l], sc2, sh2, g2, inp, l)
    z = np.zeros((2, 2048), np.float32)
    oT = run_H(xT, inp["final_norm_g"], z, z, out_dt=F32)
    return np.ascontiguousarray(oT.T).reshape(B, S, Dm).astype(np.float32)
```

```python
import contextlib
import numpy as np
import concourse.bass as bass
import concourse.mybir as mybir
from concourse.bass_utils import run_bass_kernel_spmd

F32 = mybir.dt.float32
BF16 = mybir.dt.bfloat16
I32 = mybir.dt.int32
AF = mybir.ActivationFunctionType
ALU = mybir.AluOpType
AX = mybir.AxisListType

N_LAUNCH = [0]
TRACE = [False]


class Prog:
    ENG = ('pe', 'act', 'dve', 'pool', 'sp')
    SAME = {'pe': False, 'act': True, 'dve': True, 'pool': True, 'sp': True}
    K = 6

    def __init__(self):
        self.nc = bass.Bass("TRN2", target_bir_lowering=False)
        self.es = contextlib.ExitStack()
        self.ops = {e: [] for e in self.ENG}
        self.res = {}
        self.seen = {e: {} for e in self.ENG}
        self.ndma = {e: 0 for e in self.ENG}
        self.n_t = 0

    def dram(self, name, shape, dt, kind):
        return self.nc.dram_tensor(name, list(shape), dt, kind=kind).ap()

    def sb(self, shape, dt, name=None):
        self.n_t += 1
        name = name or f"t{self.n_t}"
        return self.es.enter_context(self.nc.sbuf_tensor(name, list(shape), dt))

    def ps(self, shape, dt=F32, name=None):
        self.n_t += 1
        name = name or f"p{self.n_t}"
        return self.es.enter_context(self.nc.psum_tensor(name, list(shape), dt))

    def op(self, eng, fn, r=(), w=(), dma=False):
        deps = {}

        def add(tok):
            if tok is None:
                return
            k, v = tok
            if deps.get(k, -1) < v:
                deps[k] = v
        for key in r:
            st = self.res.get(key)
            if st:
                add(st[0])
        for key in w:
            st = self.res.get(key)
            if st:
                add(st[0])
                for t in st[1]:
                    add(t)
        if dma:
            i = self.ndma[eng]
            self.ndma[eng] += 1
            slot, n = i % self.K, i // self.K
            if n > 0:
                add((('d', eng, slot), n))
            tok = (('d', eng, slot), n + 1)
        else:
            tok = (eng, len(self.ops[eng]))
        waits = []
        for k, v in deps.items():
            if k == eng and not self.SAME[eng]:
                continue
            if self.seen[eng].get(k, -1) >= v:
                continue
            self.seen[eng][k] = v
            waits.append((k, v))
        self.ops[eng].append(dict(fn=fn, waits=waits, tok=tok, dma=dma))
        for key in r:
            self.res.setdefault(key, [None, []])[1].append(tok)
        for key in w:
            self.res[key] = [tok, []]

    def dma(self, q, out, in_, r=(), w=(), **kw):
        self.op(q, lambda e: e.dma_start(out=out, in_=in_, **kw), r=r, w=w, dma=True)

    def build(self):
        nc = self.nc
        es = self.es
        csem = {e: es.enter_context(nc.semaphore(f"s_{e}")) for e in self.ENG}
        dsem = {}
        for e in self.ENG:
            for s in range(min(self.K, self.ndma[e])):
                dsem[('d', e, s)] = es.enter_context(nc.semaphore(f"d_{e}{s}"))
        needs = set()
        for e in self.ENG:
            for o in self.ops[e]:
                for k, v in o['waits']:
                    if not isinstance(k, tuple):
                        needs.add((k, v))
        val = {}
        for e in self.ENG:
            c = 0
            for idx, o in enumerate(self.ops[e]):
                if (e, idx) in needs:
                    c += 1
                    val[(e, idx)] = c
        finals = []
        for e in self.ENG:
            for s in range(min(self.K, self.ndma[e])):
                cnt = (self.ndma[e] - s + self.K - 1) // self.K
                finals.append((('d', e, s), cnt))

        def emit(eobj, eng):
            for idx, o in enumerate(self.ops[eng]):
                for k, v in o['waits']:
                    if isinstance(k, tuple):
                        eobj.wait_ge(dsem[k], 16 * v)
                    else:
                        eobj.wait_ge(csem[k], val[(k, v)])
                ins = o['fn'](eobj)
                if o['dma']:
                    ins.then_inc(dsem[o['tok'][0]], 16)
                elif (eng, idx) in needs:
                    ins.then_inc(csem[eng], 1)
            if eng == 'sp':
                for k, v in finals:
                    eobj.wait_ge(dsem[k], 16 * v)

        with nc.Block() as block:
            @block.tensor
            def _(e):
                emit(e, 'pe')

            @block.scalar
            def _(e):
                emit(e, 'act')

            @block.vector
            def _(e):
                emit(e, 'dve')

            @block.gpsimd
            def _(e):
                emit(e, 'pool')

            @block.sync
            def _(e):
                emit(e, 'sp')
        es.close()
        return nc


def launch(nc, in_maps):
    N_LAUNCH[0] += 1
    if TRACE[0]:
        res = run_bass_kernel_spmd(nc, in_maps, core_ids=list(range(len(in_maps))), trace=True)
        print("LAUNCH exec_time_ns", res.exec_time_ns, flush=True)
        return res.results
    res = run_bass_kernel_spmd(nc, in_maps, core_ids=list(range(len(in_maps))))
    return res.results


D = 2048
NCORE = 8


def build_ada():
    P = Prog()
    cT = P.dram("cT", [128, 16, 2], F32, "ExternalInput")
    w = P.dram("w", [2048, 3072], F32, "ExternalInput")
    b = P.dram("b", [1, 3072], F32, "ExternalInput")
    out = P.dram("out", [2, 3072], F32, "ExternalOutput")
    ct = P.sb([128, 16, 2], F32)
    sc = P.sb([128, 16, 2], F32)
    bt = P.sb([2, 3072], F32)
    ot = P.sb([2, 3072], F32)
    wts = [P.sb([128, 3072], F32) for _ in range(3)]
    pss = [P.ps([128, 512]) for _ in range(6)]
    P.dma('sp', ct[:], cT, w=['ct'])
    P.dma('sp', bt[0:1, :], b, w=['bt0'])
    P.dma('sp', bt[1:2, :], b, w=['bt1'])
    P.op('act', lambda e: e.activation(out=sc[:], in_=ct[:], func=AF.Silu), r=['ct'], w=['sc'])
    for kc in range(16):
        wt = wts[kc % 3]
        q = 'sp' if kc % 2 == 0 else 'pool'
        P.dma(q, wt[:], w[kc * 128:(kc + 1) * 128, :], w=[('wt', kc % 3)])
        for n in range(6):
            P.op('pe', lambda e, kc=kc, n=n, wt=wt: e.matmul(
                pss[n][0:2, :], lhsT=sc[:, kc, :], rhs=wt[:, n * 512:(n + 1) * 512],
                start=(kc == 0), stop=(kc == 15)),
                r=['sc', ('wt', kc % 3)], w=[('ps', n)])
    for n in range(6):
        P.op('dve', lambda e, n=n: e.tensor_tensor(
            out=ot[:, n * 512:(n + 1) * 512], in0=pss[n][0:2, :], in1=bt[:, n * 512:(n + 1) * 512], op=ALU.add),
            r=[('ps', n), 'bt0', 'bt1'], w=[('ot', n)])
    P.dma('sp', out, ot[:], r=[('ot', n) for n in range(6)])
    return P.build()


def run_ada(c, ada_w, ada_b):
    nc = cached("ada", build_ada)
    cT = np.ascontiguousarray(c.T.reshape(16, 128, 2).transpose(1, 0, 2))
    wall = ada_w.reshape(2, 2048, 4, 3072)
    ball = ada_b.reshape(2, 4, 3072)
    in_maps = []
    for i in range(NCORE):
        l, j = i // 4, i % 4
        in_maps.append({"cT": cT, "w": np.ascontiguousarray(wall[l, :, j, :]),
                        "b": np.ascontiguousarray(ball[l, j][None, :])})
    res = launch(nc, in_maps)
    mod = np.zeros((2, 2, 12288), np.float32)
    for i in range(NCORE):
        l, j = i // 4, i % 4
        mod[l, :, j * 3072:(j + 1) * 3072] = res[i]["out"]
    return mod


def fm(a):
    r, c = a.shape
    return np.ascontiguousarray(a.reshape(r // 128, 128, c).transpose(1, 0, 2))


def vec_fm(v):
    return np.ascontiguousarray(v.reshape(-1, 128).T)


def emit_norm(P, xt, nk, ntok, gam, sc, sh, ones, ps_banks, rstd, tmp, outs, tag):
    Dn = nk * 128
    gm = P.sb([128, nk], F32)
    if sc is not None:
        P.op('dve', lambda e: e.scalar_tensor_tensor(out=gm[:], in0=sc[:], scalar=1.0, in1=gam[:],
                                                     op0=ALU.add, op1=ALU.mult),
             r=[(tag, 'sc'), (tag, 'gam')], w=[(tag, 'gm')])
    else:
        P.op('dve', lambda e: e.tensor_copy(out=gm[:], in_=gam[:]), r=[(tag, 'gam')], w=[(tag, 'gm')])
    nt = ntok // 512
    for t in range(nt):
        ts = slice(t * 512, (t + 1) * 512)
        bank = ps_banks[t % len(ps_banks)]
        bk = ('psb', id(bank))
        for k in range(nk):
            j = k % 2
            P.op('act', lambda e, k=k, j=j, ts=ts: e.activation(out=tmp[:, j, :], in_=xt[:, k, ts], func=AF.Square),
                 r=[(tag, 'x', k)], w=[(tag, 'tmp', j)])
            P.op('pe', lambda e, k=k, j=j, bank=bank: e.matmul(bank[:, :], lhsT=ones[:], rhs=tmp[:, j, :],
                                                             start=(k == 0), stop=(k == nk - 1)),
                 r=[(tag, 'tmp', j), 'ones'], w=[bk])
        P.op('act', lambda e, ts=ts, bank=bank: e.activation(out=rstd[:, ts], in_=bank[:, :], func=AF.Sqrt,
                                                            scale=1.0 / Dn, bias=epsb[0][:]),
             r=[bk, 'epsb'], w=[(tag, 'rstd', t)])
        P.op('dve', lambda e, ts=ts: e.reciprocal(out=rstd[:, ts], in_=rstd[:, ts]),
             r=[(tag, 'rstd', t)], w=[(tag, 'rstd', t)])
        for k in range(nk):
            j = k % 2
            P.op('dve', lambda e, k=k, j=j, ts=ts: e.tensor_tensor(out=tmp[:, j, :], in0=xt[:, k, ts], in1=rstd[:, ts],
                                                                 op=ALU.mult),
                 r=[(tag, 'x', k), (tag, 'rstd', t)], w=[(tag, 'tmp', j)])
            for i, ot in enumerate(outs):
                if sh is not None:
                    P.op('act', lambda e, k=k, j=j, ts=ts, ot=ot: e.activation(
                        out=ot[:, k, ts], in_=tmp[:, j, :], func=AF.Identity, scale=gm[:, k:k + 1], bias=sh[:, k:k + 1]),
                        r=[(tag, 'tmp', j), (tag, 'gm'), (tag, 'sh')], w=[(tag, 'h', i, t)])
                else:
                    P.op('act', lambda e, k=k, j=j, ts=ts, ot=ot: e.activation(
                        out=ot[:, k, ts], in_=tmp[:, j, :], func=AF.Copy, scale=gm[:, k:k + 1]),
                        r=[(tag, 'tmp', j), (tag, 'gm')], w=[(tag, 'h', i, t)])


epsb = [None]


def mk_consts(P):
    ones = P.sb([128, 128], F32, "ones")
    P.op('pool', lambda e: e.memset(ones[:], 1.0), w=['ones'])
    eb = P.sb([128, 1], F32, "epsb")
    P.op('pool', lambda e: e.memset(eb[:], 1e-6), w=['epsb'])
    epsb[0] = eb
    return ones


def build_H(out_dt=BF16):
    P = Prog()
    xT = P.dram("xT", [128, 16, 1024], F32, "ExternalInput")
    prm = P.dram("prm", [128, 3, 16], F32, "ExternalInput")
    hT = P.dram("hT", [128, 16, 1024], out_dt, "ExternalOutput")
    ones = mk_consts(P)
    xt = P.sb([128, 16, 1024], F32)
    ht = P.sb([128, 16, 1024], out_dt)
    pt = P.sb([128, 3, 16], F32)
    rstd = P.sb([128, 1024], F32)
    tmp = P.sb([128, 2, 512], F32)
    banks = [P.ps([128, 512]) for _ in range(2)]
    P.dma('sp', pt[:], prm, w=[('n', 'gam'), ('n', 'sc'), ('n', 'sh')])
    for k in range(16):
        P.dma('sp' if k % 2 == 0 else 'pool', xt[:, k, :], xT[:, k, :], w=[('n', 'x', k)])
    emit_norm(P, xt, 16, 1024, pt[:, 0, :], pt[:, 1, :], pt[:, 2, :], ones, banks, rstd, tmp, [ht], 'n')
    P.dma('sp', hT, ht[:], r=[('n', 'h', 0, 0), ('n', 'h', 0, 1)])
    return P.build()


def run_H(xT_full, gam, sc, sh, out_dt=BF16):
    nc = cached(('H', str(out_dt)), lambda: build_H(out_dt))
    in_maps = []
    for i in range(NCORE):
        b = i // 4
        prm = np.stack([vec_fm(gam), vec_fm(sc[b]), vec_fm(sh[b])], axis=1)
        in_maps.append({"xT": fm(xT_full[:, i * 1024:(i + 1) * 1024]), "prm": np.ascontiguousarray(prm)})
    res = launch(nc, in_maps)
    hT = np.concatenate([r["hT"].transpose(1, 0, 2).reshape(2048, 1024) for r in res], axis=1)
    return hT


def build_P(nk, ncol, ntok):
    P = Prog()
    hT = P.dram("hT", [128, nk, ntok], BF16, "ExternalInput")
    w = P.dram("w", [128, nk, ncol], F32, "ExternalInput")
    out = P.dram("out", [ncol, ntok], F32, "ExternalOutput")
    wbf = P.sb([128, nk, ncol], BF16)
    wst = [P.sb([128, ncol], F32) for _ in range(2)]
    hts = [P.sb([128, nk, 512], BF16) for _ in range(2)]
    obs = [P.sb([128, 512], F32) for _ in range(4)]
    banks = [P.ps([128, 512]) for _ in range(6)]
    for k in range(nk):
        j = k % 2
        P.dma('sp' if j == 0 else 'pool', wst[j][:], w[:, k, :], w=[('wst', j)])
        eng = 'dve' if j == 0 else 'pool'
        P.op(eng, lambda e, k=k, j=j: e.tensor_copy(out=wbf[:, k, :], in_=wst[j][:]), r=[('wst', j)], w=[('wbf', k)])
    nct = (ncol + 127) // 128
    it = 0
    for t in range(ntok // 512):
        hb = t % 2
        P.dma('sp', hts[hb][:], hT[:, :, t * 512:(t + 1) * 512], w=[('ht', hb)])
        for ct in range(nct):
            c0 = ct * 128
            cw = min(128, ncol - c0)
            bank = banks[it % 6]
            ob = obs[it % 4]
            for k in range(nk):
                P.op('pe', lambda e, k=k, c0=c0, cw=cw, bank=bank, hb=hb: e.matmul(
                    bank[0:cw, :], lhsT=wbf[:, k, c0:c0 + cw], rhs=hts[hb][:, k, :], start=(k == 0), stop=(k == nk - 1)),
                    r=[('wbf', k), ('ht', hb)], w=[('bank', it % 6)])
            if it % 2 == 0:
                P.op('act', lambda e, cw=cw, bank=bank, ob=ob: e.activation(out=ob[0:cw, :], in_=bank[0:cw, :], func=AF.Copy),
                     r=[('bank', it % 6)], w=[('ob', it % 4)])
            else:
                P.op('dve', lambda e, cw=cw, bank=bank, ob=ob: e.tensor_copy(out=ob[0:cw, :], in_=bank[0:cw, :]),
                     r=[('bank', it % 6)], w=[('ob', it % 4)])
            P.dma('pool' if it % 2 == 0 else 'act', out[c0:c0 + cw, t * 512:(t + 1) * 512], ob[0:cw, :], r=[('ob', it % 4)])
            it += 1
    return P.build()


_cache = {}


def cached(key, fn):
    if key not in _cache:
        _cache[key] = fn()
    return _cache[key]


def run_P(hT_bf, W):
    K_, NC_ = W.shape
    ntok = hT_bf.shape[1]
    ncol = NC_ // NCORE
    assert ncol * NCORE == NC_
    nk = K_ // 128
    nc = cached(('P', nk, ncol, ntok), lambda: build_P(nk, ncol, ntok))
    hfm = fm(hT_bf)
    in_maps = [{"hT": hfm, "w": fm(W[:, i * ncol:(i + 1) * ncol])} for i in range(NCORE)]
    res = launch(nc, in_maps)
    return np.concatenate([r["out"] for r in res], axis=0)


S_ = 4096


def build_LRU():
    P = Prog()
    xin = P.dram("x", [128, 2 * S_], F32, "ExternalInput")
    gin = P.dram("g", [128, 2 * S_], F32, "ExternalInput")
    prm = P.dram("prm", [128, 11], F32, "ExternalInput")
    wri = P.dram("wri", [128, 4, 128], F32, "ExternalInput")
    out = P.dram("out", [128, 2 * S_], F32, "ExternalOutput")
    pt = P.sb([128, 11], F32)
    wt = P.sb([128, 4, 128], F32)
    nsp = P.sb([128, 4], F32)
    X, XC, R, I, M, HF, HB, G = [P.sb([128, S_], F32) for _ in range(8)]
    banks = [P.ps([128, 512]) for _ in range(4)]
    P.dma('sp', pt[:], prm, w=['pt'])
    P.dma('sp', wt[:], wri, w=['wt'])
    P.op('act', lambda e: e.activation(out=nsp[:, 0:2], in_=pt[:, 9:11], func=AF.Exp, scale=-1.0), r=['pt'], w=['nsp'])
    P.op('act', lambda e: e.activation(out=nsp[:, 0:2], in_=nsp[:, 0:2], func=AF.Ln, bias=1.0), r=['nsp'], w=['nsp'])
    P.op('dve', lambda e: e.tensor_scalar(out=nsp[:, 2:4], in0=nsp[:, 0:2], scalar1=-16.0, scalar2=None, op0=ALU.mult),
         r=['nsp'], w=['nsp'])
    P.op('dve', lambda e: e.tensor_scalar(out=nsp[:, 0:2], in0=nsp[:, 0:2], scalar1=-8.0, scalar2=None, op0=ALU.mult),
         r=['nsp'], w=['nsp'])
    bi = 0
    for b in range(2):
        bs = slice(b * S_, (b + 1) * S_)
        P.dma('sp', X[:], xin[:, bs], w=['X'])
        P.dma('pool', G[:], gin[:, bs], w=['G'])
        P.op('dve', lambda e: e.tensor_scalar(out=XC[:], in0=X[:], scalar1=pt[:, 2:3], scalar2=pt[:, 4:5],
                                              op0=ALU.mult, op1=ALU.add), r=['X', 'pt'], w=['XC'])
        for j in (0, 1, 3):
            t0 = max(0, 2 - j)
            t1 = min(S_, S_ + 2 - j)
            P.op('dve', lambda e, j=j, t0=t0, t1=t1: e.scalar_tensor_tensor(
                out=XC[:, t0:t1], in0=X[:, t0 + j - 2:t1 + j - 2], scalar=pt[:, j:j + 1], in1=XC[:, t0:t1],
                op0=ALU.mult, op1=ALU.add), r=['X', 'pt', 'XC'], w=['XC'])
        P.op('act', lambda e: e.activation(out=G[:], in_=G[:], func=AF.Gelu_apprx_tanh), r=['G'], w=['G'])
        for z in range(2):
            for t in range(S_ // 512):
                ts = slice(t * 512, (t + 1) * 512)
                for (widx, dst, bcol, nm) in ((z, R, 5 + z, 'R'), (2 + z, I, 7 + z, 'I')):
                    bank = banks[bi % 4]
                    P.op('pe', lambda e, widx=widx, ts=ts, bank=bank: e.matmul(
                        bank[:, :], lhsT=wt[:, widx, :], rhs=XC[:, ts], start=True, stop=True),
                        r=['wt', 'XC'], w=[('bank', bi % 4)])
                    P.op('act', lambda e, dst=dst, ts=ts, bank=bank, bcol=bcol: e.activation(
                        out=dst[:, ts], in_=bank[:, :], func=AF.Sigmoid, bias=pt[:, bcol:bcol + 1]),
                        r=[('bank', bi % 4), 'pt'], w=[nm])
                    bi += 1
            P.op('act', lambda e, z=z: e.activation(out=M[:], in_=R[:], func=AF.Exp, scale=nsp[:, 2 + z:3 + z]),
                 r=['R', 'nsp'], w=['M'])
            P.op('act', lambda e, z=z: e.activation(out=R[:], in_=R[:], func=AF.Exp, scale=nsp[:, z:z + 1]),
                 r=['R', 'nsp'], w=['R'])
            P.op('act', lambda e: e.activation(out=M[:], in_=M[:], func=AF.Sqrt, scale=-1.0, bias=1.0), r=['M'], w=['M'])
            P.op('dve', lambda e: e.tensor_tensor(out=M[:], in0=M[:], in1=I[:], op=ALU.mult), r=['M', 'I'], w=['M'])
            P.op('dve', lambda e: e.tensor_tensor(out=M[:], in0=M[:], in1=XC[:], op=ALU.mult), r=['M', 'XC'], w=['M'])
            if z == 0:
                P.op('dve', lambda e: e.tensor_tensor_scan(out=HF[:], data0=R[:], data1=M[:], initial=0.0,
                                                           op0=ALU.mult, op1=ALU.add), r=['R', 'M'], w=['HF'])
            else:
                P.op('dve', lambda e: e.tensor_tensor_scan(out=HB[:, ::-1], data0=R[:, ::-1], data1=M[:, ::-1], initial=0.0,
                                                           op0=ALU.mult, op1=ALU.add), r=['R', 'M'], w=['HB'])
        P.op('dve', lambda e: e.tensor_tensor(out=HF[:], in0=HF[:], in1=HB[:], op=ALU.add), r=['HF', 'HB'], w=['HF'])
        P.op('dve', lambda e: e.tensor_tensor(out=HF[:], in0=HF[:], in1=G[:], op=ALU.mult), r=['HF', 'G'], w=['HF'])
        P.dma('sp', out[:, bs], HF[:], r=['HF'])
    return P.build()


def run_LRU(pT, inp, l):
    nc = cached('LRU', build_LRU)
    in_maps = []
    for i in range(NCORE):
        cs = slice(i * 128, (i + 1) * 128)
        prm = np.concatenate([inp["lru_conv_w"][l][:, cs].T, inp["lru_conv_b"][l][cs][:, None],
                              inp["lru_b_r"][l][:, cs].T, inp["lru_b_i"][l][:, cs].T, inp["lru_lambda"][l][:, cs].T], axis=1)
        wri = np.stack([inp["lru_w_r"][l][0, i], inp["lru_w_r"][l][1, i], inp["lru_w_i"][l][0, i], inp["lru_w_i"][l][1, i]], axis=1)
        in_maps.append({"x": np.ascontiguousarray(pT[i * 128:(i + 1) * 128]),
                        "g": np.ascontiguousarray(pT[1024 + i * 128:1024 + (i + 1) * 128]),
                        "prm": np.ascontiguousarray(prm.astype(np.float32)), "wri": np.ascontiguousarray(wri)})
    res = launch(nc, in_maps)
    return np.concatenate([r["out"] for r in res], axis=0)


def build_SGU():
    P = Prog()
    uT = P.dram("uT", [128, 8, 1024], F32, "ExternalInput")
    vtm = P.dram("vtm", [128, 8, 1024], F32, "ExternalInput")
    ng = P.dram("ng", [1, 1024], F32, "ExternalInput")
    wsT = P.dram("wsT", [128, 8, 128], F32, "ExternalInput")
    bs = P.dram("bs", [1, 1024], F32, "ExternalInput")
    out = P.dram("out", [128, 8, 1024], F32, "ExternalOutput")
    U = P.sb([128, 8, 1024], F32)
    V = P.sb([128, 8, 1024], F32)
    NG = P.sb([128, 1024], F32)
    BS = P.sb([128, 8, 128], F32)
    WS = P.sb([128, 8, 128], F32)
    SQ = P.sb([128, 1024], F32)
    st = P.sb([128, 8, 8], F32)
    O = P.sb([128, 8, 1024], F32)
    banks = [P.ps([128, 512]) for _ in range(4)]
    mk_consts(P)
    P.dma('sp', U[:], uT, w=['U'])
    P.dma('pool', V[:], vtm, w=[('V', n) for n in range(8)])
    P.dma('sp', NG[:], ng.partition_broadcast(128), w=['NG'])
    P.dma('sp', BS[:].rearrange("p g i -> p (g i)"), bs.partition_broadcast(128), w=['BS'])
    P.dma('sp', WS[:], wsT, w=['WS'])
    P.op('act', lambda e: e.activation(out=U[:].rearrange("p g t -> p (g t)"), in_=U[:].rearrange("p g t -> p (g t)"),
                                       func=AF.Gelu_apprx_tanh), r=['U'], w=['U'])
    bi = 0
    for n in range(8):
        Vn = V[:, n, :]
        s = st[:, n, :]
        P.op('act', lambda e, Vn=Vn, s=s: e.activation(out=Vn, in_=Vn, func=AF.Gelu_apprx_tanh, accum_out=s[:, 0:1]),
             r=[('V', n)], w=[('V', n), ('st', n)])
        P.op('act', lambda e, Vn=Vn, s=s: e.activation(out=SQ[:], in_=Vn, func=AF.Square, accum_out=s[:, 1:2]),
             r=[('V', n), ('st', n)], w=['SQ', ('st', n)])
        P.op('dve', lambda e, s=s: e.tensor_scalar(out=s[:, 2:3], in0=s[:, 0:1], scalar1=1.0 / 1024, scalar2=None, op0=ALU.mult),
             r=[('st', n)], w=[('st', n)])
        P.op('dve', lambda e, s=s: e.tensor_tensor(out=s[:, 3:4], in0=s[:, 2:3], in1=s[:, 2:3], op=ALU.mult),
             r=[('st', n)], w=[('st', n)])
        P.op('dve', lambda e, s=s: e.scalar_tensor_tensor(out=s[:, 4:5], in0=s[:, 1:2], scalar=1.0 / 1024, in1=s[:, 3:4],
                                                         op0=ALU.mult, op1=ALU.subtract), r=[('st', n)], w=[('st', n)])
        P.op('act', lambda e, s=s: e.activation(out=s[:, 5:6], in_=s[:, 4:5], func=AF.Sqrt, bias=epsb[0][:]),
             r=[('st', n), 'epsb'], w=[('st', n)])
        P.op('dve', lambda e, s=s: e.reciprocal(out=s[:, 5:6], in_=s[:, 5:6]), r=[('st', n)], w=[('st', n)])
        P.op('dve', lambda e, Vn=Vn, s=s: e.tensor_scalar(out=Vn, in0=Vn, scalar1=s[:, 2:3], scalar2=s[:, 5:6],
                                                        op0=ALU.subtract, op1=ALU.mult), r=[('V', n), ('st', n)], w=[('V', n)])
        P.op('dve', lambda e, Vn=Vn: e.tensor_tensor(out=Vn, in0=Vn, in1=NG[:], op=ALU.mult), r=[('V', n), 'NG'], w=[('V', n)])
        for gh in range(2):
            bank = banks[bi % 4]
            for gg in range(4):
                g = gh * 4 + gg
                P.op('pe', lambda e, g=g, gg=gg, n=n, bank=bank: e.matmul(
                    bank[:, gg * 128:(gg + 1) * 128], lhsT=V[:, n, g * 128:(g + 1) * 128], rhs=WS[:, g, :],
                    start=True, stop=True), r=[('V', n), 'WS'], w=[('bank', bi % 4, gg)])
            Ov = O[:, gh * 4:(gh + 1) * 4, n * 128:(n + 1) * 128]
            P.op('dve', lambda e, bank=bank, gh=gh, Ov=Ov: e.tensor_tensor(
                out=Ov, in0=bank[:, :].rearrange("p (g i) -> p g i", g=4), in1=BS[:, gh * 4:(gh + 1) * 4, :], op=ALU.add),
                r=[('bank', bi % 4, gg) for gg in range(4)] + ['BS'], w=[('O', n, gh)])
            P.op('pool', lambda e, gh=gh, n=n, Ov=Ov: e.tensor_tensor(
                out=Ov, in0=Ov, in1=U[:, gh * 4:(gh + 1) * 4, n * 128:(n + 1) * 128], op=ALU.mult),
                r=[('O', n, gh), 'U'], w=[('O', n, gh)])
            bi += 1
    P.dma('sp', out, O[:], r=[('O', n, gh) for n in range(8) for gh in range(2)])
    return P.build()


def run_SGU(pT, inp, l):
    nc = cached('SGU', lambda: (mk_dummy(), build_SGU())[1])
    in_maps = []
    wsT = np.ascontiguousarray(inp["sgu_w"][l].transpose(2, 0, 1))
    for i in range(NCORE):
        ts = slice(i * 1024, (i + 1) * 1024)
        uT = pT[5120:6144, ts].reshape(8, 128, 1024).transpose(1, 0, 2)
        vtm = pT[6144:7168, ts].T.reshape(8, 128, 1024).transpose(1, 0, 2)
        in_maps.append({"uT": np.ascontiguousarray(uT), "vtm": np.ascontiguousarray(vtm),
                        "ng": np.ascontiguousarray(inp["sgu_norm_g"][l][None, :]), "wsT": wsT,
                        "bs": np.ascontiguousarray(inp["sgu_b"][l].reshape(1, 1024))})
    res = launch(nc, in_maps)
    return np.concatenate([r["out"].transpose(1, 0, 2).reshape(1024, 1024) for r in res], axis=1)


def mk_dummy():
    pass


TWO_PI = 6.283185


def build_RET():
    P = Prog()
    qk = P.dram("qk", [32, 4, 2 * S_], F32, "ExternalInput")
    vtm = P.dram("vtm", [128, 64, 128], F32, "ExternalInput")
    gtm = P.dram("gtm", [128, 64, 128], F32, "ExternalInput")
    pos = P.dram("pos", [1, 2 * S_], I32, "ExternalInput")
    c_intra = P.dram("c_intra", [128, 128], F32, "ExternalInput")
    c_decq = P.dram("c_decq", [32, 2, 2, 128], F32, "ExternalInput")
    c_vdec = P.dram("c_vdec", [128, 2], F32, "ExternalInput")
    c_misc = P.dram("c_misc", [32, 4], F32, "ExternalInput")
    idd = P.dram("idd", [128, 128], F32, "ExternalInput")
    out = P.dram("out", [128, 64, 128], F32, "ExternalOutput")
    mk_consts(P)
    QK = P.sb([32, 4, S_], F32)
    V = P.sb([128, 32, 128], F32)
    G = P.sb([128, 32, 128], F32)
    ST = P.sb([32, 32, 2, 256], F32)
    INTRA = P.sb([128, 128], F32)
    DECQ = P.sb([32, 2, 2, 128], F32)
    VDEC = P.sb([128, 2], F32)
    MISC = P.sb([32, 4], F32)
    IDN = P.sb([128, 128], F32)
    SEG = 1024
    PI = P.sb([32, SEG], I32)
    T0, T1, T2, SN, CS, TA, TB = [P.sb([32, SEG], F32) for _ in range(7)]
    KTM = [P.sb([128, 64], F32) for _ in range(2)]
    VFB = [P.sb([128, 256], F32) for _ in range(2)]
    SCT = [P.sb([128, 128], F32) for _ in range(2)]
    QFB = [P.sb([32, 2, 2, 128], F32) for _ in range(2)]
    OS = [P.sb([128, 128], F32) for _ in range(2)]
    SQ = P.sb([128, 128], F32)
    stt = P.sb([128, 64, 8], F32)
    bk = [P.ps([128, 512]) for _ in range(6)]
    for (t_, d_, k_) in ((INTRA, c_intra, 'INTRA'), (DECQ, c_decq, 'DECQ'), (VDEC, c_vdec, 'VDEC'), (MISC, c_misc, 'MISC'),
                        (IDN, idd, 'IDN')):
        P.dma('sp', t_[:], d_, w=[k_])
    for b in range(2):
        P.dma('sp', QK[:], qk[:, :, b * S_:(b + 1) * S_], w=[('QK', s) for s in range(4)])
        P.dma('pool', V[:], vtm[:, b * 32:(b + 1) * 32, :], w=['V'])
        P.dma('pool', G[:], gtm[:, b * 32:(b + 1) * 32, :], w=[('G', n) for n in range(32)])
        P.op('act', lambda e: e.activation(out=G[:].rearrange("p n e -> p (n e)"), in_=G[:].rearrange("p n e -> p (n e)"),
                                           func=AF.Silu), r=[('G', n) for n in range(32)], w=[('G', n) for n in range(32)])
        for s in range(S_ // SEG):
            ss = slice(s * SEG, (s + 1) * SEG)
            P.dma('sp', PI[:], pos[:, b * S_ + s * SEG: b * S_ + (s + 1) * SEG].partition_broadcast(32), w=['PI'])
            P.op('dve', lambda e: e.tensor_copy(out=T0[:], in_=PI[:]), r=['PI'], w=['T0'])
            P.op('dve', lambda e: e.tensor_scalar(out=T0[:], in0=T0[:], scalar1=MISC[:, 0:1], scalar2=MISC[:, 1:2],
                                                  op0=ALU.mult, op1=ALU.mult), r=['T0', 'MISC'], w=['T0'])
            for (dst, shift) in ((SN, 0.0), (CS, 0.25)):
                nm = 'SN' if dst is SN else 'CS'
                P.op('dve', lambda e, shift=shift: e.tensor_scalar(out=T1[:], in0=T0[:], scalar1=shift, scalar2=None, op0=ALU.add),
                     r=['T0'], w=['T1'])
                P.op('dve', lambda e: e.tensor_copy(out=PI[:], in_=T1[:]), r=['T1'], w=['PI'])
                P.op('dve', lambda e: e.tensor_copy(out=T2[:], in_=PI[:]), r=['PI'], w=['T2'])
                P.op('dve', lambda e: e.tensor_tensor(out=T1[:], in0=T1[:], in1=T2[:], op=ALU.subtract), r=['T1', 'T2'], w=['T1'])
                P.op('dve', lambda e: e.tensor_scalar(out=T2[:], in0=T1[:], scalar1=0.5, scalar2=None, op0=ALU.is_gt),
                     r=['T1'], w=['T2'])
                P.op('dve', lambda e: e.tensor_tensor(out=T1[:], in0=T1[:], in1=T2[:], op=ALU.subtract), r=['T1', 'T2'], w=['T1'])
                P.op('dve', lambda e: e.tensor_scalar(out=T2[:], in0=T1[:], scalar1=-0.5, scalar2=None, op0=ALU.is_lt),
                     r=['T1'], w=['T2'])
                P.op('dve', lambda e: e.tensor_tensor(out=T1[:], in0=T1[:], in1=T2[:], op=ALU.add), r=['T1', 'T2'], w=['T1'])
                P.op('act', lambda e, dst=dst: e.activation(out=dst[:], in_=T1[:], func=AF.Sin, scale=TWO_PI), r=['T1'], w=[nm])
            for base in (0, 2):
                x1 = QK[:, base, ss]
                x2 = QK[:, base + 1, ss]
                k1, k2 = ('QK', base), ('QK', base + 1)
                eng = 'dve' if base == 0 else 'pool'
                ta, tb = (TA, TB) if base == 0 else (T0, T2)
                kta, ktb = ('TA', 'TB') if base == 0 else ('T0', 'T2')
                P.op(eng, lambda e, x1=x1, ta=ta: e.tensor_tensor(out=ta[:], in0=x1, in1=CS[:], op=ALU.mult), r=[k1, 'CS'], w=[kta])
                P.op(eng, lambda e, x2=x2, tb=tb: e.tensor_tensor(out=tb[:], in0=x2, in1=SN[:], op=ALU.mult), r=[k2, 'SN'], w=[ktb])
                P.op(eng, lambda e, ta=ta, tb=tb: e.tensor_tensor(out=ta[:], in0=ta[:], in1=tb[:], op=ALU.subtract), r=[kta, ktb], w=[kta])
                P.op(eng, lambda e, x1=x1, tb=tb: e.tensor_tensor(out=tb[:], in0=x1, in1=SN[:], op=ALU.mult), r=[k1, 'SN'], w=[ktb])
                P.op(eng, lambda e, x2=x2: e.tensor_tensor(out=x2, in0=x2, in1=CS[:], op=ALU.mult), r=[k2, 'CS'], w=[k2])
                P.op(eng, lambda e, x2=x2, tb=tb: e.tensor_tensor(out=x2, in0=x2, in1=tb[:], op=ALU.add), r=[k2, ktb], w=[k2])
                P.op(eng, lambda e, x1=x1, ta=ta: e.tensor_copy(out=x1, in_=ta[:]), r=[kta], w=[k1])
        P.op('pool', lambda e: e.memset(ST[:, 0, :, 0:128], 0.0), w=[('ST', 0)])
        P.op('pool', lambda e: e.memset(ST[:, 31, :, 128:256], 0.0), w=[('STB', 31)])
        it = 0
        for n in range(32):
            cs = slice(n * 128, (n + 1) * 128)
            bank = bk[it % 2]
            bkk = ('bk', it % 2)
            ktm = KTM[it % 2]
            vfb = VFB[it % 2]
            P.op('pe', lambda e, bank=bank, cs=cs: e.transpose(out=bank[:, 0:32], in_=QK[:, 2, cs], identity=IDN[0:32, 0:32]),
                 r=[('QK', 2), 'IDN'], w=[bkk])
            P.op('pe', lambda e, bank=bank, cs=cs: e.transpose(out=bank[:, 32:64], in_=QK[:, 3, cs], identity=IDN[0:32, 0:32]),
                 r=[('QK', 3), 'IDN'], w=[bkk])
            P.op('act', lambda e, bank=bank, ktm=ktm: e.activation(out=ktm[:], in_=bank[:, 0:64], func=AF.Copy),
                 r=[bkk], w=[('ktm', it % 2)])
            P.op('dve', lambda e, n=n, vfb=vfb: e.tensor_scalar(out=vfb[:, 0:128], in0=V[:, n, :], scalar1=VDEC[:, 0:1], scalar2=None,
                                                              op0=ALU.mult), r=['V', 'VDEC'], w=[('vfb', it % 2)])
            P.op('dve', lambda e, n=n, vfb=vfb: e.tensor_scalar(out=vfb[:, 128:256], in0=V[:, n, :], scalar1=VDEC[:, 1:2], scalar2=None,
                                                              op0=ALU.mult), r=['V', 'VDEC', ('vfb', it % 2)], w=[('vfb', it % 2)])
            b2 = bk[2 + it % 2]
            b2k = ('bk', 2 + it % 2)
            for h in range(2):
                P.op('pe', lambda e, h=h, b2=b2, ktm=ktm, vfb=vfb: e.matmul(
                    b2[0:32, h * 256:(h + 1) * 256], lhsT=ktm[:, h * 32:(h + 1) * 32], rhs=vfb[:], start=True, stop=True),
                    r=[('ktm', it % 2), ('vfb', it % 2)], w=[b2k])
            if n < 31:
                P.op('act', lambda e, n=n, b2=b2: e.activation(
                    out=ST[:, n + 1, :, 0:128], in_=b2[0:32, :].rearrange("p (h x) -> p h x", h=2)[:, :, 0:128], func=AF.Copy),
                    r=[b2k], w=[('ST', n + 1)])
            if n > 0:
                P.op('act', lambda e, n=n, b2=b2: e.activation(
                    out=ST[:, n - 1, :, 128:256], in_=b2[0:32, :].rearrange("p (h x) -> p h x", h=2)[:, :, 128:256], func=AF.Copy),
                    r=[b2k], w=[('STB', n - 1)])
            it += 1
        for n in range(1, 32):
            P.op('dve', lambda e, n=n: e.scalar_tensor_tensor(
                out=ST[:, n, :, 0:128], in0=ST[:, n - 1, :, 0:128], scalar=MISC[:, 2:3], in1=ST[:, n, :, 0:128],
                op0=ALU.mult, op1=ALU.add), r=[('ST', n - 1), ('ST', n), 'MISC'], w=[('ST', n)])
        for n in range(30, -1, -1):
            P.op('dve', lambda e, n=n: e.scalar_tensor_tensor(
                out=ST[:, n, :, 128:256], in0=ST[:, n + 1, :, 128:256], scalar=MISC[:, 2:3], in1=ST[:, n, :, 128:256],
                op0=ALU.mult, op1=ALU.add), r=[('STB', n + 1), ('STB', n), 'MISC'], w=[('STB', n)])
        for n in range(32):
            cs = slice(n * 128, (n + 1) * 128)
            j = n % 2
            bs_, bsk = bk[j], ('bk', j)
            P.op('pe', lambda e, bs_=bs_, cs=cs: e.matmul(bs_[:, 0:128], lhsT=QK[:, 2, cs], rhs=QK[:, 0, cs], start=True, stop=False),
                 r=[('QK', 0), ('QK', 2)], w=[bsk])
            P.op('pe', lambda e, bs_=bs_, cs=cs: e.matmul(bs_[:, 0:128], lhsT=QK[:, 3, cs], rhs=QK[:, 1, cs], start=False, stop=True),
                 r=[('QK', 1), ('QK', 3)], w=[bsk])
            P.op('dve', lambda e, bs_=bs_, j=j: e.tensor_tensor(out=SCT[j][:], in0=bs_[:, 0:128], in1=INTRA[:], op=ALU.mult),
                 r=[bsk, 'INTRA'], w=[('sct', j)])
            for dr in range(2):
                P.op('pool', lambda e, dr=dr, j=j, cs=cs: e.tensor_tensor(out=QFB[j][:, dr, :, :], in0=QK[:, 0:2, cs],
                                                                        in1=DECQ[:, dr, :, :], op=ALU.mult),
                     r=[('QK', 0), ('QK', 1), 'DECQ'], w=[('qfb', j, dr)])
            bo, bok = bk[4 + j], ('bk', 4 + j)
            P.op('pe', lambda e, bo=bo, j=j, n=n: e.matmul(bo[:, 0:128], lhsT=SCT[j][:], rhs=V[:, n, :], start=True, stop=False),
                 r=[('sct', j), 'V'], w=[bok])
            for dr in range(2):
                for h in range(2):
                    last = (dr == 1 and h == 1)
                    P.op('pe', lambda e, bo=bo, j=j, n=n, dr=dr, h=h, last=last: e.matmul(
                        bo[:, 0:128], lhsT=QFB[j][:, dr, h, :], rhs=ST[:, n, h, dr * 128:(dr + 1) * 128], start=False, stop=last),
                        r=[('qfb', j, dr), ('ST', n), ('STB', n)], w=[bok])
            s = stt[:, b * 32 + n, :]
            sk = ('stt', b * 32 + n)
            P.op('act', lambda e, bo=bo, j=j, s=s: e.activation(out=OS[j][:], in_=bo[:, 0:128], func=AF.Copy, accum_out=s[:, 0:1]),
                 r=[bok], w=[('os', j), sk])
            P.op('act', lambda e, j=j, s=s: e.activation(out=SQ[:], in_=OS[j][:], func=AF.Square, accum_out=s[:, 1:2]),
                 r=[('os', j), sk], w=['SQ', sk])
            P.op('dve', lambda e, s=s: e.tensor_scalar(out=s[:, 2:3], in0=s[:, 0:1], scalar1=1.0 / 128, scalar2=None, op0=ALU.mult),
                 r=[sk], w=[sk])
            P.op('dve', lambda e, s=s: e.tensor_tensor(out=s[:, 3:4], in0=s[:, 2:3], in1=s[:, 2:3], op=ALU.mult), r=[sk], w=[sk])
            P.op('dve', lambda e, s=s: e.scalar_tensor_tensor(out=s[:, 4:5], in0=s[:, 1:2], scalar=1.0 / 128, in1=s[:, 3:4],
                                                             op0=ALU.mult, op1=ALU.subtract), r=[sk], w=[sk])
            P.op('act', lambda e, s=s: e.activation(out=s[:, 5:6], in_=s[:, 4:5], func=AF.Sqrt, bias=epsb[0][:]),
                 r=[sk, 'epsb'], w=[sk])
            P.op('dve', lambda e, s=s: e.reciprocal(out=s[:, 5:6], in_=s[:, 5:6]), r=[sk], w=[sk])
            P.op('dve', lambda e, j=j, s=s: e.tensor_scalar(out=OS[j][:], in0=OS[j][:], scalar1=s[:, 2:3], scalar2=s[:, 5:6],
                                                          op0=ALU.subtract, op1=ALU.mult), r=[('os', j), sk], w=[('os', j)])
            P.op('dve', lambda e, j=j, n=n: e.tensor_tensor(out=G[:, n, :], in0=G[:, n, :], in1=OS[j][:], op=ALU.mult),
                 r=[('os', j), ('G', n)], w=[('G', n)])
        P.dma('sp', out[:, b * 32:(b + 1) * 32, :], G[:], r=[('G', n) for n in range(32)])
    return P.build()


def ret_consts(h):
    C = 128
    lg = np.log1p(-np.exp2(-5.0 - h))
    i = np.arange(C)
    intra = 0.125 * np.exp(np.abs(i[:, None] - i[None, :]) * lg)
    decq = np.zeros((32, 2, 2, C), np.float64)
    decq[:, 0, :, :] = 0.125 * np.exp((i + 1.0) * lg)
    decq[:, 1, :, :] = 0.125 * np.exp((C - i) * lg)
    vdec = np.stack([np.exp((C - 1 - i) * lg), np.exp(i * lg)], axis=1)
    inv_freq = (10000.0 ** (-np.arange(0, 64, 2, dtype=np.float32) / 64)).astype(np.float32)
    misc = np.zeros((32, 4), np.float32)
    misc[:, 0] = inv_freq
    misc[:, 1] = 1.0 / (2 * np.pi)
    misc[:, 2] = np.exp(C * lg)
    return (intra.astype(np.float32), decq.astype(np.float32), vdec.astype(np.float32), misc)


def tm_chunks(aT):
    return np.ascontiguousarray(aT.T.reshape(64, 128, aT.shape[0]).transpose(1, 0, 2))


def run_RET(pT, positions):
    nc = cached('RET', build_RET)
    in_maps = []
    idd = np.eye(128, dtype=np.float32)
    posr = np.ascontiguousarray(positions.reshape(1, -1).astype(np.int32))
    for i in range(NCORE):
        q = pT[2048 + i * 64:2048 + (i + 1) * 64]
        k = pT[2560 + i * 64:2560 + (i + 1) * 64]
        qk = np.stack([q[0:32], q[32:64], k[0:32], k[32:64]], axis=1)
        intra, decq, vdec, misc = ret_consts(i)
        in_maps.append({"qk": np.ascontiguousarray(qk), "vtm": tm_chunks(pT[3072 + i * 128:3072 + (i + 1) * 128]),
                        "gtm": tm_chunks(pT[4096 + i * 128:4096 + (i + 1) * 128]), "pos": posr,
                        "c_intra": intra, "c_decq": decq, "c_vdec": vdec, "c_misc": misc, "idd": idd})
    res = launch(nc, in_maps)
    return np.concatenate([r["out"].transpose(2, 1, 0).reshape(128, 8192) for r in res], axis=0)


def build_M():
    P = Prog()
    ysT = P.dram("ysT", [128, 4, 8, 1024], F32, "ExternalInput")
    lgT = P.dram("lgT", [128, 16, 4, 1024], F32, "ExternalInput")
    xT = P.dram("xT", [128, 16, 1024], F32, "ExternalInput")
    g1 = P.dram("g1", [128, 16], F32, "ExternalInput")
    wb = P.dram("wb", [128, 4, 8, 2048], F32, "ExternalInput")
    wo = P.dram("wo", [128, 16, 2048], F32, "ExternalInput")
    out = P.dram("out", [128, 16, 1024], F32, "ExternalOutput")
    X = P.sb([128, 16, 512], F32)
    YS = P.sb([128, 4, 8, 512], BF16)
    YST = [P.sb([128, 8, 512], F32) for _ in range(2)]
    MG = P.sb([128, 16, 512], BF16)
    G1 = P.sb([128, 16], F32)
    WBS = [P.sb([128, 4, 8, 128], F32) for _ in range(2)]
    WBB = [P.sb([128, 4, 8, 128], BF16) for _ in range(2)]
    LG = [P.sb([128, 4, 512], F32) for _ in range(2)]
    ACC = P.sb([128, 512], F32)
    TMP = P.sb([128, 512], F32)
    WOS = [P.sb([128, 16, 128], F32) for _ in range(2)]
    WOB = [P.sb([128, 16, 128], BF16) for _ in range(2)]
    banks = [P.ps([128, 512]) for _ in range(4)]
    P.dma('sp', G1[:], g1, w=['G1'])
    bi = 0
    for hf in range(2):
        hs = slice(hf * 512, (hf + 1) * 512)
        P.dma('sp', X[:], xT[:, :, hs], w=['X'])
        for n in range(4):
            P.dma('pool', YST[n % 2][:], ysT[:, n, :, hs], w=[('yst', n % 2)])
            P.op('dve' if n % 2 == 0 else 'pool', lambda e, n=n: e.tensor_copy(out=YS[:, n, :, :], in_=YST[n % 2][:]),
                 r=[('yst', n % 2)], w=[('YS', n)])
        for dt in range(16):
            j = dt % 2
            P.dma('sp', WBS[j][:], wb[:, :, :, dt * 128:(dt + 1) * 128], w=[('wbs', j)])
            P.op('pool', lambda e, j=j: e.tensor_copy(out=WBB[j][:].rearrange("p n c d -> p (n c d)"),
                                                      in_=WBS[j][:].rearrange("p n c d -> p (n c d)")),
                 r=[('wbs', j)], w=[('wbb', j)])
            P.dma('act', LG[j][:], lgT[:, dt, :, hs], w=[('lg', j)])
            P.op('act', lambda e, j=j: e.activation(out=LG[j][:].rearrange("p n t -> p (n t)"),
                                                    in_=LG[j][:].rearrange("p n t -> p (n t)"), func=AF.Sigmoid),
                 r=[('lg', j)], w=[('lg', j)])
            for n in range(4):
                bank = banks[bi % 4]
                bkk = ('bank', bi % 4)
                for c in range(8):
                    P.op('pe', lambda e, n=n, c=c, j=j, bank=bank: e.matmul(
                        bank[:, :], lhsT=WBB[j][:, n, c, :], rhs=YS[:, n, c, :], start=(c == 0), stop=(c == 7)),
                        r=[('wbb', j), ('YS', n)], w=[bkk])
                if n == 0:
                    P.op('dve', lambda e, j=j, bank=bank: e.tensor_tensor(out=ACC[:], in0=bank[:, :], in1=LG[j][:, 0, :], op=ALU.mult),
                         r=[bkk, ('lg', j)], w=['ACC'])
                else:
                    P.op('dve', lambda e, j=j, n=n, bank=bank: e.tensor_tensor(out=TMP[:], in0=bank[:, :], in1=LG[j][:, n, :],
                                                                             op=ALU.mult), r=[bkk, ('lg', j)], w=['TMP'])
                    if n < 3:
                        P.op('dve', lambda e: e.tensor_tensor(out=ACC[:], in0=ACC[:], in1=TMP[:], op=ALU.add),
                             r=['ACC', 'TMP'], w=['ACC'])
                    else:
                        P.op('dve', lambda e, dt=dt: e.tensor_tensor(out=MG[:, dt, :], in0=ACC[:], in1=TMP[:], op=ALU.add),
                             r=['ACC', 'TMP'], w=[('MG', dt)])
                bi += 1
        for dp in range(16):
            j = dp % 2
            P.dma('sp', WOS[j][:], wo[:, :, dp * 128:(dp + 1) * 128], w=[('wos', j)])
            P.op('pool', lambda e, j=j: e.tensor_copy(out=WOB[j][:].rearrange("p c d -> p (c d)"),
                                                      in_=WOS[j][:].rearrange("p c d -> p (c d)")),
                 r=[('wos', j)], w=[('wob', j)])
            bank = banks[bi % 4]
            bkk = ('bank', bi % 4)
            for c in range(16):
                P.op('pe', lambda e, c=c, j=j, bank=bank: e.matmul(bank[:, :], lhsT=WOB[j][:, c, :], rhs=MG[:, c, :],
                                                                 start=(c == 0), stop=(c == 15)),
                     r=[('wob', j), ('MG', c)], w=[bkk])
            P.op('dve', lambda e, dp=dp, bank=bank: e.scalar_tensor_tensor(
                out=X[:, dp, :], in0=bank[:, :], scalar=G1[:, dp:dp + 1], in1=X[:, dp, :], op0=ALU.mult, op1=ALU.add),
                r=[bkk, 'G1', 'X'], w=[('XO', dp)])
            bi += 1
        P.dma('sp', out[:, :, hs], X[:], r=[('XO', dp) for dp in range(16)] + ['X'], w=['X'])
    return P.build()


def run_M(ysT4, pT, xT_full, g1, w_branch, w_out):
    nc = cached('M', build_M)
    wb = np.ascontiguousarray(w_branch.reshape(4, 8, 128, 2048).transpose(2, 0, 1, 3))
    wo = fm(w_out)
    in_maps = []
    for i in range(NCORE):
        ts = slice(i * 1024, (i + 1) * 1024)
        ys = ysT4[:, :, ts].reshape(4, 8, 128, 1024).transpose(2, 0, 1, 3)
        lg = pT[10624:18816, ts].reshape(4, 16, 128, 1024).transpose(2, 1, 0, 3)
        in_maps.append({"ysT": np.ascontiguousarray(ys), "lgT": np.ascontiguousarray(lg), "xT": fm(xT_full[:, ts]),
                        "g1": vec_fm(g1[i // 4]), "wb": wb, "wo": wo})
    res = launch(nc, in_maps)
    return np.concatenate([r["out"].transpose(1, 0, 2).reshape(2048, 1024) for r in res], axis=1)


def build_E():
    P = Prog()
    xT = P.dram("xT", [128, 16, 1024], F32, "ExternalInput")
    prm = P.dram("prm", [128, 4, 16], F32, "ExternalInput")
    wr = P.dram("wr", [128, 16, 36], F32, "ExternalInput")
    rb = P.dram("rb", [1, 36], F32, "ExternalInput")
    idd = P.dram("idd", [128, 128], F32, "ExternalInput")
    wgu = P.dram("wgu", [32, 128, 16, 1024], F32, "ExternalInput")
    wdn = P.dram("wdn", [32, 128, 4, 2048], F32, "ExternalInput")
    wscr = P.dram("wscr", [32, 1024], F32, "ExternalOutput")
    out = P.dram("out", [128, 16, 1024], F32, "ExternalOutput")
    ones = mk_consts(P)
    XA = P.sb([128, 16, 1024], F32)
    H2B = P.sb([128, 16, 1024], BF16)
    H2F = P.sb([128, 16 * 512], F32)
    H2Fv = H2F[:].rearrange("p (k t) -> p k t", k=16)
    RSTD = P.sb([128, 1024], F32)
    TMP = P.sb([128, 2, 512], F32)
    PT = P.sb([128, 4, 16], F32)
    WR = P.sb([128, 16, 36], F32)
    RB = P.sb([128, 36], F32)
    IDN = P.sb([128, 128], F32)
    L = P.sb([128, 8, 36], F32)
    sm = P.sb([128, 8, 16], F32)
    OH = P.sb([128, 4], F32)
    GE = P.sb([128, 4], F32)
    IG = P.sb([128, 8], F32)
    IG2 = P.sb([128, 8], F32)
    M1 = P.sb([128, 8], F32)
    M2 = P.sb([128, 8], F32)
    WM = P.sb([128, 8], F32)
    WF = P.sb([128, 8, 32], F32)
    WGT = P.sb([32, 1024], F32)
    WB = [P.sb([128, 1024], F32) for _ in range(2)]
    GUB = [[P.sb([128, 16, 128], BF16) for _ in range(2)] for _ in range(2)]
    TMPA = [P.sb([128, 512], F32) for _ in range(2)]
    ACTB = P.sb([128, 4, 1024], BF16)
    WDB = [P.sb([128, 4, 512], BF16) for _ in range(2)]
    GS = [H2F[:, 0:2048].rearrange("p (k j) -> p k j", k=16), H2F[:, 2048:4096].rearrange("p (k j) -> p k j", k=16)]
    WDS = H2F[:, 4096:6144].rearrange("p (c d) -> p c d", c=4)
    XR = H2F[:, 6144:7168]
    banks = [P.ps([128, 512]) for _ in range(8)]
    P.dma('sp', PT[:], prm, w=[('n', 'gam'), ('n', 'sc'), ('n', 'sh'), 'G2'])
    P.dma('sp', WR[:], wr, w=['WR'])
    P.dma('sp', RB[:], rb.partition_broadcast(128), w=['RB'])
    P.dma('sp', IDN[:], idd, w=['IDN'])
    for k in range(16):
        P.dma('sp' if k % 2 == 0 else 'pool', XA[:, k, :], xT[:, k, :], w=[('n', 'x', k)])
    h2f_keys = []

    def after_tile(t):
        for tt in range(4):
            bank = banks[2 + (tt % 2)]
            bkk = ('bk', 2 + (tt % 2))
            for k in range(16):
                P.op('pe', lambda e, k=k, tt=tt, bank=bank: e.matmul(
                    bank[:, 0:36], lhsT=H2Fv[:, k, tt * 128:(tt + 1) * 128], rhs=WR[:, k, :], start=(k == 0), stop=(k == 15)),
                    r=[('n', 'h', 1, t), 'WR'], w=[bkk])
            P.op('dve', lambda e, tt=tt, bank=bank, t=t: e.tensor_tensor(out=L[:, t * 4 + tt, :], in0=bank[:, 0:36], in1=RB[:],
                                                                       op=ALU.add), r=[bkk, 'RB'], w=[('L', t * 4 + tt)])
    emit_norm_cb(P, XA, 16, 1024, PT[:, 0, :], PT[:, 1, :], PT[:, 2, :], ones, banks[0:2], RSTD, TMP,
                 [(H2B, False), (H2Fv, True)], 'n', after_tile)
    for tt in range(8):
        s = sm[:, tt, :]
        sk = ('sm', tt)
        lg = L[:, tt, 0:4]
        Lk = ('L', tt)
        P.op('dve', lambda e, lg=lg, s=s: e.tensor_reduce(out=s[:, 0:1], in_=lg, axis=AX.X, op=ALU.max), r=[Lk], w=[sk])
        P.op('dve', lambda e, lg=lg, s=s: e.tensor_scalar(out=OH[:], in0=lg, scalar1=s[:, 0:1], scalar2=None, op0=ALU.is_ge),
             r=[Lk, sk], w=['OH'])
        P.op('dve', lambda e, s=s: e.tensor_scalar(out=s[:, 1:2], in0=s[:, 0:1], scalar1=-1.0, scalar2=None, op0=ALU.mult),
             r=[sk], w=[sk])
        P.op('act', lambda e, lg=lg, s=s: e.activation(out=GE[:], in_=lg, func=AF.Exp, bias=s[:, 1:2], accum_out=s[:, 2:3]),
             r=[Lk, sk], w=['GE', sk])
        P.op('dve', lambda e, s=s: e.reciprocal(out=s[:, 3:4], in_=s[:, 2:3]), r=[sk], w=[sk])
        for g in range(4):
            le = L[:, tt, 4 + g * 8:4 + (g + 1) * 8]
            if g == 0:
                P.op('dve', lambda e, le=le: e.tensor_scalar(out=IG[:], in0=le, scalar1=OH[:, 0:1], scalar2=None, op0=ALU.mult),
                     r=[Lk, 'OH'], w=['IG'])
            else:
                P.op('dve', lambda e, le=le, g=g: e.scalar_tensor_tensor(out=IG[:], in0=le, scalar=OH[:, g:g + 1], in1=IG[:],
                                                                       op0=ALU.mult, op1=ALU.add), r=[Lk, 'OH', 'IG'], w=['IG'])
        P.op('dve', lambda e, s=s: e.tensor_reduce(out=s[:, 4:5], in_=IG[:], axis=AX.X, op=ALU.max), r=['IG', sk], w=[sk])
        P.op('dve', lambda e, s=s: e.tensor_scalar(out=M1[:], in0=IG[:], scalar1=s[:, 4:5], scalar2=None, op0=ALU.is_ge),
             r=['IG', sk], w=['M1'])
        P.op('dve', lambda e: e.scalar_tensor_tensor(out=IG2[:], in0=M1[:], scalar=-1e30, in1=IG[:], op0=ALU.mult, op1=ALU.add),
             r=['M1', 'IG'], w=['IG2'])
        P.op('dve', lambda e, s=s: e.tensor_reduce(out=s[:, 5:6], in_=IG2[:], axis=AX.X, op=ALU.max), r=['IG2', sk], w=[sk])
        P.op('dve', lambda e, s=s: e.tensor_scalar(out=M2[:], in0=IG2[:], scalar1=s[:, 5:6], scalar2=None, op0=ALU.is_ge),
             r=['IG2', sk], w=['M2'])
        P.op('dve', lambda e, s=s: e.tensor_tensor(out=s[:, 6:7], in0=s[:, 5:6], in1=s[:, 4:5], op=ALU.subtract), r=[sk], w=[sk])
        P.op('act', lambda e, s=s: e.activation(out=s[:, 7:8], in_=s[:, 6:7], func=AF.Exp), r=[sk], w=[sk])
        P.op('dve', lambda e, s=s: e.tensor_scalar(out=s[:, 8:9], in0=s[:, 7:8], scalar1=1.0, scalar2=None, op0=ALU.add),
             r=[sk], w=[sk])
        P.op('dve', lambda e, s=s: e.reciprocal(out=s[:, 8:9], in_=s[:, 8:9]), r=[sk], w=[sk])
        P.op('dve', lambda e, s=s: e.tensor_tensor(out=s[:, 9:10], in0=s[:, 8:9], in1=s[:, 3:4], op=ALU.mult), r=[sk], w=[sk])
        P.op('dve', lambda e, s=s: e.tensor_tensor(out=s[:, 10:11], in0=s[:, 9:10], in1=s[:, 7:8], op=ALU.mult), r=[sk], w=[sk])
        P.op('dve', lambda e, s=s: e.tensor_scalar(out=WM[:], in0=M1[:], scalar1=s[:, 9:10], scalar2=None, op0=ALU.mult),
             r=['M1', sk], w=['WM'])
        P.op('dve', lambda e, s=s: e.scalar_tensor_tensor(out=WM[:], in0=M2[:], scalar=s[:, 10:11], in1=WM[:],
                                                         op0=ALU.mult, op1=ALU.add), r=['M2', sk, 'WM'], w=['WM'])
        for g in range(4):
            P.op('dve', lambda e, g=g, tt=tt: e.tensor_scalar(out=WF[:, tt, g * 8:(g + 1) * 8], in0=WM[:], scalar1=OH[:, g:g + 1],
                                                            scalar2=None, op0=ALU.mult), r=['WM', 'OH'], w=[('WF', tt)])
        bank = banks[4 + tt // 4]
        P.op('pe', lambda e, tt=tt, bank=bank: e.transpose(out=bank[0:32, (tt % 4) * 128:(tt % 4 + 1) * 128], in_=WF[:, tt, :],
                                                         identity=IDN[:]), r=[('WF', tt), 'IDN'], w=[('bk', 4 + tt // 4)])
    for hh in range(2):
        P.op('act', lambda e, hh=hh: e.activation(out=WGT[:, hh * 512:(hh + 1) * 512], in_=banks[4 + hh][0:32, :], func=AF.Copy),
             r=[('bk', 4 + hh)], w=['WGT'])
    P.dma('sp', wscr, WGT[:], r=['WGT'], w=['wscr'])
    h2f_all = [('n', 'h', 1, t) for t in range(2)]
    bi = 0
    ld = 0
    for ex in range(32):
        wb = WB[ex % 2]
        wbk = ('WB', ex % 2)
        P.dma('pool', wb[:], wscr[ex:ex + 1, :].partition_broadcast(128), r=['wscr'], w=[wbk])
        for jt in range(4):
            gb = GUB[ld % 2]
            for gu in range(2):
                c0 = gu * 512 + jt * 128
                P.dma('sp' if gu == 0 else 'act', GS[gu], wgu[ex, :, :, c0:c0 + 128], w=[('GS', gu)] + (h2f_all if ex == 0 and jt == 0 else []))
                P.op('pool' if gu == 0 else 'dve', lambda e, gu=gu, gb=gb: e.tensor_copy(out=gb[gu][:], in_=GS[gu]),
                     r=[('GS', gu)], w=[('GUB', ld % 2, gu)])
            for t2 in range(2):
                ts = slice(t2 * 512, (t2 + 1) * 512)
                bg, bu = banks[(bi * 2) % 6], banks[(bi * 2 + 1) % 6]
                kg, ku = ('bk', (bi * 2) % 6), ('bk', (bi * 2 + 1) % 6)
                for gu, (bank, bkk) in enumerate(((bg, kg), (bu, ku))):
                    for k in range(16):
                        P.op('pe', lambda e, k=k, gu=gu, gb=gb, bank=bank, ts=ts: e.matmul(
                            bank[:, :], lhsT=gb[gu][:, k, :], rhs=H2B[:, k, ts], start=(k == 0), stop=(k == 15)),
                            r=[('GUB', ld % 2, gu), ('n', 'h', 0, t2)], w=[bkk])
                ta = TMPA[bi % 2]
                tak = ('TMPA', bi % 2)
                P.op('act', lambda e, ta=ta, bg=bg: e.activation(out=ta[:], in_=bg[:, :], func=AF.Silu), r=[kg], w=[tak])
                P.op('dve', lambda e, ta=ta, bu=bu: e.tensor_tensor(out=ta[:], in0=bu[:, :], in1=ta[:], op=ALU.mult), r=[ku, tak], w=[tak])
                P.op('dve', lambda e, ta=ta, wb=wb, jt=jt, ts=ts: e.tensor_tensor(out=ACTB[:, jt, ts], in0=ta[:], in1=wb[:, ts], op=ALU.mult),
                     r=[tak, wbk], w=[('ACTB', jt, t2)])
                bi += 1
            ld += 1
        for dq in range(4):
            wd = WDB[dq % 2]
            wdk = ('WDB', dq % 2)
            P.dma('sp', WDS, wdn[ex, :, :, dq * 512:(dq + 1) * 512], w=['WDS'] + (h2f_all if ex == 0 and dq == 0 else []))
            P.op('pool', lambda e, wd=wd: e.tensor_copy(out=wd[:], in_=WDS), r=['WDS'], w=[wdk])
            for dt in range(4):
                d = dq * 4 + dt
                for t2 in range(2):
                    ts = slice(t2 * 512, (t2 + 1) * 512)
                    bank = banks[6 + (bi % 2)]
                    bkk = ('bk', 6 + (bi % 2))
                    for c in range(4):
                        P.op('pe', lambda e, c=c, wd=wd, dt=dt, bank=bank, ts=ts: e.matmul(
                            bank[:, :], lhsT=wd[:, c, dt * 128:(dt + 1) * 128], rhs=ACTB[:, c, ts], start=(c == 0), stop=(c == 3)),
                            r=[wdk, ('ACTB', c, t2)], w=[bkk])
                    if ex == 0:
                        P.op('act', lambda e, d=d, ts=ts, bank=bank: e.activation(out=XA[:, d, ts], in_=bank[:, :], func=AF.Copy),
                             r=[bkk, ('n', 'x', d)], w=[('ACC', d, t2)])
                    else:
                        P.op('dve', lambda e, d=d, ts=ts, bank=bank: e.tensor_tensor(out=XA[:, d, ts], in0=bank[:, :], in1=XA[:, d, ts],
                                                                                   op=ALU.add), r=[bkk, ('ACC', d, t2)], w=[('ACC', d, t2)])
                    bi += 1
    for d in range(16):
        P.dma('sp', XR, xT[:, d, :], w=['XR'] + (['WDS', ('GS', 0), ('GS', 1)] if d == 0 else []))
        P.op('dve', lambda e, d=d: e.scalar_tensor_tensor(out=XA[:, d, :], in0=XA[:, d, :], scalar=PT[:, 3, d:d + 1], in1=XR,
                                                        op0=ALU.mult, op1=ALU.add),
             r=[('ACC', d, 0), ('ACC', d, 1), 'XR', 'G2'], w=[('ACC', d, 0), ('ACC', d, 1)])
        P.dma('pool', out[:, d, :], XA[:, d, :], r=[('ACC', d, 0), ('ACC', d, 1)])
    return P.build()


def emit_norm_cb(P, xt, nk, ntok, gam, sc, sh, ones, ps_banks, rstd, tmp, outs, tag, cb):
    Dn = nk * 128
    gm = P.sb([128, nk], F32)
    P.op('dve', lambda e: e.scalar_tensor_tensor(out=gm[:], in0=sc, scalar=1.0, in1=gam, op0=ALU.add, op1=ALU.mult),
         r=[(tag, 'sc'), (tag, 'gam')], w=[(tag, 'gm')])
    for t in range(ntok // 512):
        ts = slice(t * 512, (t + 1) * 512)
        bank = ps_banks[t % len(ps_banks)]
        bk = ('psb', id(bank))
        for k in range(nk):
            j = k % 2
            P.op('act', lambda e, k=k, j=j, ts=ts: e.activation(out=tmp[:, j, :], in_=xt[:, k, ts], func=AF.Square),
                 r=[(tag, 'x', k)], w=[(tag, 'tmp', j)])
            P.op('pe', lambda e, k=k, j=j, bank=bank: e.matmul(bank[:, :], lhsT=ones[:], rhs=tmp[:, j, :],
                                                             start=(k == 0), stop=(k == nk - 1)),
                 r=[(tag, 'tmp', j), 'ones'], w=[bk])
        P.op('act', lambda e, ts=ts, bank=bank: e.activation(out=rstd[:, ts], in_=bank[:, :], func=AF.Sqrt,
                                                            scale=1.0 / Dn, bias=epsb[0][:]),
             r=[bk, 'epsb'], w=[(tag, 'rstd', t)])
        P.op('dve', lambda e, ts=ts: e.reciprocal(out=rstd[:, ts], in_=rstd[:, ts]),
             r=[(tag, 'rstd', t)], w=[(tag, 'rstd', t)])
        for k in range(nk):
            j = k % 2
            P.op('dve', lambda e, k=k, j=j, ts=ts: e.tensor_tensor(out=tmp[:, j, :], in0=xt[:, k, ts], in1=rstd[:, ts],
                                                                 op=ALU.mult),
                 r=[(tag, 'x', k), (tag, 'rstd', t)], w=[(tag, 'tmp', j)])
            for i, (ot, local) in enumerate(outs):
                osl = slice(0, 512) if local else ts
                P.op('act', lambda e, k=k, j=j, osl=osl, ot=ot: e.activation(
                    out=ot[:, k, osl], in_=tmp[:, j, :], func=AF.Identity, scale=gm[:, k:k + 1], bias=sh[:, k:k + 1]),
                    r=[(tag, 'tmp', j), (tag, 'gm'), (tag, 'sh')], w=[(tag, 'h', i, t)])
        cb(t)


def run_E(x1T, gam, sc2, sh2, g2, inp, l):
    nc = cached('E', build_E)
    wr = fm(np.concatenate([inp["router_grp_w"][l], inp["router_exp_w"][l]], axis=1))
    rb = np.concatenate([inp["router_grp_b"][l], inp["router_exp_b"][l]])[None, :].astype(np.float32)
    wgu = np.ascontiguousarray(inp["expert_w_gu"][l].reshape(32, 16, 128, 1024).transpose(0, 2, 1, 3))
    wdn = np.ascontiguousarray(inp["expert_w_down"][l].reshape(32, 4, 128, 2048).transpose(0, 2, 1, 3))
    idd = np.eye(128, dtype=np.float32)
    in_maps = []
    for i in range(NCORE):
        b = i // 4
        prm = np.stack([vec_fm(gam), vec_fm(sc2[b]), vec_fm(sh2[b]), vec_fm(g2[b])], axis=1)
        in_maps.append({"xT": fm(x1T[:, i * 1024:(i + 1) * 1024]), "prm": np.ascontiguousarray(prm), "wr": wr,
                        "rb": np.ascontiguousarray(rb), "idd": idd, "wgu": wgu, "wdn": wdn})
    res = launch(nc, in_maps)
    return np.concatenate([r["out"].transpose(1, 0, 2).reshape(2048, 1024) for r in res], axis=1)


DECAY_SCALE_ = float(np.exp(-0.5))


def build_RW1():
    P = Prog()
    feats = P.dram("feats", [6, 128, 2 * S_], F32, "ExternalInput")
    prm = P.dram("prm", [128, 6, 2], F32, "ExternalInput")
    pv = P.dram("pv", [128, 8], F32, "ExternalInput")
    wl = P.dram("wl", [128, 5, 128], F32, "ExternalInput")
    bones = P.dram("bones", [128, 128], F32, "ExternalInput")
    outs = P.dram("outs", [11, 128, 2 * S_], F32, "ExternalOutput")
    mk_consts(P)
    PR = P.sb([128, 6, 2], F32)
    C0 = P.sb([128, 6], F32)
    PV = P.sb([128, 8], F32)
    OMK = P.sb([128, 1], F32)
    WL = P.sb([128, 5, 128], F32)
    BO = P.sb([128, 128], F32)
    STG = P.sb([128, S_], F32)
    F = [P.sb([128, S_], F32) for _ in range(6)]
    T1 = P.sb([128, S_], F32)
    T2 = P.sb([128, S_], F32)
    T3 = P.sb([128, S_], F32)
    banks = [P.ps([128, 512]) for _ in range(4)]
    P.dma('sp', PR[:], prm, w=['PR'])
    P.dma('sp', PV[:], pv, w=['PV'])
    P.dma('sp', WL[:], wl, w=['WL'])
    P.dma('sp', BO[:], bones, w=['BO'])
    P.op('dve', lambda e: e.tensor_tensor(out=C0[:], in0=PR[:, :, 0], in1=PR[:, :, 1], op=ALU.add), r=['PR'], w=['C0'])
    P.op('dve', lambda e: e.tensor_scalar(out=C0[:], in0=C0[:], scalar1=-1.0, scalar2=1.0, op0=ALU.mult, op1=ALU.add),
         r=['C0'], w=['C0'])
    P.op('dve', lambda e: e.tensor_scalar(out=OMK[:], in0=PV[:, 5:6], scalar1=-1.0, scalar2=1.0, op0=ALU.mult, op1=ALU.add),
         r=['PV'], w=['OMK'])
    bi = [0]

    def mm_act(lhsT, src, skey, dst, dkey, func, bias=None, scale=1.0, extra_r=()):
        for t in range(S_ // 512):
            ts = slice(t * 512, (t + 1) * 512)
            bank = banks[bi[0] % 4]
            bkk = ('bk', bi[0] % 4)
            P.op('pe', lambda e, ts=ts, bank=bank: e.matmul(bank[:, :], lhsT=lhsT, rhs=src[:, ts], start=True, stop=True),
                 r=[skey, 'WL', 'BO'], w=[bkk])
            if bias is not None:
                P.op('act', lambda e, ts=ts, bank=bank: e.activation(out=dst[:, ts], in_=bank[:, :], func=func, bias=bias, scale=scale),
                     r=[bkk, 'PV'] + list(extra_r), w=[dkey])
            else:
                P.op('act', lambda e, ts=ts, bank=bank: e.activation(out=dst[:, ts], in_=bank[:, :], func=func, scale=scale),
                     r=[bkk] + list(extra_r), w=[dkey])
            bi[0] += 1

    def store(idx, src, skey, b):
        P.dma('pool', outs[idx, :, b * S_:(b + 1) * S_], src[:], r=[skey])

    for b in range(2):
        bs = slice(b * S_, (b + 1) * S_)
        for a in range(6):
            P.dma('sp', STG[:], feats[a, :, bs], w=['STG'])
            Fa, fk = F[a], ('F', a)
            P.op('dve', lambda e, Fa=Fa, a=a: e.tensor_scalar(out=Fa[:], in0=STG[:], scalar1=C0[:, a:a + 1], scalar2=None, op0=ALU.mult),
                 r=['STG', 'C0'], w=[fk])
            P.op('dve', lambda e, Fa=Fa, a=a: e.scalar_tensor_tensor(out=Fa[:, 1:S_], in0=STG[:, 0:S_ - 1], scalar=PR[:, a, 0:1],
                                                                   in1=Fa[:, 1:S_], op0=ALU.mult, op1=ALU.add),
                 r=['STG', 'PR', fk], w=[fk])
            P.op('dve', lambda e, Fa=Fa, a=a: e.scalar_tensor_tensor(out=Fa[:, 0:S_ - 1], in0=STG[:, 1:S_], scalar=PR[:, a, 1:2],
                                                                   in1=Fa[:, 0:S_ - 1], op0=ALU.mult, op1=ALU.add),
                 r=['STG', 'PR', fk], w=[fk])
        R_, K_, V_, WD, AD, GD = F
        store(0, R_, ('F', 0), b)
        store(2, V_, ('F', 2), b)
        P.op('act', lambda e: e.activation(out=GD[:], in_=GD[:], func=AF.Sigmoid), r=[('F', 5)], w=[('F', 5)])
        mm_act(WL[:, 4, :], GD, ('F', 5), T1, 'T1', AF.Copy)
        store(9, T1, 'T1', b)
        P.op('dve', lambda e: e.tensor_scalar(out=T2[:], in0=K_[:], scalar1=PV[:, 4:5], scalar2=None, op0=ALU.mult),
             r=[('F', 1), 'PV'], w=['T2'])
        P.op('act', lambda e: e.activation(out=T3[:], in_=T2[:], func=AF.Square), r=['T2'], w=['T3'])
        mm_act(BO[:], T3, 'T3', T1, 'T1', AF.Sqrt, bias=epsb[0][:], extra_r=['epsb'])
        P.op('dve', lambda e: e.reciprocal(out=T1[:], in_=T1[:]), r=['T1'], w=['T1'])
        P.op('dve', lambda e: e.tensor_tensor(out=T2[:], in0=T2[:], in1=T1[:], op=ALU.mult), r=['T1', 'T2'], w=['T2'])
        store(1, T2, 'T2', b)
        P.op('act', lambda e: e.activation(out=WD[:], in_=WD[:], func=AF.Tanh), r=[('F', 3)], w=[('F', 3)])
        for z in range(2):
            mm_act(WL[:, 2 + z, :], AD, ('F', 4), T1, 'T1', AF.Sigmoid, bias=PV[:, 2 + z:3 + z])
            P.op('dve', lambda e: e.tensor_tensor(out=T3[:], in0=T1[:], in1=T2[:], op=ALU.mult), r=['T1', 'T2'], w=['T3'])
            store(3 + z, T3, 'T3', b)
            P.op('dve', lambda e: e.tensor_scalar(out=T1[:], in0=T1[:], scalar1=PV[:, 5:6], scalar2=OMK[:, 0:1],
                                                  op0=ALU.mult, op1=ALU.add), r=['T1', 'PV', 'OMK'], w=['T1'])
            P.op('dve', lambda e: e.tensor_tensor(out=T1[:], in0=T1[:], in1=K_[:], op=ALU.mult), r=['T1', ('F', 1)], w=['T1'])
            store(5 + z, T1, 'T1', b)
            P.op('dve', lambda e: e.scalar_tensor_tensor(out=T3[:], in0=R_[:], scalar=PV[:, 6:7], in1=T1[:], op0=ALU.mult, op1=ALU.mult),
                 r=[('F', 0), 'PV', 'T1', 'T3'], w=['T3'])
            mm_act(BO[:], T3, 'T3', T3, 'T3b', AF.Copy)
            if z == 0:
                P.op('dve', lambda e: e.tensor_tensor(out=STG[:], in0=T3[:], in1=V_[:], op=ALU.mult), r=['T3b', ('F', 2), 'STG'], w=['STG'])
            else:
                P.op('dve', lambda e: e.tensor_tensor(out=T3[:], in0=T3[:], in1=V_[:], op=ALU.mult), r=['T3b', ('F', 2)], w=['T3b'])
                P.op('dve', lambda e: e.tensor_tensor(out=STG[:], in0=STG[:], in1=T3[:], op=ALU.add), r=['T3b', 'STG'], w=['STG'])
                store(10, STG, 'STG', b)
            mm_act(WL[:, z, :], WD, ('F', 3), T1, 'T1', AF.Sigmoid, bias=PV[:, z:z + 1])
            P.op('dve', lambda e: e.tensor_scalar(out=T1[:], in0=T1[:], scalar1=-DECAY_SCALE_, scalar2=None, op0=ALU.mult),
                 r=['T1'], w=['T1'])
            store(7 + z, T1, 'T1', b)
    return P.build()


RW_OFF = 7168


def run_RW1(pT, inp, l):
    nc = cached('RW1', build_RW1)
    fT = pT[RW_OFF:RW_OFF + 3456]
    mu_p, mu_n = inp["rwkv_mu_prev"][l], inp["rwkv_mu_next"][l]
    bones = np.kron(np.eye(2, dtype=np.float32), np.ones((64, 64), np.float32))
    in_maps = []
    for i in range(NCORE):
        cs = slice(i * 128, (i + 1) * 128)
        rows = [slice(i * 128, (i + 1) * 128), slice(1024 + i * 128, 1024 + (i + 1) * 128),
                slice(2048 + i * 128, 2048 + (i + 1) * 128), slice(3072, 3200), slice(3200, 3328), slice(3328, 3456)]
        feats = np.stack([fT[r] for r in rows])
        prm = np.stack([np.stack([mu_p[r], mu_n[r]], axis=1) for r in rows], axis=1)
        pv = np.zeros((128, 8), np.float32)
        pv[:, 0] = inp["rwkv_w0"][l][0, cs]; pv[:, 1] = inp["rwkv_w0"][l][1, cs]
        pv[:, 2] = inp["rwkv_a0"][l][0, cs]; pv[:, 3] = inp["rwkv_a0"][l][1, cs]
        pv[:, 4] = inp["rwkv_k_k"][l][cs]; pv[:, 5] = inp["rwkv_k_a"][l][cs]
        pv[:, 6] = inp["rwkv_r_k"][l].reshape(-1)[cs]
        wl = np.zeros((128, 5, 128), np.float32)
        for z in range(2):
            wl[z * 64:(z + 1) * 64, z, :] = inp["rwkv_w_up"][l][z][:, cs]
            wl[z * 64:(z + 1) * 64, 2 + z, :] = inp["rwkv_a_up"][l][z][:, cs]
        wl[:, 4, :] = inp["rwkv_g_up"][l][:, cs]
        in_maps.append({"feats": np.ascontiguousarray(feats), "prm": np.ascontiguousarray(prm.astype(np.float32)),
                        "pv": pv, "wl": wl, "bones": bones})
    res = launch(nc, in_maps)
    return [r["outs"] for r in res]


USE_F32R = [True]


def R32(ap):
    return ap.bitcast(mybir.dt.float32r) if USE_F32R[0] else ap


def build_RW2_old(nseg=8):
    P = Prog()
    arr = P.dram("arr", [4, 8, 128, 6, 512], F32, "ExternalInput")
    cm = P.dram("cm", [128, 5, 128], F32, "ExternalInput")
    yout = P.dram("yout", [4, 128, 64, 64], F32, "ExternalOutput")
    CM = P.sb([128, 5, 128], F32)
    NL, NU, UU, UI, IDN = [CM[:, i, :] for i in range(5)]
    MSK = P.sb([128, 512], F32)
    IN = [[P.sb([128, 6, 512], F32) for _ in range(2)] for _ in range(4)]
    CUM = [P.sb([128, 512], F32) for _ in range(4)]
    YB = [[P.sb([128, 8, 64], F32) for _ in range(2)] for _ in range(4)]
    S0 = [P.sb([128, 64], F32) for _ in range(4)]
    EX = [[P.sb([128, 64], F32) for _ in range(4)] for _ in range(4)]
    names = ['A', 'B', 'K', 'R', 'BG', 'KG', 'V']
    BD = [{nm: P.sb([128, 128], F32) for nm in names} for _ in range(4)]
    WK = [{nm: P.sb([128, 128], F32) for nm in ['P0', 'PT0', 'P1', 'PT1', 'TT', 'NKA', 'MBR', 'MKR', 'BGT', 'KGT']} for _ in range(4)]
    SM = [{nm: P.sb([128, 64], F32) for nm in ['VST', 'VH', 'W0', 'NRHO']} for _ in range(4)]
    banks = [P.ps([128, 512]) for _ in range(8)]
    slot = [0]

    def ps():
        s = slot[0] % 8
        slot[0] += 1
        return banks[s][:, 0:128], ('ps', s)

    P.dma('sp', CM[:], cm, w=['CM'])
    P.op('pool', lambda e: e.memset(MSK[:], 1.0), w=['MSK'])
    P.op('pool', lambda e: e.memset(MSK[:].rearrange("p (n t) -> p n t", t=64)[:, :, 0:1], 0.0), r=['MSK'], w=['MSK'])
    ZERO = P.sb([128, 128], F32)
    P.op('pool', lambda e: e.memset(ZERO[:], 0.0), w=['ZERO'])
    for p in range(4):
        for nm in names:
            P.op('dve', lambda e, p=p, nm=nm: e.tensor_copy(out=R32(BD[p][nm][:]), in_=ZERO[:]), r=['ZERO'], w=[('BD', p, nm)])
        P.op('dve', lambda e, p=p: e.tensor_copy(out=R32(S0[p][:]), in_=ZERO[:, 0:64]), r=['ZERO'], w=[('S0', p)])
    for sg in range(nseg):
        ib = sg % 2
        for p in range(4):
            ik = ('IN', p, ib)
            P.dma('sp' if p % 2 == 0 else 'act', IN[p][ib][:], arr[p, sg], w=[ik])
            P.op('dve', lambda e, p=p, ib=ib: e.tensor_tensor_scan(out=CUM[p][:], data0=MSK[:], data1=IN[p][ib][:, 5, :], initial=0.0,
                                                                   op0=ALU.mult, op1=ALU.add), r=[ik, 'MSK'], w=[('CUM', p)])
            P.op('pool', lambda e, p=p, ib=ib: e.tensor_tensor(out=IN[p][ib][:, 5, :], in0=CUM[p][:], in1=IN[p][ib][:, 5, :],
                                                               op=ALU.subtract), r=[('CUM', p), ik], w=[ik])
        for j in range(8):
            cs = slice(j * 64, (j + 1) * 64)

            def gen(p, j=j, cs=cs):
                ik = ('IN', p, ib)
                I_ = IN[p][ib]
                Ep, En, Ex, Ec = EX[p]
                ek = [('EX', p, i) for i in range(4)]
                ck = ('CUM', p)
                P.op('act', lambda e, p=p, Ep=Ep, cs=cs: e.activation(out=Ep[:], in_=CUM[p][:, cs], func=AF.Exp), r=[ck], w=[ek[0]])
                P.op('act', lambda e, p=p, En=En, cs=cs: e.activation(out=En[:], in_=CUM[p][:, cs], func=AF.Exp, scale=-1.0), r=[ck], w=[ek[1]])
                P.op('act', lambda e, I_=I_, Ex=Ex, cs=cs: e.activation(out=Ex[:], in_=I_[:, 5, cs], func=AF.Exp), r=[ik], w=[ek[2]])
                P.op('act', lambda e, p=p, Ec=Ec, cs=cs, j=j: e.activation(out=Ec[:], in_=CUM[p][:, cs], func=AF.Exp, scale=-1.0,
                                                                         bias=CUM[p][:, j * 64 + 63:j * 64 + 64]), r=[ck], w=[ek[3]])
                yield
                specs = [('A', 1, Ex, ek[2]), ('B', 3, En, ek[1]), ('K', 4, En, ek[1]), ('R', 0, Ep, ek[0]),
                         ('BG', 3, Ec, ek[3]), ('KG', 4, Ec, ek[3]), ('V', 2, None, None)]
                for si, (nm, ai, Et, etk) in enumerate(specs):
                    for c in range(2):
                        ps_ = slice(c * 64, (c + 1) * 64)
                        dst = BD[p][nm][ps_, c * 64:(c + 1) * 64]
                        eng = 'pool' if si % 2 == 0 else 'dve'
                        if Et is None:
                            P.op(eng, lambda e, dst=dst, I_=I_, ai=ai, ps_=ps_, cs=cs: e.tensor_copy(out=R32(dst), in_=I_[ps_, ai, cs]),
                                 r=[ik], w=[('BD', p, nm)])
                        else:
                            P.op(eng, lambda e, dst=dst, I_=I_, ai=ai, ps_=ps_, cs=cs, Et=Et: e.tensor_tensor(
                                out=R32(dst), in0=I_[ps_, ai, cs], in1=Et[ps_, :], op=ALU.mult), r=[ik, etk], w=[('BD', p, nm)])
                yield
                bd = BD[p]
                bk_ = lambda nm, p=p: ('BD', p, nm)
                wk = WK[p]
                wkk = lambda nm, p=p: ('WK', p, nm)
                smm = SM[p]
                smk = lambda nm, p=p: ('SM', p, nm)

                def mm(lhsT, lk, rhs, rk, n=128):
                    o, ok = ps()
                    oo = o[:, 0:n]
                    P.op('pe', lambda e, oo=oo, lhsT=lhsT, rhs=rhs: e.matmul(oo, lhsT=R32(lhsT), rhs=R32(rhs), start=True, stop=True),
                         r=[lk, rk], w=[ok])
                    return oo, ok

                def mmacc(terms, n=64):
                    o, ok = ps()
                    oo = o[:, 0:n]
                    for ti, (lhsT, lk, rhs, rk) in enumerate(terms):
                        P.op('pe', lambda e, oo=oo, lhsT=lhsT, rhs=rhs, ti=ti: e.matmul(
                            oo, lhsT=R32(lhsT), rhs=R32(rhs), start=(ti == 0), stop=(ti == len(terms) - 1)), r=[lk, rk], w=[ok])
                    return oo, ok

                def evac_mask(dst, dk, src, sk, mask):
                    P.op('dve', lambda e, dst=dst, src=src, mask=mask: e.tensor_tensor(out=R32(dst), in0=src, in1=mask, op=ALU.mult),
                         r=['CM'], w=[dk, sk])

                def evac_copy(dst, dk, src, sk, scale=1.0):
                    P.op('act', lambda e, dst=dst, src=src, scale=scale: e.activation(out=R32(dst), in_=src, func=AF.Copy, scale=scale),
                         r=[], w=[dk, sk])

                o, ok = mm(bd['A'][:], bk_('A'), bd['B'][:], bk_('B'))
                evac_mask(wk['P0'][:], wkk('P0'), o, ok, NL)
                yield
                o, ok = mm(bd['B'][:], bk_('B'), bd['A'][:], bk_('A'))
                evac_mask(wk['PT0'][:], wkk('PT0'), o, ok, NU)
                P.op('pool', lambda e, wk=wk: e.tensor_tensor(out=R32(wk['TT'][:]), in0=wk['PT0'][:], in1=IDN, op=ALU.add),
                     r=[wkk('PT0'), 'CM'], w=[wkk('TT')])
                yield
                o, ok = mm(bd['K'][:], bk_('K'), bd['A'][:], bk_('A'))
                evac_mask(wk['NKA'][:], wkk('NKA'), o, ok, UU)
                yield
                o, ok = mm(bd['B'][:], bk_('B'), bd['R'][:], bk_('R'))
                evac_mask(wk['MBR'][:], wkk('MBR'), o, ok, UI)
                yield
                o, ok = mm(bd['K'][:], bk_('K'), bd['R'][:], bk_('R'))
                evac_mask(wk['MKR'][:], wkk('MKR'), o, ok, UI)
                yield
                o, ok = ps()
                P.op('pe', lambda e, o=o, bd=bd: e.transpose(out=o, in_=bd['V'][:], identity=IDN), r=[bk_('V'), 'CM'], w=[ok])
                evac_copy(smm['VH'][:], smk('VH'), o[:, 0:64], ok)
                P.op('dve', lambda e, o=o, smm=smm: e.tensor_tensor(out=R32(smm['VST'][:]), in0=o[:, 64:128], in1=smm['VH'][:], op=ALU.add),
                     r=[smk('VH')], w=[smk('VST'), ok])
                yield
                for (src, dstn) in (('BG', 'BGT'), ('KG', 'KGT')):
                    o, ok = ps()
                    P.op('pe', lambda e, o=o, bd=bd, src=src: e.transpose(out=o, in_=bd[src][:], identity=IDN), r=[bk_(src), 'CM'], w=[ok])
                    evac_copy(wk[dstn][:], wkk(dstn), o, ok)
                yield
                cur, curT = 'P0', 'PT0'
                for lv in range(1, 6):
                    nxt, nxtT = ('P1', 'PT1') if cur == 'P0' else ('P0', 'PT0')
                    o, ok = mm(wk[curT][:], wkk(curT), wk[cur][:], wkk(cur))
                    evac_copy(wk[nxt][:], wkk(nxt), o, ok)
                    yield
                    if lv < 5:
                        o2, ok2 = mm(wk[cur][:], wkk(cur), wk[curT][:], wkk(curT))
                        evac_copy(wk[nxtT][:], wkk(nxtT), o2, ok2)
                    yield
                    o3, ok3 = mm(wk[nxt][:], wkk(nxt), wk['TT'][:], wkk('TT'))
                    P.op('dve', lambda e, wk=wk, o3=o3: e.tensor_tensor(out=R32(wk['TT'][:]), in0=o3, in1=wk['TT'][:], op=ALU.add),
                         r=[wkk('TT')], w=[wkk('TT'), ok3])
                    cur, curT = nxt, nxtT
                    yield
                yield
                s0k = ('S0', p)
                o, ok = mmacc([(bd['A'][:], bk_('A'), S0[p][:], s0k), (wk['NKA'][:], wkk('NKA'), smm['VST'][:], smk('VST'))])
                evac_copy(smm['W0'][:], smk('W0'), o, ok)
                yield
                o, ok = mm(wk['TT'][:], wkk('TT'), smm['W0'][:], smk('W0'), n=64)
                evac_copy(smm['NRHO'][:], smk('NRHO'), o, ok, scale=-1.0)
                yield
                o, ok = mmacc([(bd['R'][:], bk_('R'), S0[p][:], s0k), (wk['MBR'][:], wkk('MBR'), smm['NRHO'][:], smk('NRHO')),
                               (wk['MKR'][:], wkk('MKR'), smm['VST'][:], smk('VST'))])
                evac_copy(YB[p][ib][:, j, :], ('YB', p, ib), o, ok)
                yield
                o, ok = mmacc([(wk['BGT'][:], wkk('BGT'), smm['NRHO'][:], smk('NRHO')), (wk['KGT'][:], wkk('KGT'), smm['VST'][:], smk('VST'))])
                P.op('dve', lambda e, p=p, Ep=Ep, o=o: e.scalar_tensor_tensor(out=R32(S0[p][:]), in0=S0[p][:], scalar=Ep[:, 63:64], in1=o,
                                                                             op0=ALU.mult, op1=ALU.add), r=[s0k, ek[0]], w=[s0k, ok])

            gens = [gen(p) for p in range(4)]
            while gens:
                for g in gens[:]:
                    try:
                        next(g)
                    except StopIteration:
                        gens.remove(g)
        for p in range(4):
            P.dma('pool', yout[p, :, sg * 8:(sg + 1) * 8, :], YB[p][ib][:], r=[('YB', p, ib)])
    return P.build()


def build_RW2(nseg=16):
    P = Prog()
    SEGL = 256
    NCH = SEGL // 64
    arr = P.dram("arr", [4, 16, 128, 6, SEGL], F32, "ExternalInput")
    cm = P.dram("cm", [128, 6, 128], F32, "ExternalInput")
    yout = P.dram("yout", [4, 128, 64, 64], F32, "ExternalOutput")
    CM = P.sb([128, 6, 128], F32)
    CMX = CM[:, 0:2, :].rearrange("p a b -> p (a b)")
    CMS = CM[:, 2:5, :].rearrange("p a b -> p (a b)")
    IDN = CM[:, 5, :]
    MSK = P.sb([128, SEGL], F32)
    IN = [[P.sb([128, 6, SEGL], F32) for _ in range(2)] for _ in range(4)]
    CUM = [P.sb([128, 2, SEGL], F32) for _ in range(4)]
    EXS = [P.sb([128, 4, SEGL], F32) for _ in range(4)]
    YB = [[P.sb([128, NCH, 64], F32) for _ in range(2)] for _ in range(4)]
    S0 = [P.sb([128, 64], F32) for _ in range(4)]
    names = ['A', 'B', 'K', 'R', 'BG', 'KG', 'V']
    BDS = [{nm: P.sb([128, NCH, 128], F32) for nm in names} for _ in range(4)]
    PP = [[P.sb([128, 256], F32) for _ in range(2)] for _ in range(4)]
    TT = [P.sb([128, 128], F32) for _ in range(4)]
    SC = [P.sb([128, 384], F32) for _ in range(4)]
    GT = [P.sb([128, 256], F32) for _ in range(4)]
    SM = [{nm: P.sb([128, 64], F32) for nm in ['VST', 'VH', 'W0', 'NRHO']} for _ in range(4)]
    banks = [P.ps([128, 512]) for _ in range(8)]
    slot = [0]

    def psw():
        s_ = slot[0] % 8
        slot[0] += 1
        return banks[s_], ('ps', s_)

    P.dma('sp', CM[:], cm, w=['CM'])
    P.op('pool', lambda e: e.memset(MSK[:], 1.0), w=['MSK'])
    P.op('pool', lambda e: e.memset(MSK[:].rearrange("p (n t) -> p n t", t=64)[:, :, 0:1], 0.0), r=['MSK'], w=['MSK'])
    ZERO = P.sb([128, NCH * 128], F32)
    P.op('pool', lambda e: e.memset(ZERO[:], 0.0), w=['ZERO'])
    for p in range(4):
        for nm in names:
            P.op('dve', lambda e, p=p, nm=nm: e.tensor_copy(out=R32(BDS[p][nm][:].rearrange("p n t -> p (n t)")), in_=ZERO[:]),
                 r=['ZERO'], w=[('BD', p, nm)])
        P.op('dve', lambda e, p=p: e.tensor_copy(out=R32(S0[p][:]), in_=ZERO[:, 0:64]), r=['ZERO'], w=[('S0', p)])

    def mmq(o, ok, lhsT, lk, rhs, rk, start=True, stop=True):
        P.op('pe', lambda e: e.matmul(o, lhsT=R32(lhsT), rhs=R32(rhs), start=start, stop=stop), r=[lk, rk], w=[ok])

    v3 = lambda ap: ap.rearrange("p (n t) -> p n t", t=64)
    for sg in range(nseg):
        ib = sg % 2
        for p in range(4):
            ik = ('IN', p, ib)
            ck = ('CUM', p)
            ek = ('EXS', p)
            I_ = IN[p][ib]
            E_ = EXS[p]
            P.dma('sp' if p % 2 == 0 else 'act', I_[:], arr[p, sg], w=[ik])
            P.op('dve', lambda e, p=p, I_=I_: e.tensor_tensor_scan(out=CUM[p][:, 0, :], data0=MSK[:], data1=I_[:, 5, :], initial=0.0,
                                                                   op0=ALU.mult, op1=ALU.add), r=[ik, 'MSK'], w=[ck])
            P.op('dve', lambda e, p=p, I_=I_: e.tensor_tensor_scan(out=CUM[p][:, 1, ::-1], data0=MSK[:], data1=I_[:, 5, ::-1], initial=0.0,
                                                                   op0=ALU.mult, op1=ALU.add), r=[ik, 'MSK', ck], w=[ck])
            P.op('act', lambda e, p=p, E_=E_: e.activation(out=E_[:, 0, :], in_=CUM[p][:, 0, :], func=AF.Exp), r=[ck], w=[ek])
            P.op('act', lambda e, p=p, E_=E_: e.activation(out=E_[:, 1, :], in_=CUM[p][:, 0, :], func=AF.Exp, scale=-1.0), r=[ck, ek], w=[ek])
            P.op('pool', lambda e, p=p, I_=I_: e.tensor_tensor(out=CUM[p][:, 0, :], in0=CUM[p][:, 0, :], in1=I_[:, 5, :], op=ALU.subtract),
                 r=[ik, ek], w=[ck])
            P.op('pool', lambda e, p=p, I_=I_: e.tensor_tensor(out=CUM[p][:, 1, :], in0=CUM[p][:, 1, :], in1=I_[:, 5, :], op=ALU.subtract),
                 r=[ik], w=[ck])
            P.op('act', lambda e, p=p, E_=E_: e.activation(out=E_[:, 2:4, :], in_=CUM[p][:, 0:2, :], func=AF.Exp), r=[ck, ek], w=[ek])
            for c in range(2):
                ps_ = slice(c * 64, (c + 1) * 64)
                for (nm, ai, ei) in (('A', 1, 2), ('B', 3, 1), ('K', 4, 1), ('R', 0, 0), ('BG', 3, 3), ('KG', 4, 3)):
                    dst = BDS[p][nm][ps_, :, c * 64:(c + 1) * 64]
                    P.op('dve', lambda e, dst=dst, I_=I_, E_=E_, ai=ai, ei=ei, ps_=ps_: e.tensor_tensor(
                        out=R32(dst), in0=v3(I_[ps_, ai, :]), in1=v3(E_[ps_, ei, :]), op=ALU.mult), r=[ik, ek], w=[('BD', p, nm)])
                dst = BDS[p]['V'][ps_, :, c * 64:(c + 1) * 64]
                P.op('pool', lambda e, dst=dst, I_=I_, ps_=ps_: e.tensor_copy(out=R32(dst), in_=v3(I_[ps_, 2, :])), r=[ik], w=[('BD', p, 'V')])
        for j in range(NCH):

            def gen(p, j=j):
                ek = ('EXS', p)
                E_ = EXS[p]
                bd = {nm: BDS[p][nm][:, j, :] for nm in names}
                bk_ = lambda nm: ('BD', p, nm)
                smm = SM[p]
                smk = lambda nm: ('SM', p, nm)
                s0k = ('S0', p)
                ttk = ('TT', p)
                GC = E_[:, 0, j * 64 + 63:j * 64 + 64]
                pp = PP[p]
                ppk = lambda i: ('PP', p, i)
                o, ok = psw()
                mmq(o[:, 0:128], ok, bd['A'], bk_('A'), bd['B'], bk_('B'))
                mmq(o[:, 128:256], ok, bd['B'], bk_('B'), bd['A'], bk_('A'))
                P.op('dve', lambda e, o=o: e.tensor_tensor(out=R32(pp[0][:]), in0=o[:, 0:256], in1=CMX, op=ALU.mult), r=['CM'], w=[ppk(0), ok])
                P.op('pool', lambda e: e.tensor_tensor(out=R32(TT[p][:]), in0=pp[0][:, 128:256], in1=IDN, op=ALU.add), r=[ppk(0), 'CM'], w=[ttk])
                yield
                o, ok = psw()
                mmq(o[:, 0:128], ok, bd['K'], bk_('K'), bd['A'], bk_('A'))
                mmq(o[:, 128:256], ok, bd['B'], bk_('B'), bd['R'], bk_('R'))
                mmq(o[:, 256:384], ok, bd['K'], bk_('K'), bd['R'], bk_('R'))
                P.op('dve', lambda e, o=o: e.tensor_tensor(out=R32(SC[p][:]), in0=o[:, 0:384], in1=CMS, op=ALU.mult), r=['CM'], w=[('SC', p), ok])
                yield
                o, ok = psw()
                P.op('pe', lambda e, o=o: e.transpose(out=o[:, 0:128], in_=bd['V'], identity=IDN), r=[bk_('V'), 'CM'], w=[ok])
                P.op('act', lambda e, o=o: e.activation(out=smm['VH'][:], in_=o[:, 0:64], func=AF.Copy), r=[], w=[smk('VH'), ok])
                P.op('dve', lambda e, o=o: e.tensor_tensor(out=R32(smm['VST'][:]), in0=o[:, 64:128], in1=smm['VH'][:], op=ALU.add),
                     r=[smk('VH')], w=[smk('VST'), ok])
                yield
                o, ok = psw()
                P.op('pe', lambda e, o=o: e.transpose(out=o[:, 0:128], in_=bd['BG'], identity=IDN), r=[bk_('BG'), 'CM'], w=[ok])
                P.op('pe', lambda e, o=o: e.transpose(out=o[:, 128:256], in_=bd['KG'], identity=IDN), r=[bk_('KG'), 'CM'], w=[ok])
                P.op('act', lambda e, o=o: e.activation(out=R32(GT[p][:]), in_=o[:, 0:256], func=AF.Copy), r=[], w=[('GT', p), ok])
                yield
                c_ = 0
                for lv in range(1, 6):
                    cur, nxt = pp[c_], pp[1 - c_]
                    kc, kn = ppk(c_), ppk(1 - c_)
                    wdt = 256 if lv < 5 else 128
                    o, ok = psw()
                    mmq(o[:, 0:128], ok, cur[:, 128:256], kc, cur[:, 0:128], kc)
                    if lv < 5:
                        mmq(o[:, 128:256], ok, cur[:, 0:128], kc, cur[:, 128:256], kc)
                    P.op('act', lambda e, o=o, nxt=nxt, wdt=wdt: e.activation(out=R32(nxt[:, 0:wdt]), in_=o[:, 0:wdt], func=AF.Copy),
                         r=[], w=[kn, ok])
                    yield
                    o3, ok3 = psw()
                    mmq(o3[:, 0:128], ok3, nxt[:, 0:128], kn, TT[p][:], ttk)
                    P.op('dve', lambda e, o3=o3: e.tensor_tensor(out=R32(TT[p][:]), in0=o3[:, 0:128], in1=TT[p][:], op=ALU.add),
                         r=[], w=[ttk, ok3])
                    c_ = 1 - c_
                    yield
                o, ok = psw()
                mmq(o[:, 0:64], ok, bd['A'], bk_('A'), S0[p][:], s0k, True, False)
                mmq(o[:, 0:64], ok, SC[p][:, 0:128], ('SC', p), smm['VST'][:], smk('VST'), False, True)
                P.op('act', lambda e, o=o: e.activation(out=R32(smm['W0'][:]), in_=o[:, 0:64], func=AF.Copy), r=[], w=[smk('W0'), ok])
                yield
                o, ok = psw()
                mmq(o[:, 0:64], ok, TT[p][:], ttk, smm['W0'][:], smk('W0'))
                P.op('act', lambda e, o=o: e.activation(out=R32(smm['NRHO'][:]), in_=o[:, 0:64], func=AF.Copy, scale=-1.0),
                     r=[], w=[smk('NRHO'), ok])
                yield
                o, ok = psw()
                mmq(o[:, 0:64], ok, bd['R'], bk_('R'), S0[p][:], s0k, True, False)
                mmq(o[:, 0:64], ok, SC[p][:, 128:256], ('SC', p), smm['NRHO'][:], smk('NRHO'), False, False)
                mmq(o[:, 0:64], ok, SC[p][:, 256:384], ('SC', p), smm['VST'][:], smk('VST'), False, True)
                P.op('act', lambda e, o=o: e.activation(out=YB[p][ib][:, j, :], in_=o[:, 0:64], func=AF.Copy), r=[], w=[('YB', p, ib), ok])
                o2, ok2 = psw()
                mmq(o2[:, 0:64], ok2, GT[p][:, 0:128], ('GT', p), smm['NRHO'][:], smk('NRHO'), True, False)
                mmq(o2[:, 0:64], ok2, GT[p][:, 128:256], ('GT', p), smm['VST'][:], smk('VST'), False, True)
                P.op('dve', lambda e, o2=o2: e.scalar_tensor_tensor(out=R32(S0[p][:]), in0=S0[p][:], scalar=GC, in1=o2[:, 0:64],
                                                                   op0=ALU.mult, op1=ALU.add), r=[ek], w=[s0k, ok2])
                yield

            gens = [gen(p) for p in range(4)]
            while gens:
                for g in gens[:]:
                    try:
                        next(g)
                    except StopIteration:
                        gens.remove(g)
        for p in range(4):
            P.dma('pool', yout[p, :, sg * NCH:(sg + 1) * NCH, :], YB[p][ib][:], r=[('YB', p, ib)])
    return P.build()


def rw2_consts():
    t = np.arange(64)
    L = (t[None, :] < t[:, None]).astype(np.float32)
    U = L.T.copy()
    UI = U + np.eye(64, dtype=np.float32)
    bd = lambda m: np.kron(np.eye(2, dtype=np.float32), m)
    return np.ascontiguousarray(np.stack([bd(-L), bd(-U), bd(U), bd(UI), bd(UI), np.eye(128, dtype=np.float32)], axis=1))


def run_RW2(rw1, nseg=8):
    nc = cached(('RW2', nseg), lambda: build_RW2_old(nseg))
    cm = np.ascontiguousarray(rw2_consts()[:, [0, 1, 2, 3, 5], :])
    in_maps = []
    for i in range(NCORE):
        o = rw1[i]
        arrs = np.zeros((4, 8, 128, 6, 512), np.float32)
        for z in range(2):
            for b in range(2):
                sel = np.stack([o[0], o[1], o[2], o[3 + z], o[5 + z], o[7 + z]], axis=1)[:, :, b * S_:(b + 1) * S_]
                if z == 1:
                    sel = sel[:, :, ::-1]
                arrs[z * 2 + b] = sel.reshape(128, 6, 8, 512).transpose(2, 0, 1, 3)
        in_maps.append({"arr": arrs, "cm": cm})
    res = launch(nc, in_maps)
    return [r["yout"] for r in res]


def build_RW3():
    P = Prog()
    yfb = P.dram("yfb", [2, 128, 8, 1024], F32, "ExternalInput")
    bvg = P.dram("bvg", [2, 128, 8, 1024], F32, "ExternalInput")
    out = P.dram("out", [128, 8, 1024], F32, "ExternalOutput")
    mk_consts(P)
    Y = [P.sb([128, 1024], F32) for _ in range(2)]
    BV = P.sb([128, 1024], F32)
    G = P.sb([128, 1024], F32)
    SQ = P.sb([128, 1024], F32)
    O = [P.sb([128, 1024], F32) for _ in range(2)]
    st = P.sb([128, 2, 4, 16], F32)
    for tt in range(8):
        ob = O[tt % 2]
        okk = ('O', tt % 2)
        P.dma('sp', BV[:], bvg[0, :, tt, :], w=['BV'])
        P.dma('sp', G[:], bvg[1, :, tt, :], w=['G'])
        for z in range(2):
            yk = ('Y', z)
            P.dma('pool', Y[z][:], yfb[z, :, tt, :], w=[yk])
            y3 = Y[z][:].rearrange("p (h v) -> p h v", h=16)
            s1, s2, mn, rs = [st[:, z, i, :] for i in range(4)]
            sk = ('st', z)
            P.op('dve', lambda e, y3=y3, s1=s1: e.tensor_reduce(out=s1, in_=y3, axis=AX.X, op=ALU.add), r=[yk], w=[sk])
            P.op('act', lambda e, z=z: e.activation(out=SQ[:], in_=Y[z][:], func=AF.Square), r=[yk], w=['SQ'])
            P.op('dve', lambda e, s2=s2: e.tensor_reduce(out=s2, in_=SQ[:].rearrange("p (h v) -> p h v", h=16), axis=AX.X, op=ALU.add),
                 r=['SQ', sk], w=[sk])
            P.op('dve', lambda e, s1=s1, mn=mn: e.tensor_scalar(out=mn, in0=s1, scalar1=1.0 / 64, scalar2=None, op0=ALU.mult), r=[sk], w=[sk])
            P.op('dve', lambda e, s1=s1, mn=mn: e.tensor_tensor(out=s1, in0=mn, in1=mn, op=ALU.mult), r=[sk], w=[sk])
            P.op('dve', lambda e, s1=s1, s2=s2: e.scalar_tensor_tensor(out=s2, in0=s2, scalar=1.0 / 64, in1=s1, op0=ALU.mult, op1=ALU.subtract),
                 r=[sk], w=[sk])
            P.op('act', lambda e, s2=s2, rs=rs: e.activation(out=rs, in_=s2, func=AF.Sqrt, bias=epsb[0][:]), r=[sk, 'epsb'], w=[sk])
            P.op('dve', lambda e, rs=rs: e.reciprocal(out=rs, in_=rs), r=[sk], w=[sk])
            for h in range(16):
                hs = slice(h * 64, (h + 1) * 64)
                eng = 'dve' if h % 2 == 0 else 'pool'
                P.op(eng, lambda e, z=z, hs=hs, h=h, mn=mn, rs=rs: e.tensor_scalar(
                    out=Y[z][:, hs], in0=Y[z][:, hs], scalar1=mn[:, h:h + 1], scalar2=rs[:, h:h + 1], op0=ALU.subtract, op1=ALU.mult),
                    r=[sk, yk], w=[yk])
        P.op('dve', lambda e, ob=ob: e.tensor_tensor(out=ob[:], in0=Y[0][:], in1=Y[1][:], op=ALU.add), r=[('Y', 0), ('Y', 1)], w=[okk])
        P.op('dve', lambda e, ob=ob: e.tensor_tensor(out=ob[:], in0=ob[:], in1=BV[:], op=ALU.add), r=['BV', okk], w=[okk])
        P.op('dve', lambda e, ob=ob: e.tensor_tensor(out=ob[:], in0=ob[:], in1=G[:], op=ALU.mult), r=['G', okk], w=[okk])
        P.dma('sp', out[:, tt, :], ob[:], r=[okk])
    return P.build()


def run_RW3(rw1, rw2):
    nc = cached('RW3', build_RW3)
    yz = np.zeros((2, 2, S_, 16, 64), np.float32)
    for i in range(NCORE):
        for z in range(2):
            for b in range(2):
                y = rw2[i][z * 2 + b].reshape(2, 64, 64, 64).transpose(2, 1, 0, 3).reshape(S_, 2, 64)
                if z == 1:
                    y = y[::-1]
                yz[z, b, :, 2 * i:2 * i + 2, :] = y
    yz = yz.reshape(2, 2 * S_, 1024)
    bv = np.concatenate([o[10] for o in rw1], axis=0).T
    g = np.concatenate([o[9] for o in rw1], axis=0).T
    in_maps = []
    for i in range(NCORE):
        ts = slice(i * 1024, (i + 1) * 1024)
        tmj = lambda a: a[ts].reshape(8, 128, 1024).transpose(1, 0, 2)
        in_maps.append({"yfb": np.ascontiguousarray(np.stack([tmj(yz[0]), tmj(yz[1])])),
                        "bvg": np.ascontiguousarray(np.stack([tmj(bv), tmj(g)]))})
    res = launch(nc, in_maps)
    yd = np.concatenate([r["out"].transpose(1, 0, 2).reshape(1024, 1024) for r in res], axis=0)
    return np.ascontiguousarray(yd.T)


def kernel(**inp):
    inp = {k: np.asarray(v) for k, v in inp.items()}
    x = inp["x"].astype(np.float32)
    B, S, Dm = x.shape
    mod = run_ada(inp["c"], inp["ada_w"], inp["ada_b"])
    xT = np.ascontiguousarray(x.reshape(B * S, Dm).T)
    for l in range(2):
        sh1, sc1, g1, sh2, sc2, g2 = [mod[l][:, j * 2048:(j + 1) * 2048] for j in range(6)]
        hT = run_H(xT, inp["norm1_g"][l], sc1, sh1)
        pT = run_P(hT, inp["w_in"][l])
        del hT
        ya = run_LRU(pT, inp, l)
        yb = run_RET(pT, inp["positions"])
        yc = run_SGU(pT, inp, l)
        rw1 = run_RW1(pT, inp, l)
        rw2 = run_RW2(rw1)
        yd = run_RW3(rw1, rw2)
        del rw1, rw2
        ys = np.stack([ya, yb, yc, yd])
        x1T = run_M(ys, pT, xT, g1, inp["w_branch"][l], inp["w_out"][l])
        del pT, ys
        xT = run_E(x1T, inp["norm2_g"][l], sc2, sh2, g2, inp, l)
    z = np.zeros((2, 2048), np.float32)
    oT = run_H(xT, inp["final_norm_g"], z, z, out_dt=F32)
    return np.ascontiguousarray(oT.T).reshape(B, S, Dm).astype(np.float32)
```

```python
import contextlib
import numpy as np
import concourse.bass as bass
import concourse.mybir as mybir
from concourse.bass_utils import run_bass_kernel_spmd

F32 = mybir.dt.float32
BF16 = mybir.dt.bfloat16
I32 = mybir.dt.int32
AF = mybir.ActivationFunctionType
ALU = mybir.AluOpType
AX = mybir.AxisListType

N_LAUNCH = [0]
TRACE = [False]


class Prog:
    ENG = ('pe', 'act', 'dve', 'pool', 'sp')
    SAME = {'pe': False, 'act': True, 'dve': True, 'pool': True, 'sp': True}
    K = 6

    def __init__(self):
        self.nc = bass.Bass("TRN2", target_bir_lowering=False)
        self.es = contextlib.ExitStack()
        self.ops = {e: [] for e in self.ENG}
        self.res = {}
        self.seen = {e: {} for e in self.ENG}
        self.ndma = {e: 0 for e in self.ENG}
        self.n_t = 0

    def dram(self, name, shape, dt, kind):
        return self.nc.dram_tensor(name, list(shape), dt, kind=kind).ap()

    def sb(self, shape, dt, name=None):
        self.n_t += 1
        name = name or f"t{self.n_t}"
        return self.es.enter_context(self.nc.sbuf_tensor(name, list(shape), dt))

    def ps(self, shape, dt=F32, name=None):
        self.n_t += 1
        name = name or f"p{self.n_t}"
        return self.es.enter_context(self.nc.psum_tensor(name, list(shape), dt))

    def op(self, eng, fn, r=(), w=(), dma=False):
        deps = {}

        def add(tok):
            if tok is None:
                return
            k, v = tok
            if deps.get(k, -1) < v:
                deps[k] = v
        for key in r:
            st = self.res.get(key)
            if st:
                add(st[0])
        for key in w:
            st = self.res.get(key)
            if st:
                add(st[0])
                for t in st[1]:
                    add(t)
        if dma:
            i = self.ndma[eng]
            self.ndma[eng] += 1
            slot, n = i % self.K, i // self.K
            if n > 0:
                add((('d', eng, slot), n))
            tok = (('d', eng, slot), n + 1)
        else:
            tok = (eng, len(self.ops[eng]))
        waits = []
        for k, v in deps.items():
            if k == eng and not self.SAME[eng]:
                continue
            if self.seen[eng].get(k, -1) >= v:
                continue
            self.seen[eng][k] = v
            waits.append((k, v))
        self.ops[eng].append(dict(fn=fn, waits=waits, tok=tok, dma=dma))
        for key in r:
            self.res.setdefault(key, [None, []])[1].append(tok)
        for key in w:
            self.res[key] = [tok, []]

    def dma(self, q, out, in_, r=(), w=(), **kw):
        self.op(q, lambda e: e.dma_start(out=out, in_=in_, **kw), r=r, w=w, dma=True)

    def build(self):
        nc = self.nc
        es = self.es
        csem = {e: es.enter_context(nc.semaphore(f"s_{e}")) for e in self.ENG}
        dsem = {}
        for e in self.ENG:
            for s in range(min(self.K, self.ndma[e])):
                dsem[('d', e, s)] = es.enter_context(nc.semaphore(f"d_{e}{s}"))
        needs = set()
        for e in self.ENG:
            for o in self.ops[e]:
                for k, v in o['waits']:
                    if not isinstance(k, tuple):
                        needs.add((k, v))
        val = {}
        for e in self.ENG:
            c = 0
            for idx, o in enumerate(self.ops[e]):
                if (e, idx) in needs:
                    c += 1
                    val[(e, idx)] = c
        finals = []
        for e in self.ENG:
            for s in range(min(self.K, self.ndma[e])):
                cnt = (self.ndma[e] - s + self.K - 1) // self.K
                finals.append((('d', e, s), cnt))

        def emit(eobj, eng):
            for idx, o in enumerate(self.ops[eng]):
                for k, v in o['waits']:
                    if isinstance(k, tuple):
                        eobj.wait_ge(dsem[k], 16 * v)
                    else:
                        eobj.wait_ge(csem[k], val[(k, v)])
                ins = o['fn'](eobj)
                if o['dma']:
                    ins.then_inc(dsem[o['tok'][0]], 16)
                elif (eng, idx) in needs:
                    ins.then_inc(csem[eng], 1)
            if eng == 'sp':
                for k, v in finals:
                    eobj.wait_ge(dsem[k], 16 * v)

        with nc.Block() as block:
            @block.tensor
            def _(e):
                emit(e, 'pe')

            @block.scalar
            def _(e):
                emit(e, 'act')

            @block.vector
            def _(e):
                emit(e, 'dve')

            @block.gpsimd
            def _(e):
                emit(e, 'pool')

            @block.sync
            def _(e):
                emit(e, 'sp')
        es.close()
        return nc


def launch(nc, in_maps):
    N_LAUNCH[0] += 1
    if TRACE[0]:
        res = run_bass_kernel_spmd(nc, in_maps, core_ids=list(range(len(in_maps))), trace=True)
        print("LAUNCH exec_time_ns", res.exec_time_ns, flush=True)
        return res.results
    res = run_bass_kernel_spmd(nc, in_maps, core_ids=list(range(len(in_maps))))
    return res.results


D = 2048
NCORE = 8


def build_ada():
    P = Prog()
    cT = P.dram("cT", [128, 16, 2], F32, "ExternalInput")
    w = P.dram("w", [2048, 3072], F32, "ExternalInput")
    b = P.dram("b", [1, 3072], F32, "ExternalInput")
    out = P.dram("out", [2, 3072], F32, "ExternalOutput")
    ct = P.sb([128, 16, 2], F32)
    sc = P.sb([128, 16, 2], F32)
    bt = P.sb([2, 3072], F32)
    ot = P.sb([2, 3072], F32)
    wts = [P.sb([128, 3072], F32) for _ in range(3)]
    pss = [P.ps([128, 512]) for _ in range(6)]
    P.dma('sp', ct[:], cT, w=['ct'])
    P.dma('sp', bt[0:1, :], b, w=['bt0'])
    P.dma('sp', bt[1:2, :], b, w=['bt1'])
    P.op('act', lambda e: e.activation(out=sc[:], in_=ct[:], func=AF.Silu), r=['ct'], w=['sc'])
    for kc in range(16):
        wt = wts[kc % 3]
        q = 'sp' if kc % 2 == 0 else 'pool'
        P.dma(q, wt[:], w[kc * 128:(kc + 1) * 128, :], w=[('wt', kc % 3)])
        for n in range(6):
            P.op('pe', lambda e, kc=kc, n=n, wt=wt: e.matmul(
                pss[n][0:2, :], lhsT=sc[:, kc, :], rhs=wt[:, n * 512:(n + 1) * 512],
                start=(kc == 0), stop=(kc == 15)),
                r=['sc', ('wt', kc % 3)], w=[('ps', n)])
    for n in range(6):
        P.op('dve', lambda e, n=n: e.tensor_tensor(
            out=ot[:, n * 512:(n + 1) * 512], in0=pss[n][0:2, :], in1=bt[:, n * 512:(n + 1) * 512], op=ALU.add),
            r=[('ps', n), 'bt0', 'bt1'], w=[('ot', n)])
    P.dma('sp', out, ot[:], r=[('ot', n) for n in range(6)])
    return P.build()


def run_ada(c, ada_w, ada_b):
    nc = cached("ada", build_ada)
    cT = np.ascontiguousarray(c.T.reshape(16, 128, 2).transpose(1, 0, 2))
    wall = ada_w.reshape(2, 2048, 4, 3072)
    ball = ada_b.reshape(2, 4, 3072)
    in_maps = []
    for i in range(NCORE):
        l, j = i // 4, i % 4
        in_maps.append({"cT": cT, "w": np.ascontiguousarray(wall[l, :, j, :]),
                        "b": np.ascontiguousarray(ball[l, j][None, :])})
    res = launch(nc, in_maps)
    mod = np.zeros((2, 2, 12288), np.float32)
    for i in range(NCORE):
        l, j = i // 4, i % 4
        mod[l, :, j * 3072:(j + 1) * 3072] = res[i]["out"]
    return mod


def fm(a):
    r, c = a.shape
    return np.ascontiguousarray(a.reshape(r // 128, 128, c).transpose(1, 0, 2))


def vec_fm(v):
    return np.ascontiguousarray(v.reshape(-1, 128).T)


def emit_norm(P, xt, nk, ntok, gam, sc, sh, ones, ps_banks, rstd, tmp, outs, tag):
    Dn = nk * 128
    gm = P.sb([128, nk], F32)
    if sc is not None:
        P.op('dve', lambda e: e.scalar_tensor_tensor(out=gm[:], in0=sc[:], scalar=1.0, in1=gam[:],
                                                     op0=ALU.add, op1=ALU.mult),
             r=[(tag, 'sc'), (tag, 'gam')], w=[(tag, 'gm')])
    else:
        P.op('dve', lambda e: e.tensor_copy(out=gm[:], in_=gam[:]), r=[(tag, 'gam')], w=[(tag, 'gm')])
    nt = ntok // 512
    for t in range(nt):
        ts = slice(t * 512, (t + 1) * 512)
        bank = ps_banks[t % len(ps_banks)]
        bk = ('psb', id(bank))
        for k in range(nk):
            j = k % 2
            P.op('act', lambda e, k=k, j=j, ts=ts: e.activation(out=tmp[:, j, :], in_=xt[:, k, ts], func=AF.Square),
                 r=[(tag, 'x', k)], w=[(tag, 'tmp', j)])
            P.op('pe', lambda e, k=k, j=j, bank=bank: e.matmul(bank[:, :], lhsT=ones[:], rhs=tmp[:, j, :],
                                                             start=(k == 0), stop=(k == nk - 1)),
                 r=[(tag, 'tmp', j), 'ones'], w=[bk])
        P.op('act', lambda e, ts=ts, bank=bank: e.activation(out=rstd[:, ts], in_=bank[:, :], func=AF.Sqrt,
                                                            scale=1.0 / Dn, bias=epsb[0][:]),
             r=[bk, 'epsb'], w=[(tag, 'rstd', t)])
        P.op('dve', lambda e, ts=ts: e.reciprocal(out=rstd[:, ts], in_=rstd[:, ts]),
             r=[(tag, 'rstd', t)], w=[(tag, 'rstd', t)])
        for k in range(nk):
            j = k % 2
            P.op('dve', lambda e, k=k, j=j, ts=ts: e.tensor_tensor(out=tmp[:, j, :], in0=xt[:, k, ts], in1=rstd[:, ts],
                                                                 op=ALU.mult),
                 r=[(tag, 'x', k), (tag, 'rstd', t)], w=[(tag, 'tmp', j)])
            for i, ot in enumerate(outs):
                if sh is not None:
                    P.op('act', lambda e, k=k, j=j, ts=ts, ot=ot: e.activation(
                        out=ot[:, k, ts], in_=tmp[:, j, :], func=AF.Identity, scale=gm[:, k:k + 1], bias=sh[:, k:k + 1]),
                        r=[(tag, 'tmp', j), (tag, 'gm'), (tag, 'sh')], w=[(tag, 'h', i, t)])
                else:
                    P.op('act', lambda e, k=k, j=j, ts=ts, ot=ot: e.activation(
                        out=ot[:, k, ts], in_=tmp[:, j, :], func=AF.Copy, scale=gm[:, k:k + 1]),
                        r=[(tag, 'tmp', j), (tag, 'gm')], w=[(tag, 'h', i, t)])


epsb = [None]


def mk_consts(P):
    ones = P.sb([128, 128], F32, "ones")
    P.op('pool', lambda e: e.memset(ones[:], 1.0), w=['ones'])
    eb = P.sb([128, 1], F32, "epsb")
    P.op('pool', lambda e: e.memset(eb[:], 1e-6), w=['epsb'])
    epsb[0] = eb
    return ones


def build_H(out_dt=BF16):
    P = Prog()
    xT = P.dram("xT", [128, 16, 1024], F32, "ExternalInput")
    prm = P.dram("prm", [128, 3, 16], F32, "ExternalInput")
    hT = P.dram("hT", [128, 16, 1024], out_dt, "ExternalOutput")
    ones = mk_consts(P)
    xt = P.sb([128, 16, 1024], F32)
    ht = P.sb([128, 16, 1024], out_dt)
    pt = P.sb([128, 3, 16], F32)
    rstd = P.sb([128, 1024], F32)
    tmp = P.sb([128, 2, 512], F32)
    banks = [P.ps([128, 512]) for _ in range(2)]
    P.dma('sp', pt[:], prm, w=[('n', 'gam'), ('n', 'sc'), ('n', 'sh')])
    for k in range(16):
        P.dma('sp' if k % 2 == 0 else 'pool', xt[:, k, :], xT[:, k, :], w=[('n', 'x', k)])
    emit_norm(P, xt, 16, 1024, pt[:, 0, :], pt[:, 1, :], pt[:, 2, :], ones, banks, rstd, tmp, [ht], 'n')
    P.dma('sp', hT, ht[:], r=[('n', 'h', 0, 0), ('n', 'h', 0, 1)])
    return P.build()


def run_H(xT_full, gam, sc, sh, out_dt=BF16):
    nc = cached(('H', str(out_dt)), lambda: build_H(out_dt))
    in_maps = []
    for i in range(NCORE):
        b = i // 4
        prm = np.stack([vec_fm(gam), vec_fm(sc[b]), vec_fm(sh[b])], axis=1)
        in_maps.append({"xT": fm(xT_full[:, i * 1024:(i + 1) * 1024]), "prm": np.ascontiguousarray(prm)})
    res = launch(nc, in_maps)
    hT = np.concatenate([r["hT"].transpose(1, 0, 2).reshape(2048, 1024) for r in res], axis=1)
    return hT


def build_P(nk, ncol, ntok):
    P = Prog()
    hT = P.dram("hT", [128, nk, ntok], BF16, "ExternalInput")
    w = P.dram("w", [128, nk, ncol], F32, "ExternalInput")
    out = P.dram("out", [ncol, ntok], F32, "ExternalOutput")
    wbf = P.sb([128, nk, ncol], BF16)
    wst = [P.sb([128, ncol], F32) for _ in range(2)]
    hts = [P.sb([128, nk, 512], BF16) for _ in range(2)]
    obs = [P.sb([128, 512], F32) for _ in range(4)]
    banks = [P.ps([128, 512]) for _ in range(6)]
    for k in range(nk):
        j = k % 2
        P.dma('sp' if j == 0 else 'pool', wst[j][:], w[:, k, :], w=[('wst', j)])
        eng = 'dve' if j == 0 else 'pool'
        P.op(eng, lambda e, k=k, j=j: e.tensor_copy(out=wbf[:, k, :], in_=wst[j][:]), r=[('wst', j)], w=[('wbf', k)])
    nct = (ncol + 127) // 128
    it = 0
    for t in range(ntok // 512):
        hb = t % 2
        P.dma('sp', hts[hb][:], hT[:, :, t * 512:(t + 1) * 512], w=[('ht', hb)])
        for ct in range(nct):
            c0 = ct * 128
            cw = min(128, ncol - c0)
            bank = banks[it % 6]
            ob = obs[it % 4]
            for k in range(nk):
                P.op('pe', lambda e, k=k, c0=c0, cw=cw, bank=bank, hb=hb: e.matmul(
                    bank[0:cw, :], lhsT=wbf[:, k, c0:c0 + cw], rhs=hts[hb][:, k, :], start=(k == 0), stop=(k == nk - 1)),
                    r=[('wbf', k), ('ht', hb)], w=[('bank', it % 6)])
            if it % 2 == 0:
                P.op('act', lambda e, cw=cw, bank=bank, ob=ob: e.activation(out=ob[0:cw, :], in_=bank[0:cw, :], func=AF.Copy),
                     r=[('bank', it % 6)], w=[('ob', it % 4)])
            else:
                P.op('dve', lambda e, cw=cw, bank=bank, ob=ob: e.tensor_copy(out=ob[0:cw, :], in_=bank[0:cw, :]),
                     r=[('bank', it % 6)], w=[('ob', it % 4)])
            P.dma('pool' if it % 2 == 0 else 'act', out[c0:c0 + cw, t * 512:(t + 1) * 512], ob[0:cw, :], r=[('ob', it % 4)])
            it += 1
    return P.build()


_cache = {}


def cached(key, fn):
    if key not in _cache:
        _cache[key] = fn()
    return _cache[key]


def run_P(hT_bf, W):
    K_, NC_ = W.shape
    ntok = hT_bf.shape[1]
    ncol = NC_ // NCORE
    assert ncol * NCORE == NC_
    nk = K_ // 128
    nc = cached(('P', nk, ncol, ntok), lambda: build_P(nk, ncol, ntok))
    hfm = fm(hT_bf)
    in_maps = [{"hT": hfm, "w": fm(W[:, i * ncol:(i + 1) * ncol])} for i in range(NCORE)]
    res = launch(nc, in_maps)
    return np.concatenate([r["out"] for r in res], axis=0)


S_ = 4096


def build_LRU():
    P = Prog()
    xin = P.dram("x", [128, 2 * S_], F32, "ExternalInput")
    gin = P.dram("g", [128, 2 * S_], F32, "ExternalInput")
    prm = P.dram("prm", [128, 11], F32, "ExternalInput")
    wri = P.dram("wri", [128, 4, 128], F32, "ExternalInput")
    out = P.dram("out", [128, 2 * S_], F32, "ExternalOutput")
    pt = P.sb([128, 11], F32)
    wt = P.sb([128, 4, 128], F32)
    nsp = P.sb([128, 4], F32)
    X, XC, R, I, M, HF, HB, G = [P.sb([128, S_], F32) for _ in range(8)]
    banks = [P.ps([128, 512]) for _ in range(4)]
    P.dma('sp', pt[:], prm, w=['pt'])
    P.dma('sp', wt[:], wri, w=['wt'])
    P.op('act', lambda e: e.activation(out=nsp[:, 0:2], in_=pt[:, 9:11], func=AF.Exp, scale=-1.0), r=['pt'], w=['nsp'])
    P.op('act', lambda e: e.activation(out=nsp[:, 0:2], in_=nsp[:, 0:2], func=AF.Ln, bias=1.0), r=['nsp'], w=['nsp'])
    P.op('dve', lambda e: e.tensor_scalar(out=nsp[:, 2:4], in0=nsp[:, 0:2], scalar1=-16.0, scalar2=None, op0=ALU.mult),
         r=['nsp'], w=['nsp'])
    P.op('dve', lambda e: e.tensor_scalar(out=nsp[:, 0:2], in0=nsp[:, 0:2], scalar1=-8.0, scalar2=None, op0=ALU.mult),
         r=['nsp'], w=['nsp'])
    bi = 0
    for b in range(2):
        bs = slice(b * S_, (b + 1) * S_)
        P.dma('sp', X[:], xin[:, bs], w=['X'])
        P.dma('pool', G[:], gin[:, bs], w=['G'])
        P.op('dve', lambda e: e.tensor_scalar(out=XC[:], in0=X[:], scalar1=pt[:, 2:3], scalar2=pt[:, 4:5],
                                              op0=ALU.mult, op1=ALU.add), r=['X', 'pt'], w=['XC'])
        for j in (0, 1, 3):
            t0 = max(0, 2 - j)
            t1 = min(S_, S_ + 2 - j)
            P.op('dve', lambda e, j=j, t0=t0, t1=t1: e.scalar_tensor_tensor(
                out=XC[:, t0:t1], in0=X[:, t0 + j - 2:t1 + j - 2], scalar=pt[:, j:j + 1], in1=XC[:, t0:t1],
                op0=ALU.mult, op1=ALU.add), r=['X', 'pt', 'XC'], w=['XC'])
        P.op('act', lambda e: e.activation(out=G[:], in_=G[:], func=AF.Gelu_apprx_tanh), r=['G'], w=['G'])
        for z in range(2):
            for t in range(S_ // 512):
                ts = slice(t * 512, (t + 1) * 512)
                for (widx, dst, bcol, nm) in ((z, R, 5 + z, 'R'), (2 + z, I, 7 + z, 'I')):
                    bank = banks[bi % 4]
                    P.op('pe', lambda e, widx=widx, ts=ts, bank=bank: e.matmul(
                        bank[:, :], lhsT=wt[:, widx, :], rhs=XC[:, ts], start=True, stop=True),
                        r=['wt', 'XC'], w=[('bank', bi % 4)])
                    P.op('act', lambda e, dst=dst, ts=ts, bank=bank, bcol=bcol: e.activation(
                        out=dst[:, ts], in_=bank[:, :], func=AF.Sigmoid, bias=pt[:, bcol:bcol + 1]),
                        r=[('bank', bi % 4), 'pt'], w=[nm])
                    bi += 1
            P.op('act', lambda e, z=z: e.activation(out=M[:], in_=R[:], func=AF.Exp, scale=nsp[:, 2 + z:3 + z]),
                 r=['R', 'nsp'], w=['M'])
            P.op('act', lambda e, z=z: e.activation(out=R[:], in_=R[:], func=AF.Exp, scale=nsp[:, z:z + 1]),
                 r=['R', 'nsp'], w=['R'])
            P.op('act', lambda e: e.activation(out=M[:], in_=M[:], func=AF.Sqrt, scale=-1.0, bias=1.0), r=['M'], w=['M'])
            P.op('dve', lambda e: e.tensor_tensor(out=M[:], in0=M[:], in1=I[:], op=ALU.mult), r=['M', 'I'], w=['M'])
            P.op('dve', lambda e: e.tensor_tensor(out=M[:], in0=M[:], in1=XC[:], op=ALU.mult), r=['M', 'XC'], w=['M'])
            if z == 0:
                P.op('dve', lambda e: e.tensor_tensor_scan(out=HF[:], data0=R[:], data1=M[:], initial=0.0,
                                                           op0=ALU.mult, op1=ALU.add), r=['R', 'M'], w=['HF'])
            else:
                P.op('dve', lambda e: e.tensor_tensor_scan(out=HB[:, ::-1], data0=R[:, ::-1], data1=M[:, ::-1], initial=0.0,
                                                           op0=ALU.mult, op1=ALU.add), r=['R', 'M'], w=['HB'])
        P.op('dve', lambda e: e.tensor_tensor(out=HF[:], in0=HF[:], in1=HB[:], op=ALU.add), r=['HF', 'HB'], w=['HF'])
        P.op('dve', lambda e: e.tensor_tensor(out=HF[:], in0=HF[:], in1=G[:], op=ALU.mult), r=['HF', 'G'], w=['HF'])
        P.dma('sp', out[:, bs], HF[:], r=['HF'])
    return P.build()


def run_LRU(pT, inp, l):
    nc = cached('LRU', build_LRU)
    in_maps = []
    for i in range(NCORE):
        cs = slice(i * 128, (i + 1) * 128)
        prm = np.concatenate([inp["lru_conv_w"][l][:, cs].T, inp["lru_conv_b"][l][cs][:, None],
                              inp["lru_b_r"][l][:, cs].T, inp["lru_b_i"][l][:, cs].T, inp["lru_lambda"][l][:, cs].T], axis=1)
        wri = np.stack([inp["lru_w_r"][l][0, i], inp["lru_w_r"][l][1, i], inp["lru_w_i"][l][0, i], inp["lru_w_i"][l][1, i]], axis=1)
        in_maps.append({"x": np.ascontiguousarray(pT[i * 128:(i + 1) * 128]),
                        "g": np.ascontiguousarray(pT[1024 + i * 128:1024 + (i + 1) * 128]),
                        "prm": np.ascontiguousarray(prm.astype(np.float32)), "wri": np.ascontiguousarray(wri)})
    res = launch(nc, in_maps)
    return np.concatenate([r["out"] for r in res], axis=0)


def build_SGU():
    P = Prog()
    uT = P.dram("uT", [128, 8, 1024], F32, "ExternalInput")
    vtm = P.dram("vtm", [128, 8, 1024], F32, "ExternalInput")
    ng = P.dram("ng", [1, 1024], F32, "ExternalInput")
    wsT = P.dram("wsT", [128, 8, 128], F32, "ExternalInput")
    bs = P.dram("bs", [1, 1024], F32, "ExternalInput")
    out = P.dram("out", [128, 8, 1024], F32, "ExternalOutput")
    U = P.sb([128, 8, 1024], F32)
    V = P.sb([128, 8, 1024], F32)
    NG = P.sb([128, 1024], F32)
    BS = P.sb([128, 8, 128], F32)
    WS = P.sb([128, 8, 128], F32)
    SQ = P.sb([128, 1024], F32)
    st = P.sb([128, 8, 8], F32)
    O = P.sb([128, 8, 1024], F32)
    banks = [P.ps([128, 512]) for _ in range(4)]
    mk_consts(P)
    P.dma('sp', U[:], uT, w=['U'])
    P.dma('pool', V[:], vtm, w=[('V', n) for n in range(8)])
    P.dma('sp', NG[:], ng.partition_broadcast(128), w=['NG'])
    P.dma('sp', BS[:].rearrange("p g i -> p (g i)"), bs.partition_broadcast(128), w=['BS'])
    P.dma('sp', WS[:], wsT, w=['WS'])
    P.op('act', lambda e: e.activation(out=U[:].rearrange("p g t -> p (g t)"), in_=U[:].rearrange("p g t -> p (g t)"),
                                       func=AF.Gelu_apprx_tanh), r=['U'], w=['U'])
    bi = 0
    for n in range(8):
        Vn = V[:, n, :]
        s = st[:, n, :]
        P.op('act', lambda e, Vn=Vn, s=s: e.activation(out=Vn, in_=Vn, func=AF.Gelu_apprx_tanh, accum_out=s[:, 0:1]),
             r=[('V', n)], w=[('V', n), ('st', n)])
        P.op('act', lambda e, Vn=Vn, s=s: e.activation(out=SQ[:], in_=Vn, func=AF.Square, accum_out=s[:, 1:2]),
             r=[('V', n), ('st', n)], w=['SQ', ('st', n)])
        P.op('dve', lambda e, s=s: e.tensor_scalar(out=s[:, 2:3], in0=s[:, 0:1], scalar1=1.0 / 1024, scalar2=None, op0=ALU.mult),
             r=[('st', n)], w=[('st', n)])
        P.op('dve', lambda e, s=s: e.tensor_tensor(out=s[:, 3:4], in0=s[:, 2:3], in1=s[:, 2:3], op=ALU.mult),
             r=[('st', n)], w=[('st', n)])
        P.op('dve', lambda e, s=s: e.scalar_tensor_tensor(out=s[:, 4:5], in0=s[:, 1:2], scalar=1.0 / 1024, in1=s[:, 3:4],
                                                         op0=ALU.mult, op1=ALU.subtract), r=[('st', n)], w=[('st', n)])
        P.op('act', lambda e, s=s: e.activation(out=s[:, 5:6], in_=s[:, 4:5], func=AF.Sqrt, bias=epsb[0][:]),
             r=[('st', n), 'epsb'], w=[('st', n)])
        P.op('dve', lambda e, s=s: e.reciprocal(out=s[:, 5:6], in_=s[:, 5:6]), r=[('st', n)], w=[('st', n)])
        P.op('dve', lambda e, Vn=Vn, s=s: e.tensor_scalar(out=Vn, in0=Vn, scalar1=s[:, 2:3], scalar2=s[:, 5:6],
                                                        op0=ALU.subtract, op1=ALU.mult), r=[('V', n), ('st', n)], w=[('V', n)])
        P.op('dve', lambda e, Vn=Vn: e.tensor_tensor(out=Vn, in0=Vn, in1=NG[:], op=ALU.mult), r=[('V', n), 'NG'], w=[('V', n)])
        for gh in range(2):
            bank = banks[bi % 4]
            for gg in range(4):
                g = gh * 4 + gg
                P.op('pe', lambda e, g=g, gg=gg, n=n, bank=bank: e.matmul(
                    bank[:, gg * 128:(gg + 1) * 128], lhsT=V[:, n, g * 128:(g + 1) * 128], rhs=WS[:, g, :],
                    start=True, stop=True), r=[('V', n), 'WS'], w=[('bank', bi % 4, gg)])
            Ov = O[:, gh * 4:(gh + 1) * 4, n * 128:(n + 1) * 128]
            P.op('dve', lambda e, bank=bank, gh=gh, Ov=Ov: e.tensor_tensor(
                out=Ov, in0=bank[:, :].rearrange("p (g i) -> p g i", g=4), in1=BS[:, gh * 4:(gh + 1) * 4, :], op=ALU.add),
                r=[('bank', bi % 4, gg) for gg in range(4)] + ['BS'], w=[('O', n, gh)])
            P.op('pool', lambda e, gh=gh, n=n, Ov=Ov: e.tensor_tensor(
                out=Ov, in0=Ov, in1=U[:, gh * 4:(gh + 1) * 4, n * 128:(n + 1) * 128], op=ALU.mult),
                r=[('O', n, gh), 'U'], w=[('O', n, gh)])
            bi += 1
    P.dma('sp', out, O[:], r=[('O', n, gh) for n in range(8) for gh in range(2)])
    return P.build()


def run_SGU(pT, inp, l):
    nc = cached('SGU', lambda: (mk_dummy(), build_SGU())[1])
    in_maps = []
    wsT = np.ascontiguousarray(inp["sgu_w"][l].transpose(2, 0, 1))
    for i in range(NCORE):
        ts = slice(i * 1024, (i + 1) * 1024)
        uT = pT[5120:6144, ts].reshape(8, 128, 1024).transpose(1, 0, 2)
        vtm = pT[6144:7168, ts].T.reshape(8, 128, 1024).transpose(1, 0, 2)
        in_maps.append({"uT": np.ascontiguousarray(uT), "vtm": np.ascontiguousarray(vtm),
                        "ng": np.ascontiguousarray(inp["sgu_norm_g"][l][None, :]), "wsT": wsT,
                        "bs": np.ascontiguousarray(inp["sgu_b"][l].reshape(1, 1024))})
    res = launch(nc, in_maps)
    return np.concatenate([r["out"].transpose(1, 0, 2).reshape(1024, 1024) for r in res], axis=1)


def mk_dummy():
    pass


TWO_PI = 6.283185


def build_RET():
    P = Prog()
    qk = P.dram("qk", [32, 4, 2 * S_], F32, "ExternalInput")
    vtm = P.dram("vtm", [128, 64, 128], F32, "ExternalInput")
    gtm = P.dram("gtm", [128, 64, 128], F32, "ExternalInput")
    pos = P.dram("pos", [1, 2 * S_], I32, "ExternalInput")
    c_intra = P.dram("c_intra", [128, 128], F32, "ExternalInput")
    c_decq = P.dram("c_decq", [32, 2, 2, 128], F32, "ExternalInput")
    c_vdec = P.dram("c_vdec", [128, 2], F32, "ExternalInput")
    c_misc = P.dram("c_misc", [32, 4], F32, "ExternalInput")
    idd = P.dram("idd", [128, 128], F32, "ExternalInput")
    out = P.dram("out", [128, 64, 128], F32, "ExternalOutput")
    mk_consts(P)
    QK = P.sb([32, 4, S_], F32)
    V = P.sb([128, 32, 128], F32)
    G = P.sb([128, 32, 128], F32)
    ST = P.sb([32, 32, 2, 256], F32)
    INTRA = P.sb([128, 128], F32)
    DECQ = P.sb([32, 2, 2, 128], F32)
    VDEC = P.sb([128, 2], F32)
    MISC = P.sb([32, 4], F32)
    IDN = P.sb([128, 128], F32)
    SEG = 1024
    PI = P.sb([32, SEG], I32)
    T0, T1, T2, SN, CS, TA, TB = [P.sb([32, SEG], F32) for _ in range(7)]
    KTM = [P.sb([128, 64], F32) for _ in range(2)]
    VFB = [P.sb([128, 256], F32) for _ in range(2)]
    SCT = [P.sb([128, 128], F32) for _ in range(2)]
    QFB = [P.sb([32, 2, 2, 128], F32) for _ in range(2)]
    OS = [P.sb([128, 128], F32) for _ in range(2)]
    SQ = P.sb([128, 128], F32)
    stt = P.sb([128, 64, 8], F32)
    bk = [P.ps([128, 512]) for _ in range(6)]
    for (t_, d_, k_) in ((INTRA, c_intra, 'INTRA'), (DECQ, c_decq, 'DECQ'), (VDEC, c_vdec, 'VDEC'), (MISC, c_misc, 'MISC'),
                        (IDN, idd, 'IDN')):
        P.dma('sp', t_[:], d_, w=[k_])
    for b in range(2):
        P.dma('sp', QK[:], qk[:, :, b * S_:(b + 1) * S_], w=[('QK', s) for s in range(4)])
        P.dma('pool', V[:], vtm[:, b * 32:(b + 1) * 32, :], w=['V'])
        P.dma('pool', G[:], gtm[:, b * 32:(b + 1) * 32, :], w=[('G', n) for n in range(32)])
        P.op('act', lambda e: e.activation(out=G[:].rearrange("p n e -> p (n e)"), in_=G[:].rearrange("p n e -> p (n e)"),
                                           func=AF.Silu), r=[('G', n) for n in range(32)], w=[('G', n) for n in range(32)])
        for s in range(S_ // SEG):
            ss = slice(s * SEG, (s + 1) * SEG)
            P.dma('sp', PI[:], pos[:, b * S_ + s * SEG: b * S_ + (s + 1) * SEG].partition_broadcast(32), w=['PI'])
            P.op('dve', lambda e: e.tensor_copy(out=T0[:], in_=PI[:]), r=['PI'], w=['T0'])
            P.op('dve', lambda e: e.tensor_scalar(out=T0[:], in0=T0[:], scalar1=MISC[:, 0:1], scalar2=MISC[:, 1:2],
                                                  op0=ALU.mult, op1=ALU.mult), r=['T0', 'MISC'], w=['T0'])
            for (dst, shift) in ((SN, 0.0), (CS, 0.25)):
                nm = 'SN' if dst is SN else 'CS'
                P.op('dve', lambda e, shift=shift: e.tensor_scalar(out=T1[:], in0=T0[:], scalar1=shift, scalar2=None, op0=ALU.add),
                     r=['T0'], w=['T1'])
                P.op('dve', lambda e: e.tensor_copy(out=PI[:], in_=T1[:]), r=['T1'], w=['PI'])
                P.op('dve', lambda e: e.tensor_copy(out=T2[:], in_=PI[:]), r=['PI'], w=['T2'])
                P.op('dve', lambda e: e.tensor_tensor(out=T1[:], in0=T1[:], in1=T2[:], op=ALU.subtract), r=['T1', 'T2'], w=['T1'])
                P.op('dve', lambda e: e.tensor_scalar(out=T2[:], in0=T1[:], scalar1=0.5, scalar2=None, op0=ALU.is_gt),
                     r=['T1'], w=['T2'])
                P.op('dve', lambda e: e.tensor_tensor(out=T1[:], in0=T1[:], in1=T2[:], op=ALU.subtract), r=['T1', 'T2'], w=['T1'])
                P.op('dve', lambda e: e.tensor_scalar(out=T2[:], in0=T1[:], scalar1=-0.5, scalar2=None, op0=ALU.is_lt),
                     r=['T1'], w=['T2'])
                P.op('dve', lambda e: e.tensor_tensor(out=T1[:], in0=T1[:], in1=T2[:], op=ALU.add), r=['T1', 'T2'], w=['T1'])
                P.op('act', lambda e, dst=dst: e.activation(out=dst[:], in_=T1[:], func=AF.Sin, scale=TWO_PI), r=['T1'], w=[nm])
            for base in (0, 2):
                x1 = QK[:, base, ss]
                x2 = QK[:, base + 1, ss]
                k1, k2 = ('QK', base), ('QK', base + 1)
                eng = 'dve' if base == 0 else 'pool'
                ta, tb = (TA, TB) if base == 0 else (T0, T2)
                kta, ktb = ('TA', 'TB') if base == 0 else ('T0', 'T2')
                P.op(eng, lambda e, x1=x1, ta=ta: e.tensor_tensor(out=ta[:], in0=x1, in1=CS[:], op=ALU.mult), r=[k1, 'CS'], w=[kta])
                P.op(eng, lambda e, x2=x2, tb=tb: e.tensor_tensor(out=tb[:], in0=x2, in1=SN[:], op=ALU.mult), r=[k2, 'SN'], w=[ktb])
                P.op(eng, lambda e, ta=ta, tb=tb: e.tensor_tensor(out=ta[:], in0=ta[:], in1=tb[:], op=ALU.subtract), r=[kta, ktb], w=[kta])
                P.op(eng, lambda e, x1=x1, tb=tb: e.tensor_tensor(out=tb[:], in0=x1, in1=SN[:], op=ALU.mult), r=[k1, 'SN'], w=[ktb])
                P.op(eng, lambda e, x2=x2: e.tensor_tensor(out=x2, in0=x2, in1=CS[:], op=ALU.mult), r=[k2, 'CS'], w=[k2])
                P.op(eng, lambda e, x2=x2, tb=tb: e.tensor_tensor(out=x2, in0=x2, in1=tb[:], op=ALU.add), r=[k2, ktb], w=[k2])
                P.op(eng, lambda e, x1=x1, ta=ta: e.tensor_copy(out=x1, in_=ta[:]), r=[kta], w=[k1])
        P.op('pool', lambda e: e.memset(ST[:, 0, :, 0:128], 0.0), w=[('ST', 0)])
        P.op('pool', lambda e: e.memset(ST[:, 31, :, 128:256], 0.0), w=[('STB', 31)])
        it = 0
        for n in range(32):
            cs = slice(n * 128, (n + 1) * 128)
            bank = bk[it % 2]
            bkk = ('bk', it % 2)
            ktm = KTM[it % 2]
            vfb = VFB[it % 2]
            P.op('pe', lambda e, bank=bank, cs=cs: e.transpose(out=bank[:, 0:32], in_=QK[:, 2, cs], identity=IDN[0:32, 0:32]),
                 r=[('QK', 2), 'IDN'], w=[bkk])
            P.op('pe', lambda e, bank=bank, cs=cs: e.transpose(out=bank[:, 32:64], in_=QK[:, 3, cs], identity=IDN[0:32, 0:32]),
                 r=[('QK', 3), 'IDN'], w=[bkk])
            P.op('act', lambda e, bank=bank, ktm=ktm: e.activation(out=ktm[:], in_=bank[:, 0:64], func=AF.Copy),
                 r=[bkk], w=[('ktm', it % 2)])
            P.op('dve', lambda e, n=n, vfb=vfb: e.tensor_scalar(out=vfb[:, 0:128], in0=V[:, n, :], scalar1=VDEC[:, 0:1], scalar2=None,
                                                              op0=ALU.mult), r=['V', 'VDEC'], w=[('vfb', it % 2)])
            P.op('dve', lambda e, n=n, vfb=vfb: e.tensor_scalar(out=vfb[:, 128:256], in0=V[:, n, :], scalar1=VDEC[:, 1:2], scalar2=None,
                                                              op0=ALU.mult), r=['V', 'VDEC', ('vfb', it % 2)], w=[('vfb', it % 2)])
            b2 = bk[2 + it % 2]
            b2k = ('bk', 2 + it % 2)
            for h in range(2):
                P.op('pe', lambda e, h=h, b2=b2, ktm=ktm, vfb=vfb: e.matmul(
                    b2[0:32, h * 256:(h + 1) * 256], lhsT=ktm[:, h * 32:(h + 1) * 32], rhs=vfb[:], start=True, stop=True),
                    r=[('ktm', it % 2), ('vfb', it % 2)], w=[b2k])
            if n < 31:
                P.op('act', lambda e, n=n, b2=b2: e.activation(
                    out=ST[:, n + 1, :, 0:128], in_=b2[0:32, :].rearrange("p (h x) -> p h x", h=2)[:, :, 0:128], func=AF.Copy),
                    r=[b2k], w=[('ST', n + 1)])
            if n > 0:
                P.op('act', lambda e, n=n, b2=b2: e.activation(
                    out=ST[:, n - 1, :, 128:256], in_=b2[0:32, :].rearrange("p (h x) -> p h x", h=2)[:, :, 128:256], func=AF.Copy),
                    r=[b2k], w=[('STB', n - 1)])
            it += 1
        for n in range(1, 32):
            P.op('dve', lambda e, n=n: e.scalar_tensor_tensor(
                out=ST[:, n, :, 0:128], in0=ST[:, n - 1, :, 0:128], scalar=MISC[:, 2:3], in1=ST[:, n, :, 0:128],
                op0=ALU.mult, op1=ALU.add), r=[('ST', n - 1), ('ST', n), 'MISC'], w=[('ST', n)])
        for n in range(30, -1, -1):
            P.op('dve', lambda e, n=n: e.scalar_tensor_tensor(
                out=ST[:, n, :, 128:256], in0=ST[:, n + 1, :, 128:256], scalar=MISC[:, 2:3], in1=ST[:, n, :, 128:256],
                op0=ALU.mult, op1=ALU.add), r=[('STB', n + 1), ('STB', n), 'MISC'], w=[('STB', n)])
        for n in range(32):
            cs = slice(n * 128, (n + 1) * 128)
            j = n % 2
            bs_, bsk = bk[j], ('bk', j)
            P.op('pe', lambda e, bs_=bs_, cs=cs: e.matmul(bs_[:, 0:128], lhsT=QK[:, 2, cs], rhs=QK[:, 0, cs], start=True, stop=False),
                 r=[('QK', 0), ('QK', 2)], w=[bsk])
            P.op('pe', lambda e, bs_=bs_, cs=cs: e.matmul(bs_[:, 0:128], lhsT=QK[:, 3, cs], rhs=QK[:, 1, cs], start=False, stop=True),
                 r=[('QK', 1), ('QK', 3)], w=[bsk])
            P.op('dve', lambda e, bs_=bs_, j=j: e.tensor_tensor(out=SCT[j][:], in0=bs_[:, 0:128], in1=INTRA[:], op=ALU.mult),
                 r=[bsk, 'INTRA'], w=[('sct', j)])
            for dr in range(2):
                P.op('pool', lambda e, dr=dr, j=j, cs=cs: e.tensor_tensor(out=QFB[j][:, dr, :, :], in0=QK[:, 0:2, cs],
                                                                        in1=DECQ[:, dr, :, :], op=ALU.mult),
                     r=[('QK', 0), ('QK', 1), 'DECQ'], w=[('qfb', j, dr)])
            bo, bok = bk[4 + j], ('bk', 4 + j)
            P.op('pe', lambda e, bo=bo, j=j, n=n: e.matmul(bo[:, 0:128], lhsT=SCT[j][:], rhs=V[:, n, :], start=True, stop=False),
                 r=[('sct', j), 'V'], w=[bok])
            for dr in range(2):
                for h in range(2):
                    last = (dr == 1 and h == 1)
                    P.op('pe', lambda e, bo=bo, j=j, n=n, dr=dr, h=h, last=last: e.matmul(
                        bo[:, 0:128], lhsT=QFB[j][:, dr, h, :], rhs=ST[:, n, h, dr * 128:(dr + 1) * 128], start=False, stop=last),
                        r=[('qfb', j, dr), ('ST', n), ('STB', n)], w=[bok])
            s = stt[:, b * 32 + n, :]
            sk = ('stt', b * 32 + n)
            P.op('act', lambda e, bo=bo, j=j, s=s: e.activation(out=OS[j][:], in_=bo[:, 0:128], func=AF.Copy, accum_out=s[:, 0:1]),
                 r=[bok], w=[('os', j), sk])
            P.op('act', lambda e, j=j, s=s: e.activation(out=SQ[:], in_=OS[j][:], func=AF.Square, accum_out=s[:, 1:2]),
                 r=[('os', j), sk], w=['SQ', sk])
            P.op('dve', lambda e, s=s: e.tensor_scalar(out=s[:, 2:3], in0=s[:, 0:1], scalar1=1.0 / 128, scalar2=None, op0=ALU.mult),
                 r=[sk], w=[sk])
            P.op('dve', lambda e, s=s: e.tensor_tensor(out=s[:, 3:4], in0=s[:, 2:3], in1=s[:, 2:3], op=ALU.mult), r=[sk], w=[sk])
            P.op('dve', lambda e, s=s: e.scalar_tensor_tensor(out=s[:, 4:5], in0=s[:, 1:2], scalar=1.0 / 128, in1=s[:, 3:4],
                                                             op0=ALU.mult, op1=ALU.subtract), r=[sk], w=[sk])
            P.op('act', lambda e, s=s: e.activation(out=s[:, 5:6], in_=s[:, 4:5], func=AF.Sqrt, bias=epsb[0][:]),
                 r=[sk, 'epsb'], w=[sk])
            P.op('dve', lambda e, s=s: e.reciprocal(out=s[:, 5:6], in_=s[:, 5:6]), r=[sk], w=[sk])
            P.op('dve', lambda e, j=j, s=s: e.tensor_scalar(out=OS[j][:], in0=OS[j][:], scalar1=s[:, 2:3], scalar2=s[:, 5:6],
                                                          op0=ALU.subtract, op1=ALU.mult), r=[('os', j), sk], w=[('os', j)])
            P.op('dve', lambda e, j=j, n=n: e.tensor_tensor(out=G[:, n, :], in0=G[:, n, :], in1=OS[j][:], op=ALU.mult),
                 r=[('os', j), ('G', n)], w=[('G', n)])
        P.dma('sp', out[:, b * 32:(b + 1) * 32, :], G[:], r=[('G', n) for n in range(32)])
    return P.build()


def ret_consts(h):
    C = 128
    lg = np.log1p(-np.exp2(-5.0 - h))
    i = np.arange(C)
    intra = 0.125 * np.exp(np.abs(i[:, None] - i[None, :]) * lg)
    decq = np.zeros((32, 2, 2, C), np.float64)
    decq[:, 0, :, :] = 0.125 * np.exp((i + 1.0) * lg)
    decq[:, 1, :, :] = 0.125 * np.exp((C - i) * lg)
    vdec = np.stack([np.exp((C - 1 - i) * lg), np.exp(i * lg)], axis=1)
    inv_freq = (10000.0 ** (-np.arange(0, 64, 2, dtype=np.float32) / 64)).astype(np.float32)
    misc = np.zeros((32, 4), np.float32)
    misc[:, 0] = inv_freq
    misc[:, 1] = 1.0 / (2 * np.pi)
    misc[:, 2] = np.exp(C * lg)
    return (intra.astype(np.float32), decq.astype(np.float32), vdec.astype(np.float32), misc)


def tm_chunks(aT):
    return np.ascontiguousarray(aT.T.reshape(64, 128, aT.shape[0]).transpose(1, 0, 2))


def run_RET(pT, positions):
    nc = cached('RET', build_RET)
    in_maps = []
    idd = np.eye(128, dtype=np.float32)
    posr = np.ascontiguousarray(positions.reshape(1, -1).astype(np.int32))
    for i in range(NCORE):
        q = pT[2048 + i * 64:2048 + (i + 1) * 64]
        k = pT[2560 + i * 64:2560 + (i + 1) * 64]
        qk = np.stack([q[0:32], q[32:64], k[0:32], k[32:64]], axis=1)
        intra, decq, vdec, misc = ret_consts(i)
        in_maps.append({"qk": np.ascontiguousarray(qk), "vtm": tm_chunks(pT[3072 + i * 128:3072 + (i + 1) * 128]),
                        "gtm": tm_chunks(pT[4096 + i * 128:4096 + (i + 1) * 128]), "pos": posr,
                        "c_intra": intra, "c_decq": decq, "c_vdec": vdec, "c_misc": misc, "idd": idd})
    res = launch(nc, in_maps)
    return np.concatenate([r["out"].transpose(2, 1, 0).reshape(128, 8192) for r in res], axis=0)


def build_M():
    P = Prog()
    ysT = P.dram("ysT", [128, 4, 8, 1024], F32, "ExternalInput")
    lgT = P.dram("lgT", [128, 16, 4, 1024], F32, "ExternalInput")
    xT = P.dram("xT", [128, 16, 1024], F32, "ExternalInput")
    g1 = P.dram("g1", [128, 16], F32, "ExternalInput")
    wb = P.dram("wb", [128, 4, 8, 2048], F32, "ExternalInput")
    wo = P.dram("wo", [128, 16, 2048], F32, "ExternalInput")
    out = P.dram("out", [128, 16, 1024], F32, "ExternalOutput")
    X = P.sb([128, 16, 512], F32)
    YS = P.sb([128, 4, 8, 512], BF16)
    YST = [P.sb([128, 8, 512], F32) for _ in range(2)]
    MG = P.sb([128, 16, 512], BF16)
    G1 = P.sb([128, 16], F32)
    WBS = [P.sb([128, 4, 8, 128], F32) for _ in range(2)]
    WBB = [P.sb([128, 4, 8, 128], BF16) for _ in range(2)]
    LG = [P.sb([128, 4, 512], F32) for _ in range(2)]
    ACC = P.sb([128, 512], F32)
    TMP = P.sb([128, 512], F32)
    WOS = [P.sb([128, 16, 128], F32) for _ in range(2)]
    WOB = [P.sb([128, 16, 128], BF16) for _ in range(2)]
    banks = [P.ps([128, 512]) for _ in range(4)]
    P.dma('sp', G1[:], g1, w=['G1'])
    bi = 0
    for hf in range(2):
        hs = slice(hf * 512, (hf + 1) * 512)
        P.dma('sp', X[:], xT[:, :, hs], w=['X'])
        for n in range(4):
            P.dma('pool', YST[n % 2][:], ysT[:, n, :, hs], w=[('yst', n % 2)])
            P.op('dve' if n % 2 == 0 else 'pool', lambda e, n=n: e.tensor_copy(out=YS[:, n, :, :], in_=YST[n % 2][:]),
                 r=[('yst', n % 2)], w=[('YS', n)])
        for dt in range(16):
            j = dt % 2
            P.dma('sp', WBS[j][:], wb[:, :, :, dt * 128:(dt + 1) * 128], w=[('wbs', j)])
            P.op('pool', lambda e, j=j: e.tensor_copy(out=WBB[j][:].rearrange("p n c d -> p (n c d)"),
                                                      in_=WBS[j][:].rearrange("p n c d -> p (n c d)")),
                 r=[('wbs', j)], w=[('wbb', j)])
            P.dma('act', LG[j][:], lgT[:, dt, :, hs], w=[('lg', j)])
            P.op('act', lambda e, j=j: e.activation(out=LG[j][:].rearrange("p n t -> p (n t)"),
                                                    in_=LG[j][:].rearrange("p n t -> p (n t)"), func=AF.Sigmoid),
                 r=[('lg', j)], w=[('lg', j)])
            for n in range(4):
                bank = banks[bi % 4]
                bkk = ('bank', bi % 4)
                for c in range(8):
                    P.op('pe', lambda e, n=n, c=c, j=j, bank=bank: e.matmul(
                        bank[:, :], lhsT=WBB[j][:, n, c, :], rhs=YS[:, n, c, :], start=(c == 0), stop=(c == 7)),
                        r=[('wbb', j), ('YS', n)], w=[bkk])
                if n == 0:
                    P.op('dve', lambda e, j=j, bank=bank: e.tensor_tensor(out=ACC[:], in0=bank[:, :], in1=LG[j][:, 0, :], op=ALU.mult),
                         r=[bkk, ('lg', j)], w=['ACC'])
                else:
                    P.op('dve', lambda e, j=j, n=n, bank=bank: e.tensor_tensor(out=TMP[:], in0=bank[:, :], in1=LG[j][:, n, :],
                                                                             op=ALU.mult), r=[bkk, ('lg', j)], w=['TMP'])
                    if n < 3:
                        P.op('dve', lambda e: e.tensor_tensor(out=ACC[:], in0=ACC[:], in1=TMP[:], op=ALU.add),
                             r=['ACC', 'TMP'], w=['ACC'])
                    else:
                        P.op('dve', lambda e, dt=dt: e.tensor_tensor(out=MG[:, dt, :], in0=ACC[:], in1=TMP[:], op=ALU.add),
                             r=['ACC', 'TMP'], w=[('MG', dt)])
                bi += 1
        for dp in range(16):
            j = dp % 2
            P.dma('sp', WOS[j][:], wo[:, :, dp * 128:(dp + 1) * 128], w=[('wos', j)])
            P.op('pool', lambda e, j=j: e.tensor_copy(out=WOB[j][:].rearrange("p c d -> p (c d)"),
                                                      in_=WOS[j][:].rearrange("p c d -> p (c d)")),
                 r=[('wos', j)], w=[('wob', j)])
            bank = banks[bi % 4]
            bkk = ('bank', bi % 4)
            for c in range(16):
                P.op('pe', lambda e, c=c, j=j, bank=bank: e.matmul(bank[:, :], lhsT=WOB[j][:, c, :], rhs=MG[:, c, :],
                                                                 start=(c == 0), stop=(c == 15)),
                     r=[('wob', j), ('MG', c)], w=[bkk])
            P.op('dve', lambda e, dp=dp, bank=bank: e.scalar_tensor_tensor(
                out=X[:, dp, :], in0=bank[:, :], scalar=G1[:, dp:dp + 1], in1=X[:, dp, :], op0=ALU.mult, op1=ALU.add),
                r=[bkk, 'G1', 'X'], w=[('XO', dp)])
            bi += 1
        P.dma('sp', out[:, :, hs], X[:], r=[('XO', dp) for dp in range(16)] + ['X'], w=['X'])
    return P.build()


def run_M(ysT4, pT, xT_full, g1, w_branch, w_out):
    nc = cached('M', build_M)
    wb = np.ascontiguousarray(w_branch.reshape(4, 8, 128, 2048).transpose(2, 0, 1, 3))
    wo = fm(w_out)
    in_maps = []
    for i in range(NCORE):
        ts = slice(i * 1024, (i + 1) * 1024)
        ys = ysT4[:, :, ts].reshape(4, 8, 128, 1024).transpose(2, 0, 1, 3)
        lg = pT[10624:18816, ts].reshape(4, 16, 128, 1024).transpose(2, 1, 0, 3)
        in_maps.append({"ysT": np.ascontiguousarray(ys), "lgT": np.ascontiguousarray(lg), "xT": fm(xT_full[:, ts]),
                        "g1": vec_fm(g1[i // 4]), "wb": wb, "wo": wo})
    res = launch(nc, in_maps)
    return np.concatenate([r["out"].transpose(1, 0, 2).reshape(2048, 1024) for r in res], axis=1)


def build_E():
    P = Prog()
    xT = P.dram("xT", [128, 16, 1024], F32, "ExternalInput")
    prm = P.dram("prm", [128, 4, 16], F32, "ExternalInput")
    wr = P.dram("wr", [128, 16, 36], F32, "ExternalInput")
    rb = P.dram("rb", [1, 36], F32, "ExternalInput")
    idd = P.dram("idd", [128, 128], F32, "ExternalInput")
    wgu = P.dram("wgu", [32, 128, 16, 1024], F32, "ExternalInput")
    wdn = P.dram("wdn", [32, 128, 4, 2048], F32, "ExternalInput")
    wscr = P.dram("wscr", [32, 1024], F32, "ExternalOutput")
    out = P.dram("out", [128, 16, 1024], F32, "ExternalOutput")
    ones = mk_consts(P)
    XA = P.sb([128, 16, 1024], F32)
    H2B = P.sb([128, 16, 1024], BF16)
    H2F = P.sb([128, 16 * 512], F32)
    H2Fv = H2F[:].rearrange("p (k t) -> p k t", k=16)
    RSTD = P.sb([128, 1024], F32)
    TMP = P.sb([128, 2, 512], F32)
    PT = P.sb([128, 4, 16], F32)
    WR = P.sb([128, 16, 36], F32)
    RB = P.sb([128, 36], F32)
    IDN = P.sb([128, 128], F32)
    L = P.sb([128, 8, 36], F32)
    sm = P.sb([128, 8, 16], F32)
    OH = P.sb([128, 4], F32)
    GE = P.sb([128, 4], F32)
    IG = P.sb([128, 8], F32)
    IG2 = P.sb([128, 8], F32)
    M1 = P.sb([128, 8], F32)
    M2 = P.sb([128, 8], F32)
    WM = P.sb([128, 8], F32)
    WF = P.sb([128, 8, 32], F32)
    WGT = P.sb([32, 1024], F32)
    WB = [P.sb([128, 1024], F32) for _ in range(2)]
    GUB = [[P.sb([128, 16, 128], BF16) for _ in range(2)] for _ in range(2)]
    TMPA = [P.sb([128, 512], F32) for _ in range(2)]
    ACTB = P.sb([128, 4, 1024], BF16)
    WDB = [P.sb([128, 4, 512], BF16) for _ in range(2)]
    GS = [H2F[:, 0:2048].rearrange("p (k j) -> p k j", k=16), H2F[:, 2048:4096].rearrange("p (k j) -> p k j", k=16)]
    WDS = H2F[:, 4096:6144].rearrange("p (c d) -> p c d", c=4)
    XR = H2F[:, 6144:7168]
    banks = [P.ps([128, 512]) for _ in range(8)]
    P.dma('sp', PT[:], prm, w=[('n', 'gam'), ('n', 'sc'), ('n', 'sh'), 'G2'])
    P.dma('sp', WR[:], wr, w=['WR'])
    P.dma('sp', RB[:], rb.partition_broadcast(128), w=['RB'])
    P.dma('sp', IDN[:], idd, w=['IDN'])
    for k in range(16):
        P.dma('sp' if k % 2 == 0 else 'pool', XA[:, k, :], xT[:, k, :], w=[('n', 'x', k)])
    h2f_keys = []

    def after_tile(t):
        for tt in range(4):
            bank = banks[2 + (tt % 2)]
            bkk = ('bk', 2 + (tt % 2))
            for k in range(16):
                P.op('pe', lambda e, k=k, tt=tt, bank=bank: e.matmul(
                    bank[:, 0:36], lhsT=H2Fv[:, k, tt * 128:(tt + 1) * 128], rhs=WR[:, k, :], start=(k == 0), stop=(k == 15)),
                    r=[('n', 'h', 1, t), 'WR'], w=[bkk])
            P.op('dve', lambda e, tt=tt, bank=bank, t=t: e.tensor_tensor(out=L[:, t * 4 + tt, :], in0=bank[:, 0:36], in1=RB[:],
                                                                       op=ALU.add), r=[bkk, 'RB'], w=[('L', t * 4 + tt)])
    emit_norm_cb(P, XA, 16, 1024, PT[:, 0, :], PT[:, 1, :], PT[:, 2, :], ones, banks[0:2], RSTD, TMP,
                 [(H2B, False), (H2Fv, True)], 'n', after_tile)
    for tt in range(8):
        s = sm[:, tt, :]
        sk = ('sm', tt)
        lg = L[:, tt, 0:4]
        Lk = ('L', tt)
        P.op('dve', lambda e, lg=lg, s=s: e.tensor_reduce(out=s[:, 0:1], in_=lg, axis=AX.X, op=ALU.max), r=[Lk], w=[sk])
        P.op('dve', lambda e, lg=lg, s=s: e.tensor_scalar(out=OH[:], in0=lg, scalar1=s[:, 0:1], scalar2=None, op0=ALU.is_ge),
             r=[Lk, sk], w=['OH'])
        P.op('dve', lambda e, s=s: e.tensor_scalar(out=s[:, 1:2], in0=s[:, 0:1], scalar1=-1.0, scalar2=None, op0=ALU.mult),
             r=[sk], w=[sk])
        P.op('act', lambda e, lg=lg, s=s: e.activation(out=GE[:], in_=lg, func=AF.Exp, bias=s[:, 1:2], accum_out=s[:, 2:3]),
             r=[Lk, sk], w=['GE', sk])
        P.op('dve', lambda e, s=s: e.reciprocal(out=s[:, 3:4], in_=s[:, 2:3]), r=[sk], w=[sk])
        for g in range(4):
            le = L[:, tt, 4 + g * 8:4 + (g + 1) * 8]
            if g == 0:
                P.op('dve', lambda e, le=le: e.tensor_scalar(out=IG[:], in0=le, scalar1=OH[:, 0:1], scalar2=None, op0=ALU.mult),
                     r=[Lk, 'OH'], w=['IG'])
            else:
                P.op('dve', lambda e, le=le, g=g: e.scalar_tensor_tensor(out=IG[:], in0=le, scalar=OH[:, g:g + 1], in1=IG[:],
                                                                       op0=ALU.mult, op1=ALU.add), r=[Lk, 'OH', 'IG'], w=['IG'])
        P.op('dve', lambda e, s=s: e.tensor_reduce(out=s[:, 4:5], in_=IG[:], axis=AX.X, op=ALU.max), r=['IG', sk], w=[sk])
        P.op('dve', lambda e, s=s: e.tensor_scalar(out=M1[:], in0=IG[:], scalar1=s[:, 4:5], scalar2=None, op0=ALU.is_ge),
             r=['IG', sk], w=['M1'])
        P.op('dve', lambda e: e.scalar_tensor_tensor(out=IG2[:], in0=M1[:], scalar=-1e30, in1=IG[:], op0=ALU.mult, op1=ALU.add),
             r=['M1', 'IG'], w=['IG2'])
        P.op('dve', lambda e, s=s: e.tensor_reduce(out=s[:, 5:6], in_=IG2[:], axis=AX.X, op=ALU.max), r=['IG2', sk], w=[sk])
        P.op('dve', lambda e, s=s: e.tensor_scalar(out=M2[:], in0=IG2[:], scalar1=s[:, 5:6], scalar2=None, op0=ALU.is_ge),
             r=['IG2', sk], w=['M2'])
        P.op('dve', lambda e, s=s: e.tensor_tensor(out=s[:, 6:7], in0=s[:, 5:6], in1=s[:, 4:5], op=ALU.subtract), r=[sk], w=[sk])
        P.op('act', lambda e, s=s: e.activation(out=s[:, 7:8], in_=s[:, 6:7], func=AF.Exp), r=[sk], w=[sk])
        P.op('dve', lambda e, s=s: e.tensor_scalar(out=s[:, 8:9], in0=s[:, 7:8], scalar1=1.0, scalar2=None, op0=ALU.add),
             r=[sk], w=[sk])
        P.op('dve', lambda e, s=s: e.reciprocal(out=s[:, 8:9], in_=s[:, 8:9]), r=[sk], w=[sk])
        P.op('dve', lambda e, s=s: e.tensor_tensor(out=s[:, 9:10], in0=s[:, 8:9], in1=s[:, 3:4], op=ALU.mult), r=[sk], w=[sk])
        P.op('dve', lambda e, s=s: e.tensor_tensor(out=s[:, 10:11], in0=s[:, 9:10], in1=s[:, 7:8], op=ALU.mult), r=[sk], w=[sk])
        P.op('dve', lambda e, s=s: e.tensor_scalar(out=WM[:], in0=M1[:], scalar1=s[:, 9:10], scalar2=None, op0=ALU.mult),
             r=['M1', sk], w=['WM'])
        P.op('dve', lambda e, s=s: e.scalar_tensor_tensor(out=WM[:], in0=M2[:], scalar=s[:, 10:11], in1=WM[:],
                                                         op0=ALU.mult, op1=ALU.add), r=['M2', sk, 'WM'], w=['WM'])
        for g in range(4):
            P.op('dve', lambda e, g=g, tt=tt: e.tensor_scalar(out=WF[:, tt, g * 8:(g + 1) * 8], in0=WM[:], scalar1=OH[:, g:g + 1],
                                                            scalar2=None, op0=ALU.mult), r=['WM', 'OH'], w=[('WF', tt)])
        bank = banks[4 + tt // 4]
        P.op('pe', lambda e, tt=tt, bank=bank: e.transpose(out=bank[0:32, (tt % 4) * 128:(tt % 4 + 1) * 128], in_=WF[:, tt, :],
                                                         identity=IDN[:]), r=[('WF', tt), 'IDN'], w=[('bk', 4 + tt // 4)])
    for hh in range(2):
        P.op('act', lambda e, hh=hh: e.activation(out=WGT[:, hh * 512:(hh + 1) * 512], in_=banks[4 + hh][0:32, :], func=AF.Copy),
             r=[('bk', 4 + hh)], w=['WGT'])
    P.dma('sp', wscr, WGT[:], r=['WGT'], w=['wscr'])
    h2f_all = [('n', 'h', 1, t) for t in range(2)]
    bi = 0
    gi = 0
    ld = 0
    for ex in range(32):
        wb = WB[ex % 2]
        wbk = ('WB', ex % 2)
        P.dma('pool', wb[:], wscr[ex:ex + 1, :].partition_broadcast(128), r=['wscr'], w=[wbk])
        for jt in range(4):
            gb = GUB[ld % 2]
            for gu in range(2):
                c0 = gu * 512 + jt * 128
                P.dma('sp' if gu == 0 else 'act', GS[gu], wgu[ex, :, :, c0:c0 + 128], w=[('GS', gu)] + (h2f_all if ex == 0 and jt == 0 else []))
                P.op('pool' if gu == 0 else 'dve', lambda e, gu=gu, gb=gb: e.tensor_copy(out=gb[gu][:], in_=GS[gu]),
                     r=[('GS', gu)], w=[('GUB', ld % 2, gu)])
            for t2 in range(2):
                ts = slice(t2 * 512, (t2 + 1) * 512)
                bg, bu = banks[(gi * 2) % 4], banks[(gi * 2 + 1) % 4]
                kg, ku = ('bk', (gi * 2) % 4), ('bk', (gi * 2 + 1) % 4)
                for gu, (bank, bkk) in enumerate(((bg, kg), (bu, ku))):
                    for k in range(16):
                        P.op('pe', lambda e, k=k, gu=gu, gb=gb, bank=bank, ts=ts: e.matmul(
                            bank[:, :], lhsT=gb[gu][:, k, :], rhs=H2B[:, k, ts], start=(k == 0), stop=(k == 15)),
                            r=[('GUB', ld % 2, gu), ('n', 'h', 0, t2)], w=[bkk])
                ta = TMPA[gi % 2]
                tak = ('TMPA', gi % 2)
                P.op('act', lambda e, ta=ta, bg=bg: e.activation(out=ta[:], in_=bg[:, :], func=AF.Silu), r=[kg], w=[tak])
                P.op('dve', lambda e, ta=ta, bu=bu: e.tensor_tensor(out=ta[:], in0=bu[:, :], in1=ta[:], op=ALU.mult), r=[ku, tak], w=[tak])
                P.op('dve', lambda e, ta=ta, wb=wb, jt=jt, ts=ts: e.tensor_tensor(out=ACTB[:, jt, ts], in0=ta[:], in1=wb[:, ts], op=ALU.mult),
                     r=[tak, wbk], w=[('ACTB', jt, t2)])
                gi += 1
            ld += 1
        for dq in range(4):
            wd = WDB[dq % 2]
            wdk = ('WDB', dq % 2)
            P.dma('sp', WDS, wdn[ex, :, :, dq * 512:(dq + 1) * 512], w=['WDS'] + (h2f_all if ex == 0 and dq == 0 else []))
            P.op('pool', lambda e, wd=wd: e.tensor_copy(out=wd[:], in_=WDS), r=['WDS'], w=[wdk])
            for dt in range(4):
                d = dq * 4 + dt
                for t2 in range(2):
                    ts = slice(t2 * 512, (t2 + 1) * 512)
                    bank = banks[4 + (bi % 4)]
                    bkk = ('bk', 4 + (bi % 4))
                    for c in range(4):
                        P.op('pe', lambda e, c=c, wd=wd, dt=dt, bank=bank, ts=ts: e.matmul(
                            bank[:, :], lhsT=wd[:, c, dt * 128:(dt + 1) * 128], rhs=ACTB[:, c, ts], start=(c == 0), stop=(c == 3)),
                            r=[wdk, ('ACTB', c, t2)], w=[bkk])
                    if ex == 0:
                        P.op('act', lambda e, d=d, ts=ts, bank=bank: e.activation(out=XA[:, d, ts], in_=bank[:, :], func=AF.Copy),
                             r=[bkk, ('n', 'x', d)], w=[('ACC', d, t2)])
                    else:
                        P.op('dve', lambda e, d=d, ts=ts, bank=bank: e.tensor_tensor(out=XA[:, d, ts], in0=bank[:, :], in1=XA[:, d, ts],
                                                                                   op=ALU.add), r=[bkk, ('ACC', d, t2)], w=[('ACC', d, t2)])
                    bi += 1
    for d in range(16):
        P.dma('sp', XR, xT[:, d, :], w=['XR'] + (['WDS', ('GS', 0), ('GS', 1)] if d == 0 else []))
        P.op('dve', lambda e, d=d: e.scalar_tensor_tensor(out=XA[:, d, :], in0=XA[:, d, :], scalar=PT[:, 3, d:d + 1], in1=XR,
                                                        op0=ALU.mult, op1=ALU.add),
             r=[('ACC', d, 0), ('ACC', d, 1), 'XR', 'G2'], w=[('ACC', d, 0), ('ACC', d, 1)])
        P.dma('pool', out[:, d, :], XA[:, d, :], r=[('ACC', d, 0), ('ACC', d, 1)])
    return P.build()


def emit_norm_cb(P, xt, nk, ntok, gam, sc, sh, ones, ps_banks, rstd, tmp, outs, tag, cb):
    Dn = nk * 128
    gm = P.sb([128, nk], F32)
    P.op('dve', lambda e: e.scalar_tensor_tensor(out=gm[:], in0=sc, scalar=1.0, in1=gam, op0=ALU.add, op1=ALU.mult),
         r=[(tag, 'sc'), (tag, 'gam')], w=[(tag, 'gm')])
    for t in range(ntok // 512):
        ts = slice(t * 512, (t + 1) * 512)
        bank = ps_banks[t % len(ps_banks)]
        bk = ('psb', id(bank))
        for k in range(nk):
            j = k % 2
            P.op('act', lambda e, k=k, j=j, ts=ts: e.activation(out=tmp[:, j, :], in_=xt[:, k, ts], func=AF.Square),
                 r=[(tag, 'x', k)], w=[(tag, 'tmp', j)])
            P.op('pe', lambda e, k=k, j=j, bank=bank: e.matmul(bank[:, :], lhsT=ones[:], rhs=tmp[:, j, :],
                                                             start=(k == 0), stop=(k == nk - 1)),
                 r=[(tag, 'tmp', j), 'ones'], w=[bk])
        P.op('act', lambda e, ts=ts, bank=bank: e.activation(out=rstd[:, ts], in_=bank[:, :], func=AF.Sqrt,
                                                            scale=1.0 / Dn, bias=epsb[0][:]),
             r=[bk, 'epsb'], w=[(tag, 'rstd', t)])
        P.op('dve', lambda e, ts=ts: e.reciprocal(out=rstd[:, ts], in_=rstd[:, ts]),
             r=[(tag, 'rstd', t)], w=[(tag, 'rstd', t)])
        for k in range(nk):
            j = k % 2
            P.op('dve', lambda e, k=k, j=j, ts=ts: e.tensor_tensor(out=tmp[:, j, :], in0=xt[:, k, ts], in1=rstd[:, ts],
                                                                 op=ALU.mult),
                 r=[(tag, 'x', k), (tag, 'rstd', t)], w=[(tag, 'tmp', j)])
            for i, (ot, local) in enumerate(outs):
                osl = slice(0, 512) if local else ts
                P.op('act', lambda e, k=k, j=j, osl=osl, ot=ot: e.activation(
                    out=ot[:, k, osl], in_=tmp[:, j, :], func=AF.Identity, scale=gm[:, k:k + 1], bias=sh[:, k:k + 1]),
                    r=[(tag, 'tmp', j), (tag, 'gm'), (tag, 'sh')], w=[(tag, 'h', i, t)])
        cb(t)


def run_E(x1T, gam, sc2, sh2, g2, inp, l):
    nc = cached('E', build_E)
    wr = fm(np.concatenate([inp["router_grp_w"][l], inp["router_exp_w"][l]], axis=1))
    rb = np.concatenate([inp["router_grp_b"][l], inp["router_exp_b"][l]])[None, :].astype(np.float32)
    wgu = np.ascontiguousarray(inp["expert_w_gu"][l].reshape(32, 16, 128, 1024).transpose(0, 2, 1, 3))
    wdn = np.ascontiguousarray(inp["expert_w_down"][l].reshape(32, 4, 128, 2048).transpose(0, 2, 1, 3))
    idd = np.eye(128, dtype=np.float32)
    in_maps = []
    for i in range(NCORE):
        b = i // 4
        prm = np.stack([vec_fm(gam), vec_fm(sc2[b]), vec_fm(sh2[b]), vec_fm(g2[b])], axis=1)
        in_maps.append({"xT": fm(x1T[:, i * 1024:(i + 1) * 1024]), "prm": np.ascontiguousarray(prm), "wr": wr,
                        "rb": np.ascontiguousarray(rb), "idd": idd, "wgu": wgu, "wdn": wdn})
    res = launch(nc, in_maps)
    return np.concatenate([r["out"].transpose(1, 0, 2).reshape(2048, 1024) for r in res], axis=1)


DECAY_SCALE_ = float(np.exp(-0.5))


def build_RW1():
    P = Prog()
    feats = P.dram("feats", [6, 128, 2 * S_], F32, "ExternalInput")
    prm = P.dram("prm", [128, 6, 2], F32, "ExternalInput")
    pv = P.dram("pv", [128, 8], F32, "ExternalInput")
    wl = P.dram("wl", [128, 5, 128], F32, "ExternalInput")
    bones = P.dram("bones", [128, 128], F32, "ExternalInput")
    outs = P.dram("outs", [11, 128, 2 * S_], F32, "ExternalOutput")
    mk_consts(P)
    PR = P.sb([128, 6, 2], F32)
    C0 = P.sb([128, 6], F32)
    PV = P.sb([128, 8], F32)
    OMK = P.sb([128, 1], F32)
    WL = P.sb([128, 5, 128], F32)
    BO = P.sb([128, 128], F32)
    STG = P.sb([128, S_], F32)
    F = [P.sb([128, S_], F32) for _ in range(6)]
    T1 = P.sb([128, S_], F32)
    T2 = P.sb([128, S_], F32)
    T3 = P.sb([128, S_], F32)
    banks = [P.ps([128, 512]) for _ in range(4)]
    P.dma('sp', PR[:], prm, w=['PR'])
    P.dma('sp', PV[:], pv, w=['PV'])
    P.dma('sp', WL[:], wl, w=['WL'])
    P.dma('sp', BO[:], bones, w=['BO'])
    P.op('dve', lambda e: e.tensor_tensor(out=C0[:], in0=PR[:, :, 0], in1=PR[:, :, 1], op=ALU.add), r=['PR'], w=['C0'])
    P.op('dve', lambda e: e.tensor_scalar(out=C0[:], in0=C0[:], scalar1=-1.0, scalar2=1.0, op0=ALU.mult, op1=ALU.add),
         r=['C0'], w=['C0'])
    P.op('dve', lambda e: e.tensor_scalar(out=OMK[:], in0=PV[:, 5:6], scalar1=-1.0, scalar2=1.0, op0=ALU.mult, op1=ALU.add),
         r=['PV'], w=['OMK'])
    bi = [0]

    def mm_act(lhsT, src, skey, dst, dkey, func, bias=None, scale=1.0, extra_r=()):
        for t in range(S_ // 512):
            ts = slice(t * 512, (t + 1) * 512)
            bank = banks[bi[0] % 4]
            bkk = ('bk', bi[0] % 4)
            P.op('pe', lambda e, ts=ts, bank=bank: e.matmul(bank[:, :], lhsT=lhsT, rhs=src[:, ts], start=True, stop=True),
                 r=[skey, 'WL', 'BO'], w=[bkk])
            if bias is not None:
                P.op('act', lambda e, ts=ts, bank=bank: e.activation(out=dst[:, ts], in_=bank[:, :], func=func, bias=bias, scale=scale),
                     r=[bkk, 'PV'] + list(extra_r), w=[dkey])
            else:
                P.op('act', lambda e, ts=ts, bank=bank: e.activation(out=dst[:, ts], in_=bank[:, :], func=func, scale=scale),
                     r=[bkk] + list(extra_r), w=[dkey])
            bi[0] += 1

    def store(idx, src, skey, b):
        P.dma('pool', outs[idx, :, b * S_:(b + 1) * S_], src[:], r=[skey])

    for b in range(2):
        bs = slice(b * S_, (b + 1) * S_)
        for a in range(6):
            P.dma('sp', STG[:], feats[a, :, bs], w=['STG'])
            Fa, fk = F[a], ('F', a)
            P.op('dve', lambda e, Fa=Fa, a=a: e.tensor_scalar(out=Fa[:], in0=STG[:], scalar1=C0[:, a:a + 1], scalar2=None, op0=ALU.mult),
                 r=['STG', 'C0'], w=[fk])
            P.op('dve', lambda e, Fa=Fa, a=a: e.scalar_tensor_tensor(out=Fa[:, 1:S_], in0=STG[:, 0:S_ - 1], scalar=PR[:, a, 0:1],
                                                                   in1=Fa[:, 1:S_], op0=ALU.mult, op1=ALU.add),
                 r=['STG', 'PR', fk], w=[fk])
            P.op('dve', lambda e, Fa=Fa, a=a: e.scalar_tensor_tensor(out=Fa[:, 0:S_ - 1], in0=STG[:, 1:S_], scalar=PR[:, a, 1:2],
                                                                   in1=Fa[:, 0:S_ - 1], op0=ALU.mult, op1=ALU.add),
                 r=['STG', 'PR', fk], w=[fk])
        R_, K_, V_, WD, AD, GD = F
        store(0, R_, ('F', 0), b)
        store(2, V_, ('F', 2), b)
        P.op('act', lambda e: e.activation(out=GD[:], in_=GD[:], func=AF.Sigmoid), r=[('F', 5)], w=[('F', 5)])
        mm_act(WL[:, 4, :], GD, ('F', 5), T1, 'T1', AF.Copy)
        store(9, T1, 'T1', b)
        P.op('dve', lambda e: e.tensor_scalar(out=T2[:], in0=K_[:], scalar1=PV[:, 4:5], scalar2=None, op0=ALU.mult),
             r=[('F', 1), 'PV'], w=['T2'])
        P.op('act', lambda e: e.activation(out=T3[:], in_=T2[:], func=AF.Square), r=['T2'], w=['T3'])
        mm_act(BO[:], T3, 'T3', T1, 'T1', AF.Sqrt, bias=epsb[0][:], extra_r=['epsb'])
        P.op('dve', lambda e: e.reciprocal(out=T1[:], in_=T1[:]), r=['T1'], w=['T1'])
        P.op('dve', lambda e: e.tensor_tensor(out=T2[:], in0=T2[:], in1=T1[:], op=ALU.mult), r=['T1', 'T2'], w=['T2'])
        store(1, T2, 'T2', b)
        P.op('act', lambda e: e.activation(out=WD[:], in_=WD[:], func=AF.Tanh), r=[('F', 3)], w=[('F', 3)])
        for z in range(2):
            mm_act(WL[:, 2 + z, :], AD, ('F', 4), T1, 'T1', AF.Sigmoid, bias=PV[:, 2 + z:3 + z])
            P.op('dve', lambda e: e.tensor_tensor(out=T3[:], in0=T1[:], in1=T2[:], op=ALU.mult), r=['T1', 'T2'], w=['T3'])
            store(3 + z, T3, 'T3', b)
            P.op('dve', lambda e: e.tensor_scalar(out=T1[:], in0=T1[:], scalar1=PV[:, 5:6], scalar2=OMK[:, 0:1],
                                                  op0=ALU.mult, op1=ALU.add), r=['T1', 'PV', 'OMK'], w=['T1'])
            P.op('dve', lambda e: e.tensor_tensor(out=T1[:], in0=T1[:], in1=K_[:], op=ALU.mult), r=['T1', ('F', 1)], w=['T1'])
            store(5 + z, T1, 'T1', b)
            P.op('dve', lambda e: e.scalar_tensor_tensor(out=T3[:], in0=R_[:], scalar=PV[:, 6:7], in1=T1[:], op0=ALU.mult, op1=ALU.mult),
                 r=[('F', 0), 'PV', 'T1', 'T3'], w=['T3'])
            mm_act(BO[:], T3, 'T3', T3, 'T3b', AF.Copy)
            if z == 0:
                P.op('dve', lambda e: e.tensor_tensor(out=STG[:], in0=T3[:], in1=V_[:], op=ALU.mult), r=['T3b', ('F', 2), 'STG'], w=['STG'])
            else:
                P.op('dve', lambda e: e.tensor_tensor(out=T3[:], in0=T3[:], in1=V_[:], op=ALU.mult), r=['T3b', ('F', 2)], w=['T3b'])
                P.op('dve', lambda e: e.tensor_tensor(out=STG[:], in0=STG[:], in1=T3[:], op=ALU.add), r=['T3b', 'STG'], w=['STG'])
                store(10, STG, 'STG', b)
            mm_act(WL[:, z, :], WD, ('F', 3), T1, 'T1', AF.Sigmoid, bias=PV[:, z:z + 1])
            P.op('dve', lambda e: e.tensor_scalar(out=T1[:], in0=T1[:], scalar1=-DECAY_SCALE_, scalar2=None, op0=ALU.mult),
                 r=['T1'], w=['T1'])
            store(7 + z, T1, 'T1', b)
    return P.build()


RW_OFF = 7168


def run_RW1(pT, inp, l):
    nc = cached('RW1', build_RW1)
    fT = pT[RW_OFF:RW_OFF + 3456]
    mu_p, mu_n = inp["rwkv_mu_prev"][l], inp["rwkv_mu_next"][l]
    bones = np.kron(np.eye(2, dtype=np.float32), np.ones((64, 64), np.float32))
    in_maps = []
    for i in range(NCORE):
        cs = slice(i * 128, (i + 1) * 128)
        rows = [slice(i * 128, (i + 1) * 128), slice(1024 + i * 128, 1024 + (i + 1) * 128),
                slice(2048 + i * 128, 2048 + (i + 1) * 128), slice(3072, 3200), slice(3200, 3328), slice(3328, 3456)]
        feats = np.stack([fT[r] for r in rows])
        prm = np.stack([np.stack([mu_p[r], mu_n[r]], axis=1) for r in rows], axis=1)
        pv = np.zeros((128, 8), np.float32)
        pv[:, 0] = inp["rwkv_w0"][l][0, cs]; pv[:, 1] = inp["rwkv_w0"][l][1, cs]
        pv[:, 2] = inp["rwkv_a0"][l][0, cs]; pv[:, 3] = inp["rwkv_a0"][l][1, cs]
        pv[:, 4] = inp["rwkv_k_k"][l][cs]; pv[:, 5] = inp["rwkv_k_a"][l][cs]
        pv[:, 6] = inp["rwkv_r_k"][l].reshape(-1)[cs]
        wl = np.zeros((128, 5, 128), np.float32)
        for z in range(2):
            wl[z * 64:(z + 1) * 64, z, :] = inp["rwkv_w_up"][l][z][:, cs]
            wl[z * 64:(z + 1) * 64, 2 + z, :] = inp["rwkv_a_up"][l][z][:, cs]
        wl[:, 4, :] = inp["rwkv_g_up"][l][:, cs]
        in_maps.append({"feats": np.ascontiguousarray(feats), "prm": np.ascontiguousarray(prm.astype(np.float32)),
                        "pv": pv, "wl": wl, "bones": bones})
    res = launch(nc, in_maps)
    return [r["outs"] for r in res]


USE_F32R = [True]


def R32(ap):
    return ap.bitcast(mybir.dt.float32r) if USE_F32R[0] else ap


def build_RW2_old(nseg=8):
    P = Prog()
    arr = P.dram("arr", [4, 8, 128, 6, 512], F32, "ExternalInput")
    cm = P.dram("cm", [128, 5, 128], F32, "ExternalInput")
    yout = P.dram("yout", [4, 128, 64, 64], F32, "ExternalOutput")
    CM = P.sb([128, 5, 128], F32)
    NL, NU, UU, UI, IDN = [CM[:, i, :] for i in range(5)]
    MSK = P.sb([128, 512], F32)
    IN = [[P.sb([128, 6, 512], F32) for _ in range(2)] for _ in range(4)]
    CUM = [P.sb([128, 512], F32) for _ in range(4)]
    YB = [[P.sb([128, 8, 64], F32) for _ in range(2)] for _ in range(4)]
    S0 = [P.sb([128, 64], F32) for _ in range(4)]
    EX = [[P.sb([128, 64], F32) for _ in range(4)] for _ in range(4)]
    names = ['A', 'B', 'K', 'R', 'BG', 'KG', 'V']
    BD = [{nm: P.sb([128, 128], F32) for nm in names} for _ in range(4)]
    WK = [{nm: P.sb([128, 128], F32) for nm in ['P0', 'PT0', 'P1', 'PT1', 'TT', 'NKA', 'MBR', 'MKR', 'BGT', 'KGT']} for _ in range(4)]
    SM = [{nm: P.sb([128, 64], F32) for nm in ['VST', 'VH', 'W0', 'NRHO']} for _ in range(4)]
    banks = [P.ps([128, 512]) for _ in range(8)]
    slot = [0]

    def ps():
        s = slot[0] % 8
        slot[0] += 1
        return banks[s][:, 0:128], ('ps', s)

    P.dma('sp', CM[:], cm, w=['CM'])
    P.op('pool', lambda e: e.memset(MSK[:], 1.0), w=['MSK'])
    P.op('pool', lambda e: e.memset(MSK[:].rearrange("p (n t) -> p n t", t=64)[:, :, 0:1], 0.0), r=['MSK'], w=['MSK'])
    ZERO = P.sb([128, 128], F32)
    P.op('pool', lambda e: e.memset(ZERO[:], 0.0), w=['ZERO'])
    for p in range(4):
        for nm in names:
            P.op('dve', lambda e, p=p, nm=nm: e.tensor_copy(out=R32(BD[p][nm][:]), in_=ZERO[:]), r=['ZERO'], w=[('BD', p, nm)])
        P.op('dve', lambda e, p=p: e.tensor_copy(out=R32(S0[p][:]), in_=ZERO[:, 0:64]), r=['ZERO'], w=[('S0', p)])
    for sg in range(nseg):
        ib = sg % 2
        for p in range(4):
            ik = ('IN', p, ib)
            P.dma('sp' if p % 2 == 0 else 'act', IN[p][ib][:], arr[p, sg], w=[ik])
            P.op('dve', lambda e, p=p, ib=ib: e.tensor_tensor_scan(out=CUM[p][:], data0=MSK[:], data1=IN[p][ib][:, 5, :], initial=0.0,
                                                                   op0=ALU.mult, op1=ALU.add), r=[ik, 'MSK'], w=[('CUM', p)])
            P.op('pool', lambda e, p=p, ib=ib: e.tensor_tensor(out=IN[p][ib][:, 5, :], in0=CUM[p][:], in1=IN[p][ib][:, 5, :],
                                                               op=ALU.subtract), r=[('CUM', p), ik], w=[ik])
        for j in range(8):
            cs = slice(j * 64, (j + 1) * 64)

            def gen(p, j=j, cs=cs):
                ik = ('IN', p, ib)
                I_ = IN[p][ib]
                Ep, En, Ex, Ec = EX[p]
                ek = [('EX', p, i) for i in range(4)]
                ck = ('CUM', p)
                P.op('act', lambda e, p=p, Ep=Ep, cs=cs: e.activation(out=Ep[:], in_=CUM[p][:, cs], func=AF.Exp), r=[ck], w=[ek[0]])
                P.op('act', lambda e, p=p, En=En, cs=cs: e.activation(out=En[:], in_=CUM[p][:, cs], func=AF.Exp, scale=-1.0), r=[ck], w=[ek[1]])
                P.op('act', lambda e, I_=I_, Ex=Ex, cs=cs: e.activation(out=Ex[:], in_=I_[:, 5, cs], func=AF.Exp), r=[ik], w=[ek[2]])
                P.op('act', lambda e, p=p, Ec=Ec, cs=cs, j=j: e.activation(out=Ec[:], in_=CUM[p][:, cs], func=AF.Exp, scale=-1.0,
                                                                         bias=CUM[p][:, j * 64 + 63:j * 64 + 64]), r=[ck], w=[ek[3]])
                yield
                specs = [('A', 1, Ex, ek[2]), ('B', 3, En, ek[1]), ('K', 4, En, ek[1]), ('R', 0, Ep, ek[0]),
                         ('BG', 3, Ec, ek[3]), ('KG', 4, Ec, ek[3]), ('V', 2, None, None)]
                for si, (nm, ai, Et, etk) in enumerate(specs):
                    for c in range(2):
                        ps_ = slice(c * 64, (c + 1) * 64)
                        dst = BD[p][nm][ps_, c * 64:(c + 1) * 64]
                        eng = 'pool' if si % 2 == 0 else 'dve'
                        if Et is None:
                            P.op(eng, lambda e, dst=dst, I_=I_, ai=ai, ps_=ps_, cs=cs: e.tensor_copy(out=R32(dst), in_=I_[ps_, ai, cs]),
                                 r=[ik], w=[('BD', p, nm)])
                        else:
                            P.op(eng, lambda e, dst=dst, I_=I_, ai=ai, ps_=ps_, cs=cs, Et=Et: e.tensor_tensor(
                                out=R32(dst), in0=I_[ps_, ai, cs], in1=Et[ps_, :], op=ALU.mult), r=[ik, etk], w=[('BD', p, nm)])
                yield
                bd = BD[p]
                bk_ = lambda nm, p=p: ('BD', p, nm)
                wk = WK[p]
                wkk = lambda nm, p=p: ('WK', p, nm)
                smm = SM[p]
                smk = lambda nm, p=p: ('SM', p, nm)

                def mm(lhsT, lk, rhs, rk, n=128):
                    o, ok = ps()
                    oo = o[:, 0:n]
                    P.op('pe', lambda e, oo=oo, lhsT=lhsT, rhs=rhs: e.matmul(oo, lhsT=R32(lhsT), rhs=R32(rhs), start=True, stop=True),
                         r=[lk, rk], w=[ok])
                    return oo, ok

                def mmacc(terms, n=64):
                    o, ok = ps()
                    oo = o[:, 0:n]
                    for ti, (lhsT, lk, rhs, rk) in enumerate(terms):
                        P.op('pe', lambda e, oo=oo, lhsT=lhsT, rhs=rhs, ti=ti: e.matmul(
                            oo, lhsT=R32(lhsT), rhs=R32(rhs), start=(ti == 0), stop=(ti == len(terms) - 1)), r=[lk, rk], w=[ok])
                    return oo, ok

                def evac_mask(dst, dk, src, sk, mask):
                    P.op('dve', lambda e, dst=dst, src=src, mask=mask: e.tensor_tensor(out=R32(dst), in0=src, in1=mask, op=ALU.mult),
                         r=['CM'], w=[dk, sk])

                def evac_copy(dst, dk, src, sk, scale=1.0):
                    P.op('act', lambda e, dst=dst, src=src, scale=scale: e.activation(out=R32(dst), in_=src, func=AF.Copy, scale=scale),
                         r=[], w=[dk, sk])

                o, ok = mm(bd['A'][:], bk_('A'), bd['B'][:], bk_('B'))
                evac_mask(wk['P0'][:], wkk('P0'), o, ok, NL)
                yield
                o, ok = mm(bd['B'][:], bk_('B'), bd['A'][:], bk_('A'))
                evac_mask(wk['PT0'][:], wkk('PT0'), o, ok, NU)
                P.op('pool', lambda e, wk=wk: e.tensor_tensor(out=R32(wk['TT'][:]), in0=wk['PT0'][:], in1=IDN, op=ALU.add),
                     r=[wkk('PT0'), 'CM'], w=[wkk('TT')])
                yield
                o, ok = mm(bd['K'][:], bk_('K'), bd['A'][:], bk_('A'))
                evac_mask(wk['NKA'][:], wkk('NKA'), o, ok, UU)
                yield
                o, ok = mm(bd['B'][:], bk_('B'), bd['R'][:], bk_('R'))
                evac_mask(wk['MBR'][:], wkk('MBR'), o, ok, UI)
                yield
                o, ok = mm(bd['K'][:], bk_('K'), bd['R'][:], bk_('R'))
                evac_mask(wk['MKR'][:], wkk('MKR'), o, ok, UI)
                yield
                o, ok = ps()
                P.op('pe', lambda e, o=o, bd=bd: e.transpose(out=o, in_=bd['V'][:], identity=IDN), r=[bk_('V'), 'CM'], w=[ok])
                evac_copy(smm['VH'][:], smk('VH'), o[:, 0:64], ok)
                P.op('dve', lambda e, o=o, smm=smm: e.tensor_tensor(out=R32(smm['VST'][:]), in0=o[:, 64:128], in1=smm['VH'][:], op=ALU.add),
                     r=[smk('VH')], w=[smk('VST'), ok])
                yield
                for (src, dstn) in (('BG', 'BGT'), ('KG', 'KGT')):
                    o, ok = ps()
                    P.op('pe', lambda e, o=o, bd=bd, src=src: e.transpose(out=o, in_=bd[src][:], identity=IDN), r=[bk_(src), 'CM'], w=[ok])
                    evac_copy(wk[dstn][:], wkk(dstn), o, ok)
                yield
                cur, curT = 'P0', 'PT0'
                for lv in range(1, 6):
                    nxt, nxtT = ('P1', 'PT1') if cur == 'P0' else ('P0', 'PT0')
                    o, ok = mm(wk[curT][:], wkk(curT), wk[cur][:], wkk(cur))
                    evac_copy(wk[nxt][:], wkk(nxt), o, ok)
                    yield
                    if lv < 5:
                        o2, ok2 = mm(wk[cur][:], wkk(cur), wk[curT][:], wkk(curT))
                        evac_copy(wk[nxtT][:], wkk(nxtT), o2, ok2)
                    yield
                    o3, ok3 = mm(wk[nxt][:], wkk(nxt), wk['TT'][:], wkk('TT'))
                    P.op('dve', lambda e, wk=wk, o3=o3: e.tensor_tensor(out=R32(wk['TT'][:]), in0=o3, in1=wk['TT'][:], op=ALU.add),
                         r=[wkk('TT')], w=[wkk('TT'), ok3])
                    cur, curT = nxt, nxtT
                    yield
                yield
                s0k = ('S0', p)
                o, ok = mmacc([(bd['A'][:], bk_('A'), S0[p][:], s0k), (wk['NKA'][:], wkk('NKA'), smm['VST'][:], smk('VST'))])
                evac_copy(smm['W0'][:], smk('W0'), o, ok)
                yield
                o, ok = mm(wk['TT'][:], wkk('TT'), smm['W0'][:], smk('W0'), n=64)
                evac_copy(smm['NRHO'][:], smk('NRHO'), o, ok, scale=-1.0)
                yield
                o, ok = mmacc([(bd['R'][:], bk_('R'), S0[p][:], s0k), (wk['MBR'][:], wkk('MBR'), smm['NRHO'][:], smk('NRHO')),
                               (wk['MKR'][:], wkk('MKR'), smm['VST'][:], smk('VST'))])
                evac_copy(YB[p][ib][:, j, :], ('YB', p, ib), o, ok)
                yield
                o, ok = mmacc([(wk['BGT'][:], wkk('BGT'), smm['NRHO'][:], smk('NRHO')), (wk['KGT'][:], wkk('KGT'), smm['VST'][:], smk('VST'))])
                P.op('dve', lambda e, p=p, Ep=Ep, o=o: e.scalar_tensor_tensor(out=R32(S0[p][:]), in0=S0[p][:], scalar=Ep[:, 63:64], in1=o,
                                                                             op0=ALU.mult, op1=ALU.add), r=[s0k, ek[0]], w=[s0k, ok])

            gens = [gen(p) for p in range(4)]
            while gens:
                for g in gens[:]:
                    try:
                        next(g)
                    except StopIteration:
                        gens.remove(g)
        for p in range(4):
            P.dma('pool', yout[p, :, sg * 8:(sg + 1) * 8, :], YB[p][ib][:], r=[('YB', p, ib)])
    return P.build()


def build_RW2(nseg=16):
    P = Prog()
    SEGL = 256
    NCH = SEGL // 64
    arr = P.dram("arr", [4, 16, 128, 6, SEGL], F32, "ExternalInput")
    cm = P.dram("cm", [128, 6, 128], F32, "ExternalInput")
    yout = P.dram("yout", [4, 128, 64, 64], F32, "ExternalOutput")
    CM = P.sb([128, 6, 128], F32)
    CMX = CM[:, 0:2, :].rearrange("p a b -> p (a b)")
    CMS = CM[:, 2:5, :].rearrange("p a b -> p (a b)")
    IDN = CM[:, 5, :]
    MSK = P.sb([128, SEGL], F32)
    IN = [[P.sb([128, 6, SEGL], F32) for _ in range(2)] for _ in range(4)]
    CUM = [P.sb([128, 2, SEGL], F32) for _ in range(4)]
    EXS = [P.sb([128, 4, SEGL], F32) for _ in range(4)]
    YB = [[P.sb([128, NCH, 64], F32) for _ in range(2)] for _ in range(4)]
    S0 = [P.sb([128, 64], F32) for _ in range(4)]
    names = ['A', 'B', 'K', 'R', 'BG', 'KG', 'V']
    BDS = [{nm: P.sb([128, NCH, 128], F32) for nm in names} for _ in range(4)]
    PP = [[P.sb([128, 256], F32) for _ in range(2)] for _ in range(4)]
    TT = [P.sb([128, 128], F32) for _ in range(4)]
    SC = [P.sb([128, 384], F32) for _ in range(4)]
    GT = [P.sb([128, 256], F32) for _ in range(4)]
    SM = [{nm: P.sb([128, 64], F32) for nm in ['VST', 'VH', 'W0', 'NRHO']} for _ in range(4)]
    banks = [P.ps([128, 512]) for _ in range(8)]
    slot = [0]

    def psw():
        s_ = slot[0] % 8
        slot[0] += 1
        return banks[s_], ('ps', s_)

    P.dma('sp', CM[:], cm, w=['CM'])
    P.op('pool', lambda e: e.memset(MSK[:], 1.0), w=['MSK'])
    P.op('pool', lambda e: e.memset(MSK[:].rearrange("p (n t) -> p n t", t=64)[:, :, 0:1], 0.0), r=['MSK'], w=['MSK'])
    ZERO = P.sb([128, NCH * 128], F32)
    P.op('pool', lambda e: e.memset(ZERO[:], 0.0), w=['ZERO'])
    for p in range(4):
        for nm in names:
            P.op('dve', lambda e, p=p, nm=nm: e.tensor_copy(out=R32(BDS[p][nm][:].rearrange("p n t -> p (n t)")), in_=ZERO[:]),
                 r=['ZERO'], w=[('BD', p, nm)])
        P.op('dve', lambda e, p=p: e.tensor_copy(out=R32(S0[p][:]), in_=ZERO[:, 0:64]), r=['ZERO'], w=[('S0', p)])

    def mmq(o, ok, lhsT, lk, rhs, rk, start=True, stop=True):
        P.op('pe', lambda e: e.matmul(o, lhsT=R32(lhsT), rhs=R32(rhs), start=start, stop=stop), r=[lk, rk], w=[ok])

    v3 = lambda ap: ap.rearrange("p (n t) -> p n t", t=64)
    for sg in range(nseg):
        ib = sg % 2
        for p in range(4):
            ik = ('IN', p, ib)
            ck = ('CUM', p)
            ek = ('EXS', p)
            I_ = IN[p][ib]
            E_ = EXS[p]
            P.dma('sp' if p % 2 == 0 else 'act', I_[:], arr[p, sg], w=[ik])
            P.op('dve', lambda e, p=p, I_=I_: e.tensor_tensor_scan(out=CUM[p][:, 0, :], data0=MSK[:], data1=I_[:, 5, :], initial=0.0,
                                                                   op0=ALU.mult, op1=ALU.add), r=[ik, 'MSK'], w=[ck])
            P.op('dve', lambda e, p=p, I_=I_: e.tensor_tensor_scan(out=CUM[p][:, 1, ::-1], data0=MSK[:], data1=I_[:, 5, ::-1], initial=0.0,
                                                                   op0=ALU.mult, op1=ALU.add), r=[ik, 'MSK', ck], w=[ck])
            P.op('act', lambda e, p=p, E_=E_: e.activation(out=E_[:, 0, :], in_=CUM[p][:, 0, :], func=AF.Exp), r=[ck], w=[ek])
            P.op('act', lambda e, p=p, E_=E_: e.activation(out=E_[:, 1, :], in_=CUM[p][:, 0, :], func=AF.Exp, scale=-1.0), r=[ck, ek], w=[ek])
            P.op('pool', lambda e, p=p, I_=I_: e.tensor_tensor(out=CUM[p][:, 0, :], in0=CUM[p][:, 0, :], in1=I_[:, 5, :], op=ALU.subtract),
                 r=[ik, ek], w=[ck])
            P.op('pool', lambda e, p=p, I_=I_: e.tensor_tensor(out=CUM[p][:, 1, :], in0=CUM[p][:, 1, :], in1=I_[:, 5, :], op=ALU.subtract),
                 r=[ik], w=[ck])
            P.op('act', lambda e, p=p, E_=E_: e.activation(out=E_[:, 2:4, :], in_=CUM[p][:, 0:2, :], func=AF.Exp), r=[ck, ek], w=[ek])
            for c in range(2):
                ps_ = slice(c * 64, (c + 1) * 64)
                for (nm, ai, ei) in (('A', 1, 2), ('B', 3, 1), ('K', 4, 1), ('R', 0, 0), ('BG', 3, 3), ('KG', 4, 3)):
                    dst = BDS[p][nm][ps_, :, c * 64:(c + 1) * 64]
                    P.op('dve', lambda e, dst=dst, I_=I_, E_=E_, ai=ai, ei=ei, ps_=ps_: e.tensor_tensor(
                        out=R32(dst), in0=v3(I_[ps_, ai, :]), in1=v3(E_[ps_, ei, :]), op=ALU.mult), r=[ik, ek], w=[('BD', p, nm)])
                dst = BDS[p]['V'][ps_, :, c * 64:(c + 1) * 64]
                P.op('pool', lambda e, dst=dst, I_=I_, ps_=ps_: e.tensor_copy(out=R32(dst), in_=v3(I_[ps_, 2, :])), r=[ik], w=[('BD', p, 'V')])
        for j in range(NCH):

            def gen(p, j=j):
                ek = ('EXS', p)
                E_ = EXS[p]
                bd = {nm: BDS[p][nm][:, j, :] for nm in names}
                bk_ = lambda nm: ('BD', p, nm)
                smm = SM[p]
                smk = lambda nm: ('SM', p, nm)
                s0k = ('S0', p)
                ttk = ('TT', p)
                GC = E_[:, 0, j * 64 + 63:j * 64 + 64]
                pp = PP[p]
                ppk = lambda i: ('PP', p, i)
                o, ok = psw()
                mmq(o[:, 0:128], ok, bd['A'], bk_('A'), bd['B'], bk_('B'))
                mmq(o[:, 128:256], ok, bd['B'], bk_('B'), bd['A'], bk_('A'))
                P.op('dve', lambda e, o=o: e.tensor_tensor(out=R32(pp[0][:]), in0=o[:, 0:256], in1=CMX, op=ALU.mult), r=['CM'], w=[ppk(0), ok])
                P.op('pool', lambda e: e.tensor_tensor(out=R32(TT[p][:]), in0=pp[0][:, 128:256], in1=IDN, op=ALU.add), r=[ppk(0), 'CM'], w=[ttk])
                yield
                o, ok = psw()
                mmq(o[:, 0:128], ok, bd['K'], bk_('K'), bd['A'], bk_('A'))
                mmq(o[:, 128:256], ok, bd['B'], bk_('B'), bd['R'], bk_('R'))
                mmq(o[:, 256:384], ok, bd['K'], bk_('K'), bd['R'], bk_('R'))
                P.op('dve', lambda e, o=o: e.tensor_tensor(out=R32(SC[p][:]), in0=o[:, 0:384], in1=CMS, op=ALU.mult), r=['CM'], w=[('SC', p), ok])
                yield
                o, ok = psw()
                P.op('pe', lambda e, o=o: e.transpose(out=o[:, 0:128], in_=bd['V'], identity=IDN), r=[bk_('V'), 'CM'], w=[ok])
                P.op('act', lambda e, o=o: e.activation(out=smm['VH'][:], in_=o[:, 0:64], func=AF.Copy), r=[], w=[smk('VH'), ok])
                P.op('dve', lambda e, o=o: e.tensor_tensor(out=R32(smm['VST'][:]), in0=o[:, 64:128], in1=smm['VH'][:], op=ALU.add),
                     r=[smk('VH')], w=[smk('VST'), ok])
                yield
                o, ok = psw()
                P.op('pe', lambda e, o=o: e.transpose(out=o[:, 0:128], in_=bd['BG'], identity=IDN), r=[bk_('BG'), 'CM'], w=[ok])
                P.op('pe', lambda e, o=o: e.transpose(out=o[:, 128:256], in_=bd['KG'], identity=IDN), r=[bk_('KG'), 'CM'], w=[ok])
                P.op('act', lambda e, o=o: e.activation(out=R32(GT[p][:]), in_=o[:, 0:256], func=AF.Copy), r=[], w=[('GT', p), ok])
                yield
                c_ = 0
                for lv in range(1, 6):
                    cur, nxt = pp[c_], pp[1 - c_]
                    kc, kn = ppk(c_), ppk(1 - c_)
                    wdt = 256 if lv < 5 else 128
                    o, ok = psw()
                    mmq(o[:, 0:128], ok, cur[:, 128:256], kc, cur[:, 0:128], kc)
                    if lv < 5:
                        mmq(o[:, 128:256], ok, cur[:, 0:128], kc, cur[:, 128:256], kc)
                    P.op('act', lambda e, o=o, nxt=nxt, wdt=wdt: e.activation(out=R32(nxt[:, 0:wdt]), in_=o[:, 0:wdt], func=AF.Copy),
                         r=[], w=[kn, ok])
                    yield
                    o3, ok3 = psw()
                    mmq(o3[:, 0:128], ok3, nxt[:, 0:128], kn, TT[p][:], ttk)
                    P.op('dve', lambda e, o3=o3: e.tensor_tensor(out=R32(TT[p][:]), in0=o3[:, 0:128], in1=TT[p][:], op=ALU.add),
                         r=[], w=[ttk, ok3])
                    c_ = 1 - c_
                    yield
                o, ok = psw()
                mmq(o[:, 0:64], ok, bd['A'], bk_('A'), S0[p][:], s0k, True, False)
                mmq(o[:, 0:64], ok, SC[p][:, 0:128], ('SC', p), smm['VST'][:], smk('VST'), False, True)
                P.op('act', lambda e, o=o: e.activation(out=R32(smm['W0'][:]), in_=o[:, 0:64], func=AF.Copy), r=[], w=[smk('W0'), ok])
                yield
                o, ok = psw()
                mmq(o[:, 0:64], ok, TT[p][:], ttk, smm['W0'][:], smk('W0'))
                P.op('act', lambda e, o=o: e.activation(out=R32(smm['NRHO'][:]), in_=o[:, 0:64], func=AF.Copy, scale=-1.0),
                     r=[], w=[smk('NRHO'), ok])
                yield
                o, ok = psw()
                mmq(o[:, 0:64], ok, bd['R'], bk_('R'), S0[p][:], s0k, True, False)
                mmq(o[:, 0:64], ok, SC[p][:, 128:256], ('SC', p), smm['NRHO'][:], smk('NRHO'), False, False)
                mmq(o[:, 0:64], ok, SC[p][:, 256:384], ('SC', p), smm['VST'][:], smk('VST'), False, True)
                P.op('act', lambda e, o=o: e.activation(out=YB[p][ib][:, j, :], in_=o[:, 0:64], func=AF.Copy), r=[], w=[('YB', p, ib), ok])
                o2, ok2 = psw()
                mmq(o2[:, 0:64], ok2, GT[p][:, 0:128], ('GT', p), smm['NRHO'][:], smk('NRHO'), True, False)
                mmq(o2[:, 0:64], ok2, GT[p][:, 128:256], ('GT', p), smm['VST'][:], smk('VST'), False, True)
                P.op('dve', lambda e, o2=o2: e.scalar_tensor_tensor(out=R32(S0[p][:]), in0=S0[p][:], scalar=GC, in1=o2[:, 0:64],
                                                                   op0=ALU.mult, op1=ALU.add), r=[ek], w=[s0k, ok2])
                yield

            gens = [gen(p) for p in range(4)]
            while gens:
                for g in gens[:]:
                    try:
                        next(g)
                    except StopIteration:
                        gens.remove(g)
        for p in range(4):
            P.dma('pool', yout[p, :, sg * NCH:(sg + 1) * NCH, :], YB[p][ib][:], r=[('YB', p, ib)])
    return P.build()


def rw2_consts():
    t = np.arange(64)
    L = (t[None, :] < t[:, None]).astype(np.float32)
    U = L.T.copy()
    UI = U + np.eye(64, dtype=np.float32)
    bd = lambda m: np.kron(np.eye(2, dtype=np.float32), m)
    return np.ascontiguousarray(np.stack([bd(-L), bd(-U), bd(U), bd(UI), bd(UI), np.eye(128, dtype=np.float32)], axis=1))


def run_RW2(rw1, nseg=8):
    nc = cached(('RW2', nseg), lambda: build_RW2_old(nseg))
    cm = np.ascontiguousarray(rw2_consts()[:, [0, 1, 2, 3, 5], :])
    in_maps = []
    for i in range(NCORE):
        o = rw1[i]
        arrs = np.zeros((4, 8, 128, 6, 512), np.float32)
        for z in range(2):
            for b in range(2):
                sel = np.stack([o[0], o[1], o[2], o[3 + z], o[5 + z], o[7 + z]], axis=1)[:, :, b * S_:(b + 1) * S_]
                if z == 1:
                    sel = sel[:, :, ::-1]
                arrs[z * 2 + b] = sel.reshape(128, 6, 8, 512).transpose(2, 0, 1, 3)
        in_maps.append({"arr": arrs, "cm": cm})
    res = launch(nc, in_maps)
    return [r["yout"] for r in res]


def build_RW3():
    P = Prog()
    yfb = P.dram("yfb", [2, 128, 8, 1024], F32, "ExternalInput")
    bvg = P.dram("bvg", [2, 128, 8, 1024], F32, "ExternalInput")
    out = P.dram("out", [128, 8, 1024], F32, "ExternalOutput")
    mk_consts(P)
    Y = [P.sb([128, 1024], F32) for _ in range(2)]
    BV = P.sb([128, 1024], F32)
    G = P.sb([128, 1024], F32)
    SQ = P.sb([128, 1024], F32)
    O = [P.sb([128, 1024], F32) for _ in range(2)]
    st = P.sb([128, 2, 4, 16], F32)
    for tt in range(8):
        ob = O[tt % 2]
        okk = ('O', tt % 2)
        P.dma('sp', BV[:], bvg[0, :, tt, :], w=['BV'])
        P.dma('sp', G[:], bvg[1, :, tt, :], w=['G'])
        for z in range(2):
            yk = ('Y', z)
            P.dma('pool', Y[z][:], yfb[z, :, tt, :], w=[yk])
            y3 = Y[z][:].rearrange("p (h v) -> p h v", h=16)
            s1, s2, mn, rs = [st[:, z, i, :] for i in range(4)]
            sk = ('st', z)
            P.op('dve', lambda e, y3=y3, s1=s1: e.tensor_reduce(out=s1, in_=y3, axis=AX.X, op=ALU.add), r=[yk], w=[sk])
            P.op('act', lambda e, z=z: e.activation(out=SQ[:], in_=Y[z][:], func=AF.Square), r=[yk], w=['SQ'])
            P.op('dve', lambda e, s2=s2: e.tensor_reduce(out=s2, in_=SQ[:].rearrange("p (h v) -> p h v", h=16), axis=AX.X, op=ALU.add),
                 r=['SQ', sk], w=[sk])
            P.op('dve', lambda e, s1=s1, mn=mn: e.tensor_scalar(out=mn, in0=s1, scalar1=1.0 / 64, scalar2=None, op0=ALU.mult), r=[sk], w=[sk])
            P.op('dve', lambda e, s1=s1, mn=mn: e.tensor_tensor(out=s1, in0=mn, in1=mn, op=ALU.mult), r=[sk], w=[sk])
            P.op('dve', lambda e, s1=s1, s2=s2: e.scalar_tensor_tensor(out=s2, in0=s2, scalar=1.0 / 64, in1=s1, op0=ALU.mult, op1=ALU.subtract),
                 r=[sk], w=[sk])
            P.op('act', lambda e, s2=s2, rs=rs: e.activation(out=rs, in_=s2, func=AF.Sqrt, bias=epsb[0][:]), r=[sk, 'epsb'], w=[sk])
            P.op('dve', lambda e, rs=rs: e.reciprocal(out=rs, in_=rs), r=[sk], w=[sk])
            for h in range(16):
                hs = slice(h * 64, (h + 1) * 64)
                eng = 'dve' if h % 2 == 0 else 'pool'
                P.op(eng, lambda e, z=z, hs=hs, h=h, mn=mn, rs=rs: e.tensor_scalar(
                    out=Y[z][:, hs], in0=Y[z][:, hs], scalar1=mn[:, h:h + 1], scalar2=rs[:, h:h + 1], op0=ALU.subtract, op1=ALU.mult),
                    r=[sk, yk], w=[yk])
        P.op('dve', lambda e, ob=ob: e.tensor_tensor(out=ob[:], in0=Y[0][:], in1=Y[1][:], op=ALU.add), r=[('Y', 0), ('Y', 1)], w=[okk])
        P.op('dve', lambda e, ob=ob: e.tensor_tensor(out=ob[:], in0=ob[:], in1=BV[:], op=ALU.add), r=['BV', okk], w=[okk])
        P.op('dve', lambda e, ob=ob: e.tensor_tensor(out=ob[:], in0=ob[:], in1=G[:], op=ALU.mult), r=['G', okk], w=[okk])
        P.dma('sp', out[:, tt, :], ob[:], r=[okk])
    return P.build()


def run_RW3(rw1, rw2):
    nc = cached('RW3', build_RW3)
    yz = np.zeros((2, 2, S_, 16, 64), np.float32)
    for i in range(NCORE):
        for z in range(2):
            for b in range(2):
                y = rw2[i][z * 2 + b].reshape(2, 64, 64, 64).transpose(2, 1, 0, 3).reshape(S_, 2, 64)
                if z == 1:
                    y = y[::-1]
                yz[z, b, :, 2 * i:2 * i + 2, :] = y
    yz = yz.reshape(2, 2 * S_, 1024)
    bv = np.concatenate([o[10] for o in rw1], axis=0).T
    g = np.concatenate([o[9] for o in rw1], axis=0).T
    in_maps = []
    for i in range(NCORE):
        ts = slice(i * 1024, (i + 1) * 1024)
        tmj = lambda a: a[ts].reshape(8, 128, 1024).transpose(1, 0, 2)
        in_maps.append({"yfb": np.ascontiguousarray(np.stack([tmj(yz[0]), tmj(yz[1])])),
                        "bvg": np.ascontiguousarray(np.stack([tmj(bv), tmj(g)]))})
    res = launch(nc, in_maps)
    yd = np.concatenate([r["out"].transpose(1, 0, 2).reshape(1024, 1024) for r in res], axis=0)
    return np.ascontiguousarray(yd.T)


def kernel(**inp):
    inp = {k: np.asarray(v) for k, v in inp.items()}
    x = inp["x"].astype(np.float32)
    B, S, Dm = x.shape
    mod = run_ada(inp["c"], inp["ada_w"], inp["ada_b"])
    xT = np.ascontiguousarray(x.reshape(B * S, Dm).T)
    for l in range(2):
        sh1, sc1, g1, sh2, sc2, g2 = [mod[l][:, j * 2048:(j + 1) * 2048] for j in range(6)]
        hT = run_H(xT, inp["norm1_g"][l], sc1, sh1)
        pT = run_P(hT, inp["w_in"][l])
        del hT
        ya = run_LRU(pT, inp, l)
        yb = run_RET(pT, inp["positions"])
        yc = run_SGU(pT, inp, l)
        rw1 = run_RW1(pT, inp, l)
        rw2 = run_RW2(rw1)
        yd = run_RW3(rw1, rw2)
        del rw1, rw2
        ys = np.stack([ya, yb, yc, yd])
        x1T = run_M(ys, pT, xT, g1, inp["w_branch"][l], inp["w_out"][l])
        del pT, ys
        xT = run_E(x1T, inp["norm2_g"][l], sc2, sh2, g2, inp, l)
    z = np.zeros((2, 2048), np.float32)
    oT = run_H(xT, inp["final_norm_g"], z, z, out_dt=F32)
    return np.ascontiguousarray(oT.T).reshape(B, S, Dm).astype(np.float32)
```
